# Optimizing a Trainium2 kernel written in Bass

```python
import jax, jax.numpy as jnp
from jax import lax
import numpy as np

D_MODEL = 1024
BATCH = 8
SEQ = 4096
DEPTH = 4

CHUNK = 64
PLE_DIM = 256
CONV_CH = 256
CONV_WIDTH = 31
GLA_HEADS = 6
GLA_DK = 32
GLA_DV = 64
GLA_GATE_RANK = 16
GLA_GATE_TAU = 16.0
FOX_HEADS = 6
FOX_DH = 64
FOX_BLOCK = 128
MIX_WIDTH = CONV_CH + GLA_HEADS * GLA_DV + FOX_HEADS * FOX_DH
PEER_HEADS = 8
PEER_NKEYS = 128
PEER_N = PEER_NKEYS * PEER_NKEYS
PEER_QDIM = 256
PEER_HALF = PEER_QDIM // 2
PEER_TOPK = 16
PEER_TOKEN_BLOCK = 128
DEEPNORM_ALPHA = (2.0 * DEPTH) ** 0.25
DEEPNORM_BETA = (8.0 * DEPTH) ** -0.25
LN_EPS = 1e-5
IN_SIZES = (CONV_CH, CONV_CH,
            GLA_HEADS * GLA_DK, GLA_HEADS * GLA_DK,
            GLA_HEADS * GLA_DV, GLA_HEADS * GLA_DV,
            GLA_GATE_RANK,
            FOX_HEADS * FOX_DH, FOX_HEADS * FOX_DH, FOX_HEADS * FOX_DH,
            FOX_HEADS)
IN_COLS = sum(IN_SIZES)
IN_SPLITS = tuple(int(s) for s in np.cumsum(IN_SIZES)[:-1])

kernel_name = "hybrid_conv_gla_fox_peer_deepnorm"


def layer_norm(x, g, b):
    xf = x.astype(jnp.float32)
    mu = jnp.mean(xf, axis=-1, keepdims=True)
    var = jnp.mean(jnp.square(xf - mu), axis=-1, keepdims=True)
    y = (xf - mu) * lax.rsqrt(var + LN_EPS)
    return (y * g.astype(jnp.float32) + b.astype(jnp.float32)).astype(x.dtype)


def conv_module(a_val, a_gate, w, bconv, g, b):
    u = a_val * jax.nn.sigmoid(a_gate)
    y = lax.conv_general_dilated(
        u, w[:, None, :].astype(u.dtype), window_strides=(1,),
        padding=[(CONV_WIDTH - 1, 0)],
        dimension_numbers=('NWC', 'WIO', 'NWC'),
        feature_group_count=CONV_CH) + bconv.astype(u.dtype)
    return jax.nn.silu(layer_norm(y, g, b))


def gla(q, k, v, g_out, lr, w2, b2, norm_g):
    bsz, s = q.shape[:2]
    n_chunks = s // CHUNK
    f32 = jnp.float32
    q = q.reshape(bsz, s, GLA_HEADS, GLA_DK).astype(f32) * (GLA_DK ** -0.5)
    k = k.reshape(bsz, s, GLA_HEADS, GLA_DK).astype(f32)
    v = v.reshape(bsz, s, GLA_HEADS, GLA_DV).astype(f32)
    gk = jax.nn.log_sigmoid((lr @ w2 + b2).astype(f32)) / GLA_GATE_TAU
    gk = gk.reshape(bsz, s, GLA_HEADS, GLA_DK)

    def to_chunks(t):
        return t.reshape(bsz, n_chunks, CHUNK, GLA_HEADS, t.shape[-1]).transpose(1, 0, 3, 2, 4)

    mask = jnp.tril(jnp.ones((CHUNK, CHUNK), dtype=bool))[:, :, None]

    def step(state, inp):
        qc, kc, vc, gc = inp
        cum = jnp.cumsum(gc, axis=2)
        inter = jnp.einsum('bhld,bhde->bhle', qc * jnp.exp(cum), state)
        diff = cum[:, :, :, None, :] - cum[:, :, None, :, :]
        decay = jnp.exp(jnp.where(mask, diff, -jnp.inf))
        attn = jnp.einsum('bhtd,bhsd,bhtsd->bhts', qc, kc, decay)
        intra = jnp.einsum('bhts,bhse->bhte', attn, vc)
        last = cum[:, :, -1:, :]
        k_dec = kc * jnp.exp(last - cum)
        new_state = state * jnp.exp(last[:, :, 0, :])[..., None] + \
            jnp.einsum('bhld,bhle->bhde', k_dec, vc)
        return new_state, inter + intra

    state0 = jnp.zeros((bsz, GLA_HEADS, GLA_DK, GLA_DV), f32)
    _, o = lax.scan(step, state0, (to_chunks(q), to_chunks(k), to_chunks(v), to_chunks(gk)))
    o = o.transpose(1, 0, 3, 2, 4).reshape(bsz, s, GLA_HEADS, GLA_DV)
    o = o * lax.rsqrt(jnp.mean(jnp.square(o), axis=-1, keepdims=True) + LN_EPS)
    o = o * norm_g.astype(f32).reshape(GLA_HEADS, GLA_DV)
    o = o.reshape(bsz, s, GLA_HEADS * GLA_DV) * jax.nn.silu(g_out.astype(f32))
    return o.astype(g_out.dtype)


def fox(q, k, v, f_logit, b_f):
    bsz, s = q.shape[:2]
    q = q.reshape(bsz, s, FOX_HEADS, FOX_DH)
    k = k.reshape(bsz, s, FOX_HEADS, FOX_DH)
    v = v.reshape(bsz, s, FOX_HEADS, FOX_DH)
    log_f = jax.nn.log_sigmoid(f_logit.astype(jnp.float32) + b_f.astype(jnp.float32))
    c = jnp.cumsum(log_f, axis=1).transpose(0, 2, 1)
    scale = FOX_DH ** -0.5
    outs = []
    for i in range(s // FOX_BLOCK):
        q0, q1 = i * FOX_BLOCK, (i + 1) * FOX_BLOCK
        sc = jnp.einsum('bqhd,bkhd->bhqk', q[:, q0:q1], k[:, :q1]).astype(jnp.float32) * scale
        sc = sc + c[:, :, q0:q1, None] - c[:, :, None, :q1]
        causal = jnp.arange(q0, q1)[:, None] >= jnp.arange(q1)[None, :]
        pr = jax.nn.softmax(jnp.where(causal, sc, -jnp.inf), axis=-1)
        outs.append(jnp.einsum('bhqk,bkhd->bqhd', pr.astype(v.dtype), v[:, :q1]))
    return jnp.concatenate(outs, axis=1).reshape(bsz, s, FOX_HEADS * FOX_DH)


def peer(x, wq, keys, u, v):
    bsz, s, d = x.shape
    qp = (x @ wq).reshape(bsz, s, PEER_HEADS, 2, PEER_HALF)
    sc = jnp.einsum('bshcd,hcnd->bshcn', qp, keys).astype(jnp.float32)
    sv, si = lax.top_k(sc, PEER_TOPK)
    cand = (sv[..., 0, :, None] + sv[..., 1, None, :]).reshape(bsz, s, PEER_HEADS, PEER_TOPK * PEER_TOPK)
    cv, ci = lax.top_k(cand, PEER_TOPK)
    i1 = jnp.take_along_axis(si[..., 0, :], ci // PEER_TOPK, axis=-1)
    i2 = jnp.take_along_axis(si[..., 1, :], ci % PEER_TOPK, axis=-1)
    n_tok = bsz * s
    n_blk = n_tok // PEER_TOKEN_BLOCK
    eidx = (i1 * PEER_NKEYS + i2).reshape(n_blk, PEER_TOKEN_BLOCK, PEER_HEADS * PEER_TOPK)
    gates = jax.nn.softmax(cv, axis=-1).reshape(n_blk, PEER_TOKEN_BLOCK, PEER_HEADS * PEER_TOPK)
    xt = x.reshape(n_blk, PEER_TOKEN_BLOCK, d)

    def block(args):
        xb, ib, gb = args
        h = jax.nn.gelu(jnp.einsum('td,tkd->tk', xb, u[ib]).astype(jnp.float32))
        return jnp.einsum('tk,tkd->td', (gb * h).astype(xb.dtype), v[ib])

    out = lax.map(block, (xt, eidx, gates))
    return out.reshape(bsz, s, d)


def setup_inputs(seed: int = 0) -> dict:
    key = jax.random.key(seed)
    ks = jax.random.split(key, 24)
    n = lambda k, shape: jax.random.normal(k, shape, jnp.float32)
    L, D = DEPTH, D_MODEL
    return {
        "x": n(ks[0], (BATCH, SEQ, D)),
        "p": n(ks[1], (DEPTH, BATCH, SEQ, PLE_DIM)),
        "w_in": n(ks[2], (L, D, IN_COLS)) * D ** -0.5,
        "conv_w": n(ks[3], (L, CONV_WIDTH, CONV_CH)) * CONV_WIDTH ** -0.5,
        "conv_b": 0.02 * n(ks[4], (L, CONV_CH)),
        "conv_ln_g": 1.0 + 0.02 * n(ks[5], (L, CONV_CH)),
        "conv_ln_b": 0.02 * n(ks[6], (L, CONV_CH)),
        "gla_gate_w": n(ks[7], (L, GLA_GATE_RANK, GLA_HEADS * GLA_DK)) * GLA_GATE_RANK ** -0.5,
        "gla_gate_b": 0.1 * n(ks[8], (L, GLA_HEADS * GLA_DK)),
        "gla_norm_g": 1.0 + 0.02 * n(ks[9], (L, GLA_HEADS * GLA_DV)),
        "fox_forget_b": 3.0 + 0.5 * n(ks[10], (L, FOX_HEADS)),
        "w_out": n(ks[11], (L, MIX_WIDTH, D)) * MIX_WIDTH ** -0.5 * DEEPNORM_BETA,
        "ln1_g": 1.0 + 0.02 * n(ks[12], (L, D)),
        "ln1_b": 0.02 * n(ks[13], (L, D)),
        "peer_wq": n(ks[14], (L, D, PEER_HEADS * PEER_QDIM)) * D ** -0.5,
        "peer_keys": n(ks[15], (L, PEER_HEADS, 2, PEER_NKEYS, PEER_HALF)) * PEER_HALF ** -0.5,
        "peer_u": n(ks[16], (L, PEER_N, D)) * D ** -0.5,
        "peer_v": n(ks[17], (L, PEER_N, D)) * DEEPNORM_BETA * PEER_HEADS ** -0.5,
        "ple_w": n(ks[18], (L, PLE_DIM, D)) * PLE_DIM ** -0.5 * DEEPNORM_BETA,
        "ple_gw": n(ks[19], (L, D, D)) * D ** -0.5,
        "ple_gb": 0.02 * n(ks[20], (L, D)),
        "ln2_g": 1.0 + 0.02 * n(ks[21], (L, D)),
        "ln2_b": 0.02 * n(ks[22], (L, D)),
    }


def reference(x, p, w_in, conv_w, conv_b, conv_ln_g, conv_ln_b, gla_gate_w, gla_gate_b, gla_norm_g,
              fox_forget_b, w_out, ln1_g, ln1_b, peer_wq, peer_keys, peer_u, peer_v,
              ple_w, ple_gw, ple_gb, ln2_g, ln2_b):
    for i in range(DEPTH):
        h = x @ w_in[i]
        (a_val, a_gate, b_q, b_k, b_v, b_g, b_lr, c_q, c_k, c_v, c_f) = jnp.split(h, IN_SPLITS, axis=-1)
        a_out = conv_module(a_val, a_gate, conv_w[i], conv_b[i], conv_ln_g[i], conv_ln_b[i])
        b_out = gla(b_q, b_k, b_v, b_g, b_lr, gla_gate_w[i], gla_gate_b[i], gla_norm_g[i])
        c_out = fox(c_q, c_k, c_v, c_f, fox_forget_b[i])
        mix = jnp.concatenate([a_out, b_out, c_out], axis=-1) @ w_out[i]
        x = layer_norm(DEEPNORM_ALPHA * x + mix, ln1_g[i], ln1_b[i])
        r = DEEPNORM_ALPHA * x + peer(x, peer_wq[i], peer_keys[i], peer_u[i], peer_v[i])
        r = r + jax.nn.sigmoid(r @ ple_gw[i] + ple_gb[i]) * (p[i] @ ple_w[i])
        x = layer_norm(r, ln2_g[i], ln2_b[i])
    return x
```

```python
import numpy as np
import ml_dtypes
from contextlib import ExitStack
import concourse.bass as bass
import concourse.mybir as mybir
from concourse.bass_utils import run_bass_kernel_spmd

F32 = mybir.dt.float32
BF16 = mybir.dt.bfloat16
U32 = mybir.dt.uint32
I32 = mybir.dt.int32
AF = mybir.ActivationFunctionType
ALU = mybir.AluOpType
AX = mybir.AxisListType

D = 1024
S = 4096
NT = 32
DEPTH = 4
IN_COLS = 2838
ALPHA = (2.0 * DEPTH) ** 0.25
EPS = 1e-5
C_AVAL, C_AGATE, C_BQ, C_BK, C_BV, C_BG, C_BLR, C_CQ, C_CK, C_CV, C_CF = (
    0, 256, 512, 704, 896, 1280, 1664, 1680, 2064, 2448, 2832)


class Op:
    __slots__ = ("eng", "fn", "deps", "stream", "needed", "val", "isdma")


class Buf:
    __slots__ = ("name", "w", "r")

    def __init__(self, name):
        self.name = name
        self.w = {}
        self.r = {}


class KB:
    def __init__(self, nc, es):
        self.nc = nc
        self.es = es
        self.ops = []
        self.eng = {"pe": nc.tensor, "act": nc.scalar, "dve": nc.vector,
                    "pool": nc.gpsimd, "sp": nc.sync}
        self.sems = {}
        self.nsem = 0

    def sem(self, name):
        if name not in self.sems:
            self.sems[name] = self.es.enter_context(self.nc.semaphore("s_" + name))
            self.nsem += 1
        return self.sems[name]

    def add(self, eng, fn, reads=(), writes=(), dma=None):
        op = Op()
        op.eng = eng
        op.fn = fn
        op.isdma = dma is not None
        op.stream = ("d_" + dma) if dma is not None else eng
        op.needed = False
        op.val = 0
        deps = set()
        for b in reads:
            deps.update(b.w.values())
        for b in writes:
            deps.update(b.w.values())
            deps.update(b.r.values())
        op.deps = [d for d in deps if not (eng == "pe" and d.stream == "pe")]
        for d in op.deps:
            d.needed = True
        for b in reads:
            b.r[op.stream] = op
        for b in writes:
            b.w = {op.stream: op}
            b.r = {}
        self.ops.append(op)
        return op

    def emit(self):
        counters = {}
        for op in self.ops:
            if op.needed or op.isdma:
                inc = 16 if op.isdma else 1
                counters[op.stream] = counters.get(op.stream, 0) + inc
                op.val = counters[op.stream]
        for st in counters:
            self.sem(st)
        seen = {e: {} for e in self.eng}
        n = 0
        for op in self.ops:
            e = self.eng[op.eng]
            need = {}
            for d in op.deps:
                if d.val > need.get(d.stream, 0):
                    need[d.stream] = d.val
            sn = seen[op.eng]
            for st, v in need.items():
                if sn.get(st, 0) < v:
                    e.wait_ge(self.sems[st], v)
                    sn[st] = v
                    n += 1
            if op.fn is not None:
                ins = op.fn(e)
                n += 1
                if op.needed or op.isdma:
                    ins.then_inc(self.sems[op.stream], 16 if op.isdma else 1)
        return n


def apx(base, dims):
    return bass.AP(base.tensor, base.offset, [list(base.ap[0])] + [list(d) for d in dims])


class Prog:
    def __init__(self, n_layers=DEPTH, dbg=None):
        self.n_layers = n_layers
        self.dbg = dbg or {}
        self.nc = bass.Bass("TRN2", target_bir_lowering=False)
        self.es = ExitStack()
        self.kb = KB(self.nc, self.es)
        self.bufs = {}

    def dram_in(self, name, shape, dt=F32):
        return self.nc.dram_tensor(name, list(shape), dt, kind="ExternalInput").ap()

    def dram_out(self, name, shape, dt=F32):
        return self.nc.dram_tensor(name, list(shape), dt, kind="ExternalOutput").ap()

    def dram_tmp(self, name, shape, dt=F32):
        return self.nc.dram_tensor(name, list(shape), dt, kind="Internal").ap()

    def sb(self, name, shape, dt=F32):
        return self.es.enter_context(self.nc.sbuf_tensor(name, list(shape), dt))

    def B(self, name):
        if name not in self.bufs:
            self.bufs[name] = Buf(name)
        return self.bufs[name]

    def _b(self, xs):
        return [self.B(x) if isinstance(x, str) else x for x in xs]

    def op(self, eng, fn, r=(), w=()):
        return self.kb.add(eng, fn, self._b(r), self._b(w))

    def dma(self, q, sem, out, in_, r=(), w=(), **kw):
        e = {"sp": "sp", "pool": "pool", "act": "act"}[q]
        return self.kb.add(e, lambda g: g.dma_start(out=out, in_=in_, **kw),
                           self._b(r), self._b(w), dma=sem)

    def mm(self, out, lhsT, rhs, start, stop, r=(), w=()):
        return self.op("pe", lambda g: g.matmul(out, lhsT, rhs, start=start, stop=stop), r, w)

    def tr(self, out, in_, ident, r=(), w=()):
        return self.op("pe", lambda g: g.transpose(out, in_, ident), r, w)

    def act(self, out, in_, func, r=(), w=(), **kw):
        return self.op("act", lambda g: g.activation(out=out, in_=in_, func=func, **kw), r, w)

    def tt(self, eng, out, in0, in1, op, r=(), w=()):
        return self.op(eng, lambda g: g.tensor_tensor(out=out, in0=in0, in1=in1, op=op), r, w)

    def ts(self, eng, out, in0, s1, s2, op0, op1=None, r=(), w=(), **kw):
        if op1 is None:
            return self.op(eng, lambda g: g.tensor_scalar(out=out, in0=in0, scalar1=s1, scalar2=None,
                                                          op0=op0, **kw), r, w)
        return self.op(eng, lambda g: g.tensor_scalar(out=out, in0=in0, scalar1=s1, scalar2=s2,
                                                      op0=op0, op1=op1, **kw), r, w)

    def stt(self, out, in0, scalar, in1, op0, op1, r=(), w=(), **kw):
        return self.op("dve", lambda g: g.scalar_tensor_tensor(out=out, in0=in0, scalar=scalar, in1=in1,
                                                               op0=op0, op1=op1, **kw), r, w)

    def cp(self, eng, out, in_, r=(), w=()):
        if eng == "act":
            return self.op("act", lambda g: g.copy(out=out, in_=in_), r, w)
        return self.op(eng, lambda g: g.tensor_copy(out=out, in_=in_), r, w)


CP_CB, CP_LG, CP_LB, CP_CW, CP_GB, CP_NG, CP_FB, CP_N = 0, 2, 4, 6, 68, 74, 80, 86
CF_IDENT, CF_ONES, CF_SCM, CF_IOTA, CF_E0, CF_E1, CF_M0, CF_M1, CF_N = 0, 128, 256, 768, 784, 785, 786, 787, 788
CB_IDENT, CB_CMASK, CB_GMASK, CB_ONES, CB_N = 0, 128, 256, 384, 512


def host_consts():
    cf = np.zeros((128, CF_N), np.float32)
    cf[:, CF_IDENT:CF_IDENT + 128] = np.eye(128, dtype=np.float32)
    cf[:, CF_ONES:CF_ONES + 128] = 1.0
    scm = np.ones((512,), np.float32)
    scm[::64] = 0.0
    cf[:, CF_SCM:CF_SCM + 512] = scm[None, :]
    cf[:, CF_IOTA:CF_IOTA + 16] = np.arange(16, dtype=np.float32)[None, :]
    cf[64, CF_E0] = 1.0
    cf[65, CF_E1] = 1.0
    cf[0:64, CF_M0] = 1.0
    cf[64:128, CF_M1] = 1.0
    cb = np.zeros((128, CB_N), np.float32)
    cb[:, CB_IDENT:CB_IDENT + 128] = np.eye(128)
    s = np.arange(128)[:, None]
    t = np.arange(128)[None, :]
    cb[:, CB_CMASK:CB_CMASK + 128] = (t >= s)
    cb[:, CB_GMASK:CB_GMASK + 128] = (t >= s) & ((t // 64) == (s // 64))
    cb[:, CB_ONES:CB_ONES + 128] = 1.0
    return cf, cb.astype(ml_dtypes.bfloat16)


GLA_STAGE = 99
PEER_STAGE = 99
PEER_TILES = NT


class Program(Prog):
    def build(self):
        nc = self.nc
        P = self
        L = DEPTH
        self.x_in = P.dram_in("x", [S, D])
        self.p_in = P.dram_in("p", [L, S, 256])
        self.w_in = P.dram_in("w_in", [L, D, IN_COLS])
        self.chanp_d = P.dram_in("chanp", [L, 128, CP_N])
        self.gatew_d = P.dram_in("gla_gate_w", [L, 16, 192])
        self.w_out = P.dram_in("w_out", [L, D, D])
        self.ln1_g = P.dram_in("ln1_g", [L, D]); self.ln1_b = P.dram_in("ln1_b", [L, D])
        self.wq = P.dram_in("peer_wq", [L, D, 2048])
        self.keysT = P.dram_in("keysT", [L, 16, 128, 128])
        self.pu = P.dram_in("peer_u", [L, 16384, D])
        self.pv = P.dram_in("peer_v", [L, 16384, D])
        self.ple_w = P.dram_in("ple_w", [L, 256, D])
        self.ple_gw = P.dram_in("ple_gw", [L, D, D])
        self.ple_gb = P.dram_in("ple_gb", [L, D])
        self.ln2_g = P.dram_in("ln2_g", [L, D]); self.ln2_b = P.dram_in("ln2_b", [L, D])
        self.cf_d = P.dram_in("cf32", [128, CF_N])
        self.cb_d = P.dram_in("cb16", [128, CB_N], BF16)
        self.y = P.dram_out("y", [S, D])
        self.xcur = P.dram_tmp("xcur", [S, D])
        self.x1d = P.dram_tmp("x1d", [S, D])
        self.mixT = P.dram_tmp("mixT", [D, S], BF16)
        self.dbg_out = {}
        for name, (shape, dt, attr, bufs) in self.dbg.items():
            self.dbg_out[name] = P.dram_out("dbg_" + name, shape, dt)
        self.XT = P.sb("XT", [128, 8, S], BF16)
        self.FA = P.sb("FA", [128, 4, 4128], F32)
        self.BA = P.sb("BA", [128, 4, S], BF16)
        self.WB = [P.sb("WB0", [128, 8, 256], BF16), P.sb("WB1", [128, 8, 256], BF16)]
        self.CF = P.sb("CF", [128, CF_N], F32)
        self.CB = P.sb("CB", [128, CB_N], BF16)
        self.CHP = P.sb("CHP", [128, CP_N], F32)
        self.XBT = [P.sb("XBT0", [128, 1024], BF16), P.sb("XBT1", [128, 1024], BF16)]
        self.T5 = [P.sb(f"T5_{i}", [128, 512], F32) for i in range(6)]
        self.VT = P.sb("VT", [128, NT, 64], BF16)
        self.KET = P.sb("KET", [128, 2, NT, 32], BF16)
        self.CT = P.sb("CT", [128, NT], F32)
        self.PT = [P.sb(f"PT{i}", [128, 512], BF16) for i in range(3)]
        self.GW = P.sb("GW", [16, 192], BF16)
        self.SM = P.sb("SM", [128, 64], F32)
        self.PK = P.sb("PK", [128, 1536], F32)
        self.EIDX = P.sb("EIDX", [128, 128], U32)
        self.PS = [self.es.enter_context(nc.psum_tensor(f"ps{i}", [128, 512], F32)) for i in range(8)]
        self.ps_rot = 0
        self.identb = self.CB[:, CB_IDENT:CB_IDENT + 128]
        self.identf = self.CF[:, CF_IDENT:CF_IDENT + 128]
        self.onesf = self.CF[:, CF_ONES:CF_ONES + 128]
        self.onesb = self.CB[:, CB_ONES:CB_ONES + 128]
        P.dma("sp", "cst", self.CF[:], self.cf_d, w=["CF"])
        P.dma("sp", "cst", self.CB[:], self.cb_d, w=["CB"])
        P.op("dve", lambda g: g.memset(self.FA[:, 0, 0:30], 0.0), w=["F0"])

        for l in range(self.n_layers):
            self.layer(l)

        fin = []
        for name, (shape, dt, attr, bufs) in self.dbg.items():
            P.dma("sp", "dbg", self.dbg_out[name], getattr(self, attr), r=bufs, w=["dbg_" + name])
            fin.append("dbg_" + name)
        fin.append("y")
        P.op("sp", None, r=fin)
        n = self.kb.emit()
        return n

    def psn(self, banks):
        b = banks[self.ps_rot % len(banks)]
        self.ps_rot += 1
        return b

    def F(self, i, lo=0, hi=4096):
        return self.FA[:, i, lo:hi]

    def layer(self, l):
        P = self
        src = self.x_in if l == 0 else self.xcur
        P.dma("sp", "chp", self.CHP[:], self.chanp_d[l], r=[], w=["CHP"])
        P.dma("pool", "gw", self.GW[:], self.gatew_d[l], r=[], w=["GW"])
        self.phase_xT(src, "xsrc")
        self.unit_conv(l)
        self.lrt_done = False
        for h in range(6):
            self.unit_gla(l, h)
        for h in range(6):
            self.unit_fox(l, h)
        self.phase_out(l, src)
        self.phase_peer(l)

    def phase_xT(self, src, srcname):
        P = self
        for i in range(NT):
            s = i % 2
            xb = self.XBT[s]
            P.dma("pool", f"xbt{s}", xb[:], src[i * 128:(i + 1) * 128, :], r=[srcname], w=[f"XBT{s}"])
            self.transpose_to_XT(xb, f"XBT{s}", i)

    def transpose_to_XT(self, xb, xbname, i):
        P = self
        bank = self.psn([0, 1])
        psb = self.PS[bank][:].bitcast(BF16)
        for c in range(8):
            P.tr(psb[:, c * 128:(c + 1) * 128], xb[:, c * 128:(c + 1) * 128], self.identb,
                 r=[xbname, "CB"], w=[f"ps{bank}"])
        P.cp("act" if i % 2 else "dve", self.XT[:, :, i * 128:(i + 1) * 128],
             psb.rearrange("p (c t) -> p c t", c=8), r=[f"ps{bank}"], w=["XT"])

    def load_w(self, slot, l, cols):
        off = 0
        for (c0, n) in cols:
            kw = dict(allow_slow_non_contiguous=True) if n == 1 else {}
            self.dma("pool", f"wb{slot}", self.WB[slot][:, :, off:off + n],
                     self.w_in[l, :, c0:c0 + n].rearrange("(c p) n -> p c n", p=128), r=[], w=[f"WB{slot}"], **kw)
            off += n

    def proj_fm(self, bank, slot, woff, m, g, prow=0):
        for c in range(8):
            self.mm(self.PS[bank][prow:prow + m, :], self.WB[slot][:, c, woff:woff + m],
                    self.XT[:, c, g * 512:(g + 1) * 512], start=(c == 0), stop=(c == 7),
                    r=[f"WB{slot}", "XT"], w=[f"ps{bank}"])

    def proj_v_tm(self, slot, woff, g, banks):
        bank = self.psn(banks)
        for j in range(4):
            i = 4 * g + j
            for c in range(8):
                self.mm(self.PS[bank][:, j * 64:(j + 1) * 64], self.XT[:, c, i * 128:(i + 1) * 128],
                        self.WB[slot][:, c, woff:woff + 64], start=(c == 0), stop=(c == 7),
                        r=[f"WB{slot}", "XT"], w=[f"ps{bank}"])
        self.cp("act", self.VT[:, 4 * g:4 * g + 4, :],
                self.PS[bank][:, 0:256].rearrange("p (j e) -> p j e", j=4), r=[f"ps{bank}"], w=["VT"])

    def unit_conv(self, l):
        P = self
        UP = self.FA[:, 0, :]
        for j in range(2):
            slot = j
            self.load_w(slot, l, [(C_AVAL + j * 128, 128), (C_AGATE + j * 128, 128)])
            for g in range(8):
                a = self.psn([2, 3, 4, 5]); b = self.psn([2, 3, 4, 5])
                self.proj_fm(a, slot, 0, 128, g)
                self.proj_fm(b, slot, 128, 128, g)
                t = self.T5[g % 2]
                P.act(t[:], self.PS[b][:], AF.Sigmoid, r=[f"ps{b}"], w=[f"T5_{g % 2}"])
                P.tt("dve", UP[:, 30 + g * 512:30 + (g + 1) * 512], self.PS[a][:], t[:], ALU.mult,
                     r=[f"ps{a}", f"T5_{g % 2}"], w=["F0"])
            acc = self.F(1 + j)
            cw = self.CHP[:, CP_CW + j * 31:CP_CW + (j + 1) * 31]
            P.ts("dve", acc, UP[:, 0:4096], cw[:, 0:1], self.CHP[:, CP_CB + j:CP_CB + j + 1],
                 ALU.mult, ALU.add, r=["F0", "CHP"], w=[f"F{1 + j}"])
            for k in range(1, 31):
                P.stt(acc, UP[:, k:k + 4096], cw[:, k:k + 1], acc, ALU.mult, ALU.add,
                      r=["F0", "CHP", f"F{1 + j}"], w=[f"F{1 + j}"])
        for g in range(8):
            gs = slice(g * 512, (g + 1) * 512)
            pm = self.psn([2, 3, 4, 5]); pe = self.psn([2, 3, 4, 5])
            for j in range(2):
                P.mm(self.PS[pm][:], self.onesf, self.F(1 + j)[:, gs], start=(j == 0), stop=(j == 1),
                     r=["CF", f"F{1 + j}"], w=[f"ps{pm}"])
            for j in range(2):
                sq = self.T5[j]
                P.act(sq[:], self.F(1 + j)[:, gs], AF.Square, r=[f"F{1 + j}"], w=[f"T5_{j}"])
                P.mm(self.PS[pe][:], self.onesf, sq[:], start=(j == 0), stop=(j == 1),
                     r=["CF", f"T5_{j}"], w=[f"ps{pe}"])
            mean = self.T5[2]; msq = self.T5[3]; var = self.T5[4]
            P.act(mean[:], self.PS[pm][:], AF.Copy, r=[f"ps{pm}"], w=["T5_2"], scale=1.0 / 256)
            P.act(msq[:], mean[:], AF.Square, r=["T5_2"], w=["T5_3"])
            P.stt(var[:], self.PS[pe][:], 1.0 / 256, msq[:], ALU.mult, ALU.subtract, r=[f"ps{pe}", "T5_3"], w=["T5_4"])
            P.ts("dve", var[:], var[:], 0.0, EPS, ALU.max, ALU.add, r=["T5_4"], w=["T5_4"])
            P.act(var[:], var[:], AF.Sqrt, r=["T5_4"], w=["T5_4"])
            P.op("dve", lambda g_, v=var: g_.reciprocal(out=v[:], in_=v[:]), r=["T5_4"], w=["T5_4"])
            for j in range(2):
                t = self.T5[j]
                P.tt("dve", t[:], self.F(1 + j)[:, gs], mean[:], ALU.subtract, r=[f"F{1 + j}", "T5_2"], w=[f"T5_{j}"])
                P.tt("dve", t[:], t[:], var[:], ALU.mult, r=[f"T5_{j}", "T5_4"], w=[f"T5_{j}"])
                P.act(self.BA[:, j, gs], t[:], AF.Silu, r=[f"T5_{j}", "CHP"], w=[f"B{j}"],
                      scale=self.CHP[:, CP_LG + j:CP_LG + j + 1], bias=self.CHP[:, CP_LB + j:CP_LB + j + 1])
        for j in range(2):
            P.dma("sp", f"mixw{j}", self.mixT[j * 128:(j + 1) * 128, :], self.BA[:, j, :], r=[f"B{j}"], w=["mixT"])

    def gla_end(self, h):
        self.dma("sp", f"mixw{h % 2}", self.mixT[256 + h * 64:320 + h * 64, :], self.BA[0:64, 2, :], r=["B2"], w=["mixT"])

    def unit_gla(self, l, h):
        P = self
        slot = h % 2
        wn = f"WB{slot}"
        banks = [0, 1, 2, 3, 4, 5]
        QE = self.BA[:, 0, :]; KE = self.BA[:, 1, :]; AT = self.BA[:, 2, :]; SB = self.BA[:, 3, :]
        GZ = self.FA[:, 0, 30:4126]; EC = self.F(1); EN = self.F(2); ST = self.F(3)
        D1 = GZ; D0 = EN
        rq = slice(0, 32)
        self.load_w(slot, l, [(C_BQ + h * 32, 32), (C_BK + h * 32, 32), (C_BV + h * 64, 64), (C_BG + h * 64, 64),
                              (C_BLR, 16)])
        for g in range(8):
            gs = slice(g * 512, (g + 1) * 512)
            a0 = self.psn(banks)
            self.proj_fm(a0, slot, 192, 16, g)
            lrt = self.PT[g % 2]; lrn = f"PT{g % 2}"
            P.cp("act", lrt[0:16, :], self.PS[a0][0:16, :], r=[f"ps{a0}"], w=[lrn])
            a = self.psn(banks)
            P.mm(self.PS[a][rq, :], self.GW[0:16, h * 32:(h + 1) * 32], lrt[0:16, :], start=True, stop=True,
                 r=["GW", lrn], w=[f"ps{a}"])
            P.ts("dve", GZ[rq, gs], self.PS[a][rq, :], self.CHP[rq, CP_GB + h:CP_GB + h + 1], None, ALU.add,
                 r=[f"ps{a}", "CHP"], w=["F0"])
        P.act(GZ[rq, :], GZ[rq, :], AF.Exp, r=["F0"], w=["F0"], scale=-1.0)
        P.ts("dve", GZ[rq, :], GZ[rq, :], 1.0, None, ALU.add, r=["F0"], w=["F0"])
        P.act(GZ[rq, :], GZ[rq, :], AF.Ln, r=["F0"], w=["F0"])
        scm = self.CF[rq, CF_SCM:CF_SCM + 512]
        for g in range(8):
            gs = slice(g * 512, (g + 1) * 512)
            P.op("dve", lambda g_, gs=gs: g_.tensor_tensor_scan(
                out=ST[rq, gs], data0=scm, data1=GZ[rq, gs], initial=0.0, op0=ALU.mult, op1=ALU.subtract),
                r=["F0", "CF"], w=["F3"])
        P.act(EC[rq, :], ST[rq, :], AF.Exp, r=["F3"], w=["F1"], scale=1.0 / 16)
        P.act(EN[rq, :], ST[rq, :], AF.Exp, r=["F3"], w=["F2"], scale=-1.0 / 16)
        if GLA_STAGE < 1:
            return
        for g in range(8):
            gs = slice(g * 512, (g + 1) * 512)
            a = self.psn(banks)
            self.proj_fm(a, slot, 0, 32, g)
            P.stt(QE[rq, gs], self.PS[a][rq, :], 32.0 ** -0.5, EC[rq, gs], ALU.mult, ALU.mult,
                  r=[f"ps{a}", "F1"], w=["B0"])
            b = self.psn(banks)
            self.proj_fm(b, slot, 32, 32, g)
            P.tt("dve", KE[rq, gs], self.PS[b][rq, :], EN[rq, gs], ALU.mult, r=[f"ps{b}", "F2"], w=["B1"])
            self.proj_v_tm(slot, 64, g, banks)
        if GLA_STAGE < 2:
            return
        pk = self.psn(banks)
        pkb = self.PS[pk][:].bitcast(BF16)
        for i in range(NT):
            P.tr(pkb[:, i * 32:(i + 1) * 32], KE[rq, i * 128:(i + 1) * 128], self.identb[0:32, 0:32],
                 r=["B1", "CB"], w=[f"ps{pk}"])
        for m in range(2):
            P.ts("dve", self.KET[:, m, :, :], pkb.rearrange("p (i d) -> p i d", i=NT),
                 self.CF[:, CF_M0 + m:CF_M0 + m + 1], None, ALU.mult, r=[f"ps{pk}", "CF"], w=["KET"])
        if GLA_STAGE < 3:
            return
        ATv = AT.rearrange("p (i t) -> p i t", i=NT)
        gmask4 = apx(self.CB[:, CB_GMASK:CB_GMASK + 128], [[0, 4], [1, 128]])
        for i4 in range(8):
            a = self.psn(banks)
            for j in range(4):
                i = 4 * i4 + j
                P.mm(self.PS[a][:, j * 128:(j + 1) * 128], KE[rq, i * 128:(i + 1) * 128], QE[rq, i * 128:(i + 1) * 128],
                     start=True, stop=True, r=["B0", "B1"], w=[f"ps{a}"])
            for j in range(4):
                if GLA_STAGE == 3.5:
                    break
                P.tt("dve", ATv[:, 4 * i4 + j, :], self.PS[a][:, j * 128:(j + 1) * 128],
                     self.CB[:, CB_GMASK:CB_GMASK + 128], ALU.mult, r=[f"ps{a}", "CB"], w=["B2"])
        if GLA_STAGE < 4:
            return self.gla_end(h)
        for c8 in range(8):
            a = self.psn(banks)
            for cc in range(8):
                c = c8 * 8 + cc
                i = c // 2; r0 = (c % 2) * 64
                P.mm(self.PS[a][rq, cc * 64:(cc + 1) * 64], self.KET[:, c % 2, i, :], self.VT[:, i, :],
                     start=True, stop=True, r=["KET", "VT"], w=[f"ps{a}"])
            ac = apx(EC[rq, 64 * (c8 * 8) + 63:64 * (c8 * 8) + 64], [[64, 8], [0, 64]])
            out = apx(D1[rq, c8 * 8:c8 * 8 + 1], [[1, 8], [64, 64]])
            P.tt("dve", out, self.PS[a][rq, :].rearrange("p (c e) -> p c e", c=8), ac, ALU.mult,
                 r=[f"ps{a}", "F1"], w=["F0"])
        if GLA_STAGE < 5:
            return
        P.cp("dve", D0[rq, :].rearrange("p (e c) -> p e c", e=64), apx(EC[rq, 63:64], [[0, 64], [64, 64]]),
             r=["F1"], w=["F2"])
        P.op("dve", lambda g_: g_.memset(apx(D0[rq, 0:1], [[64, 64]]), 0.0), w=["F2"])
        P.op("dve", lambda g_: g_.tensor_tensor_scan(out=ST[rq, :], data0=D0[rq, :], data1=D1[rq, :], initial=0.0,
                                                    op0=ALU.mult, op1=ALU.add), r=["F0", "F2"], w=["F3"])
        if GLA_STAGE < 6:
            return
        P.cp("act", SB[rq, :].rearrange("p (c e) -> p c e", c=64), apx(ST[rq, 0:1], [[1, 64], [64, 64]]),
             r=["F3"], w=["B3"])
        if GLA_STAGE < 7:
            return
        r0m = 256 + h * 64
        BO = KE
        for g in range(8):
            gs = slice(g * 512, (g + 1) * 512)
            po = self.psn(banks)
            for cc in range(8):
                c = g * 8 + cc
                i = c // 2; r0 = (c % 2) * 64
                cs = slice(cc * 64, (cc + 1) * 64)
                P.mm(self.PS[po][0:64, cs], self.VT[:, i, :], ATv[:, i, r0:r0 + 64],
                     start=True, stop=(c == 0), r=["VT", "B2"], w=[f"ps{po}"])
                if c > 0:
                    P.mm(self.PS[po][0:64, cs], SB[rq, (c - 1) * 64:c * 64], QE[rq, c * 64:(c + 1) * 64],
                         start=False, stop=True, r=["B3", "B0"], w=[f"ps{po}"])
            ot = self.T5[0]; sq = self.T5[1]; rst = self.T5[2]; sg = self.T5[3]
            P.act(ot[0:64, :], self.PS[po][0:64, :], AF.Copy, r=[f"ps{po}"], w=["T5_0"])
            P.act(sq[0:64, :], self.PS[po][0:64, :], AF.Square, r=[f"ps{po}"], w=["T5_1"])
            pm = self.psn(banks)
            P.mm(self.PS[pm][0:64, :], self.onesf[0:64, 0:64], sq[0:64, :], start=True, stop=True,
                 r=["CF", "T5_1"], w=[f"ps{pm}"])
            P.ts("dve", rst[0:64, :], self.PS[pm][0:64, :], 1.0 / 64, EPS, ALU.mult, ALU.add, r=[f"ps{pm}"], w=["T5_2"])
            P.act(rst[0:64, :], rst[0:64, :], AF.Sqrt, r=["T5_2"], w=["T5_2"])
            P.op("dve", lambda g_, rst=rst: g_.reciprocal(out=rst[0:64, :], in_=rst[0:64, :]), r=["T5_2"], w=["T5_2"])
            pg = self.psn(banks)
            self.proj_fm(pg, slot, 128, 64, g)
            P.act(sg[0:64, :], self.PS[pg][0:64, :], AF.Silu, r=[f"ps{pg}"], w=["T5_3"])
            P.tt("dve", ot[0:64, :], ot[0:64, :], rst[0:64, :], ALU.mult, r=["T5_0", "T5_2"], w=["T5_0"])
            P.stt(BO[0:64, gs], ot[0:64, :], self.CHP[0:64, CP_NG + h:CP_NG + h + 1],
                  sg[0:64, :], ALU.mult, ALU.mult, r=["T5_0", "T5_3", "CHP"], w=["B1"])
        P.dma("sp", f"mixw{h % 2}", self.mixT[r0m:r0m + 64, :], BO[0:64, :], r=["B1"], w=["mixT"])

    def unit_fox(self, l, h):
        P = self
        slot = h % 2
        WBs = self.WB[slot]
        wn = f"WB{slot}"
        self.load_w(slot, l, [(C_CQ + h * 64, 64), (C_CF + h, 1), (C_CF + h, 1),
                              (C_CK + h * 64, 64), (C_CV + h * 64, 64)])
        QA = self.BA[:, 0, :]; KA = self.BA[:, 1, :]; CO = self.BA[:, 2, :]; TB = self.BA[:, 3, :]
        FZ = self.F(1); CC = self.F(2); HF = self.F(3)
        r2 = slice(64, 66)
        fb = self.CHP[64:66, CP_FB + h:CP_FB + h + 1]
        banks = [0, 1, 2, 3, 4, 5]
        for g in range(8):
            gs = slice(g * 512, (g + 1) * 512)
            a = self.psn(banks)
            self.proj_fm(a, slot, 0, 66, g)
            P.act(QA[0:64, gs], self.PS[a][0:64, :], AF.Copy, r=[f"ps{a}"], w=["B0"], scale=0.125)
            P.ts("dve", FZ[r2, gs], self.PS[a][r2, :], fb, None, ALU.add, r=[f"ps{a}", "CHP"], w=["F1"])
            b = self.psn(banks)
            self.proj_fm(b, slot, 66, 64, g)
            P.cp("dve", KA[0:64, gs], self.PS[b][0:64, :], r=[f"ps{b}"], w=["B1"])
            self.proj_v_tm(slot, 130, g, banks)
        P.act(FZ[r2, :], FZ[r2, :], AF.Exp, r=["F1"], w=["F1"], scale=-1.0)
        P.ts("dve", FZ[r2, :], FZ[r2, :], 1.0, None, ALU.add, r=["F1"], w=["F1"])
        P.act(FZ[r2, :], FZ[r2, :], AF.Ln, r=["F1"], w=["F1"])
        ones512 = self.CF[r2, CF_ONES:CF_ONES + 128]
        for pc in range(32):
            sl = slice(pc * 128, (pc + 1) * 128)
            init = 0.0 if pc == 0 else CC[r2, pc * 128 - 1:pc * 128]
            P.op("dve", lambda g_, sl=sl, init=init: g_.tensor_tensor_scan(
                out=CC[r2, sl], data0=ones512, data1=FZ[r2, sl], initial=init,
                op0=ALU.mult, op1=ALU.subtract), r=["F1", "F2", "CF"], w=["F2"])
        P.cp("dve", TB[r2, :], CC[r2, :], r=["F2"], w=["B3"])
        P.cp("dve", HF[r2, :], TB[r2, :], r=["B3"], w=["F3"])
        P.tt("dve", FZ[r2, :], CC[r2, :], HF[r2, :], ALU.subtract, r=["F2", "F3"], w=["F1"])
        P.ts("dve", HF[r2, :], HF[r2, :], self.CF[r2, CF_E0:CF_E0 + 1], None, ALU.mult, r=["F3", "CF"], w=["F3"])
        P.stt(QA[r2, :], FZ[r2, :], self.CF[r2, CF_E1:CF_E1 + 1], HF[r2, :], ALU.mult, ALU.add,
              r=["F1", "F3", "CF"], w=["B0"])
        P.op("dve", lambda g_: g_.memset(KA[r2, :], 1.0), w=["B1"])
        pb = self.psn(banks)
        for i in range(NT):
            P.tr(self.PS[pb][:, i:i + 1], CC[64:65, i * 128:(i + 1) * 128], self.identf[64:65, 64:65],
                 r=["F2", "CF"], w=[f"ps{pb}"])
        P.act(self.CT[:], self.PS[pb][:, 0:NT], AF.Copy, r=[f"ps{pb}"], w=["CT"], scale=-1.0)
        pO, pS = 6, 7
        it = 0
        for qg in range(8):
            nkb = 4 * (qg + 1)
            gs0 = qg * 512
            for kb in range(nkb):
                col0 = max(0, (kb - 4 * qg) * 128)
                pst = self.psn(banks)
                P.mm(self.PS[pst][:, col0:512], KA[0:66, kb * 128:(kb + 1) * 128], QA[0:66, gs0 + col0:gs0 + 512],
                     start=True, stop=True, r=["B0", "B1"], w=[f"ps{pst}"])
                pt = self.PT[it % 3]; ptn = f"PT{it % 3}"; it += 1
                P.act(pt[:, col0:512], self.PS[pst][:, col0:512], AF.Exp, r=[f"ps{pst}", "CT"], w=[ptn],
                      bias=self.CT[:, kb:kb + 1], scale=1.0)
                if kb >= 4 * qg:
                    P.tt("pool", pt[:, col0:col0 + 128], pt[:, col0:col0 + 128],
                         self.CB[:, CB_CMASK:CB_CMASK + 128], ALU.mult, r=[ptn, "CB"], w=[ptn])
                P.mm(self.PS[pO][0:64, col0:512], self.VT[:, kb, :], pt[:, col0:512],
                     start=(kb == 0), stop=(kb == nkb - 1), r=["VT", ptn], w=[f"ps{pO}"])
                P.mm(self.PS[pS][0:64, col0:512], self.onesb[:, 0:64], pt[:, col0:512],
                     start=(kb == 0), stop=(kb == nkb - 1), r=["CB", ptn], w=[f"ps{pS}"])
            rs = self.T5[5]
            P.op("dve", lambda g_, rs=rs: g_.reciprocal(out=rs[0:64, :], in_=self.PS[pS][0:64, :]),
                 r=[f"ps{pS}"], w=["T5_5"])
            P.tt("dve", CO[0:64, gs0:gs0 + 512], self.PS[pO][0:64, :], rs[0:64, :], ALU.mult,
                 r=[f"ps{pO}", "T5_5"], w=["B2"])
        r0 = 640 + h * 64
        P.dma("sp", f"mixw{h % 2}", self.mixT[r0:r0 + 64, :], CO[0:64, :], r=["B2"], w=["mixT"])

    def phase_out(self, l, src):
        P = self
        WO = self.BA[:, 0:2, :].rearrange("p a (c n) -> p (a c) n", n=1024)
        for c in range(8):
            P.dma("pool", "wo", WO[:, c, :], self.w_out[l, c * 128:(c + 1) * 128, :], r=[], w=["B0", "B1"])
        G1 = self.FA[:, 0, 30:1054]; B1 = self.FA[:, 0, 1054:2078]
        P.dma("sp", "lnp", G1, self.ln1_g[l:l + 1, :].partition_broadcast(128), r=[], w=["F0"])
        P.dma("sp", "lnp", B1, self.ln1_b[l:l + 1, :].partition_broadcast(128), r=[], w=["F0"])
        for i in range(NT):
            g = i // 4
            ms = g % 2
            MX = self.BA[:, 2 + ms, :].rearrange("p (c n) -> p c n", c=8)
            if i % 4 == 0:
                P.dma("sp", f"mx{ms}", MX, self.mixT[:, g * 512:(g + 1) * 512].rearrange("(c p) n -> p c n", p=128),
                      r=["mixT"], w=[f"B{2 + ms}"])
            s2 = i % 2
            XR = self.FA[:, 1, s2 * 1024:(s2 + 1) * 1024]
            R = self.FA[:, 1, 2048 + s2 * 1024:2048 + (s2 + 1) * 1024]
            xrn = f"XR{s2}"; rn = f"R{s2}"
            P.dma("sp", f"xr{s2}", XR, src[i * 128:(i + 1) * 128, :], r=["xsrc"], w=[xrn])
            for half in range(2):
                pb = self.psn([2, 3, 4, 5])
                for c in range(8):
                    P.mm(self.PS[pb][:], MX[:, c, (i % 4) * 128:(i % 4 + 1) * 128], WO[:, c, half * 512:(half + 1) * 512],
                         start=(c == 0), stop=(c == 7), r=[f"B{2 + ms}", "B0", "B1"], w=[f"ps{pb}"])
                P.stt(R[:, half * 512:(half + 1) * 512], XR[:, half * 512:(half + 1) * 512], ALPHA, self.PS[pb][:],
                      ALU.mult, ALU.add, r=[xrn, f"ps{pb}"], w=[rn])
            self.layernorm_tile(R, rn, G1, B1, "F0")
            P.dma("sp", f"x1w{s2}", self.x1d[i * 128:(i + 1) * 128, :], R, r=[rn], w=["x1d"])
            xb = self.XBT[s2]
            P.cp("act", xb[:], R, r=[rn], w=[f"XBT{s2}"])
            self.transpose_to_XT(xb, f"XBT{s2}", i)

    def layernorm_tile(self, R, rn, G, Bb, gbn):
        P = self
        st = self.SM[:, 0:12]; mv = self.SM[:, 12:14]; rs = self.SM[:, 14:15]
        for half in range(2):
            P.op("dve", lambda g_, half=half: g_.bn_stats(out=st[:, half * 6:(half + 1) * 6],
                                                        in_=R[:, half * 512:(half + 1) * 512]), r=[rn], w=["SM"])
        P.op("dve", lambda g_: g_.bn_aggr(out=mv, in_=st), r=["SM"], w=["SM"])
        P.ts("dve", rs, mv[:, 1:2], EPS, None, ALU.add, r=["SM"], w=["SM"])
        P.act(rs, rs, AF.Sqrt, r=["SM"], w=["SM"])
        P.op("dve", lambda g_: g_.reciprocal(out=rs, in_=rs), r=["SM"], w=["SM"])
        P.ts("dve", R, R, mv[:, 0:1], rs, ALU.subtract, ALU.mult, r=[rn, "SM"], w=[rn])
        P.tt("pool", R, R, G, ALU.mult, r=[rn, gbn], w=[rn])
        P.tt("dve", R, R, Bb, ALU.add, r=[rn, gbn], w=[rn])

    def phase_peer(self, l):
        P = self
        last = (l == self.n_layers - 1)
        dst = self.y if (l == DEPTH - 1) else self.xcur
        dstn = "y" if (l == DEPTH - 1) else "xsrc"
        allb = [0, 1, 2, 3, 4, 5, 6, 7]
        WQ = self.BA[:, 0:4, :].rearrange("p a (c n) -> p (a c) n", n=2048)
        for c in range(8):
            P.dma("pool", "wo", WQ[:, c, :], self.wq[l, c * 128:(c + 1) * 128, :], r=[], w=["B0", "B1", "B2", "B3"])
        KT = self.VT[:].rearrange("p a b -> p (a b)").rearrange("p (h n) -> p h n", h=16)
        P.dma("pool", "kt", KT, self.keysT[l].rearrange("h d n -> d h n"), r=[], w=["VT"])
        QPT = self.KET[:].rearrange("p a b c -> p (a b c)").rearrange("p (h t) -> p h t", h=16)
        GW2 = self.FA[:, 3, 0:4096].bitcast(BF16).rearrange("p (c n) -> p c n", c=8)
        for c in range(8):
            P.dma("pool", "gw2", GW2[:, c, :], self.ple_gw[l, c * 128:(c + 1) * 128, :], r=[], w=["F3"])
        PW = self.FA[:, 2, 0:1024].bitcast(BF16).rearrange("p (c n) -> p c n", c=2)
        for c in range(2):
            P.dma("pool", "gw2", PW[:, c, :], self.ple_w[l, c * 128:(c + 1) * 128, :], r=[], w=["F2"])
        G2 = self.FA[:, 2, 1024:2048]; B2 = self.FA[:, 2, 2048:3072]; GB = self.FA[:, 2, 3072:4096]
        P.dma("sp", "lnp", G2, self.ln2_g[l:l + 1, :].partition_broadcast(128), r=[], w=["F2"])
        P.dma("sp", "lnp", B2, self.ln2_b[l:l + 1, :].partition_broadcast(128), r=[], w=["F2"])
        P.dma("sp", "lnp", GB, self.ple_gb[l:l + 1, :].partition_broadcast(128), r=[], w=["F2"])
        US = [self.FA[:, 0, 30 + k * 1024:30 + (k + 1) * 1024] for k in range(3)]
        VS = [self.FA[:, 0, 3102:4126], self.FA[:, 1, 0:1024], self.FA[:, 1, 1024:2048]]
        X1 = self.FA[:, 1, 2048:3072]; ACC = self.FA[:, 1, 3072:4096]
        PK = self.PK
        SV = PK[:, 0:256].rearrange("p (h k) -> p h k", h=16)
        SIf = PK[:, 256:512].rearrange("p (h k) -> p h k", h=16)
        CV = PK[:, 512:640].rearrange("p (h k) -> p h k", h=8)
        CA = PK[:, 640:768].rearrange("p (h k) -> p h k", h=8)
        CBf = PK[:, 768:896].rearrange("p (h k) -> p h k", h=8)
        I1 = PK[:, 896:1024]; I2 = PK[:, 1024:1152]
        GATE = PK[:, 1152:1280]; H = PK[:, 1280:1408]; W = PK[:, 1408:1536]
        SI = self.PT[1][:].bitcast(U32).rearrange("p (h k) -> p h k", h=16)
        CI = self.PT[2][:].bitcast(U32)[:, 0:128].rearrange("p (h k) -> p h k", h=8)
        CI2 = self.PT[2][:].bitcast(U32)[:, 128:256].rearrange("p (h k) -> p h k", h=8)
        EIDX = self.EIDX[:]
        iota = self.CF[:, CF_IOTA:CF_IOTA + 16]
        pu_flat = self.pu.rearrange("l n d -> (l n) d")
        pv_flat = self.pv.rearrange("l n d -> (l n) d")
        NEG = -1.0e30

        def top16(vals, wk, sv_out, si_out, rd, wkn):
            P.op("dve", lambda g_: g_.max(out=sv_out[:, 0:8], in_=vals), r=rd, w=["PK"])
            P.op("dve", lambda g_: g_.max_index(out=si_out[:, 0:8], in_max=sv_out[:, 0:8], in_values=vals),
                 r=rd + ["PK"], w=["PKI"])
            P.op("dve", lambda g_: g_.match_replace(out=wk, in_to_replace=sv_out[:, 0:8], in_values=vals,
                                                   imm_value=NEG), r=rd + ["PK"], w=[wkn])
            P.op("dve", lambda g_: g_.max(out=sv_out[:, 8:16], in_=wk), r=[wkn], w=["PK"])
            P.op("dve", lambda g_: g_.max_index(out=si_out[:, 8:16], in_max=sv_out[:, 8:16], in_values=wk),
                 r=[wkn, "PK"], w=["PKI"])

        for i in range(PEER_TILES):
            ts_ = slice(i * 128, (i + 1) * 128)
            for q4 in range(4):
                bk = self.psn(allb)
                for j in range(4):
                    hc = 4 * q4 + j
                    for c in range(8):
                        P.mm(self.PS[bk][:, j * 128:(j + 1) * 128], WQ[:, c, hc * 128:(hc + 1) * 128], self.XT[:, c, ts_],
                             start=(c == 0), stop=(c == 7), r=["B0", "XT"], w=[f"ps{bk}"])
                P.cp("act", QPT[:, 4 * q4:4 * q4 + 4, :], self.PS[bk][:].rearrange("p (j t) -> p j t", j=4),
                     r=[f"ps{bk}"], w=["KET"])
            for q4 in range(4):
                bk = self.psn(allb)
                for j in range(4):
                    hc = 4 * q4 + j
                    P.mm(self.PS[bk][:, j * 128:(j + 1) * 128], QPT[:, hc, :], KT[:, hc, :], start=True, stop=True,
                         r=["KET", "VT"], w=[f"ps{bk}"])
                sc = self.T5[q4 % 2]; scn = f"T5_{q4 % 2}"
                wk = self.T5[2 + q4 % 2]; wkn = f"T5_{2 + q4 % 2}"
                P.cp("act", sc[:], self.PS[bk][:], r=[f"ps{bk}"], w=[scn])
                for j in range(4):
                    hc = 4 * q4 + j
                    top16(sc[:, j * 128:(j + 1) * 128], wk[:, j * 128:(j + 1) * 128], SV[:, hc, :], SI[:, hc, :],
                          [scn], wkn)
            if PEER_STAGE < 1:
                continue
            for h in range(8):
                cd = self.T5[4]; cdn = "T5_4"
                cand = cd[:, 0:256]; cwk = cd[:, 256:512]
                P.tt("dve", cand.rearrange("p (a b) -> p a b", a=16), apx(SV[:, 2 * h, :], [[1, 16], [0, 16]]),
                     apx(SV[:, 2 * h + 1, :], [[0, 16], [1, 16]]), ALU.add, r=["PK"], w=[cdn])
                top16(cand, cwk, CV[:, h, :], CI[:, h, :], [cdn], cdn)
            if PEER_STAGE < 2:
                continue
            P.op("dve", lambda g_: g_.tensor_single_scalar(out=CI2[:], in_=CI[:], scalar=4, op=ALU.logical_shift_right),
                 r=["PKI"], w=["PKI2"])
            P.cp("dve", CA[:], CI2[:], r=["PKI2"], w=["PKA"])
            P.op("dve", lambda g_: g_.tensor_single_scalar(out=CI2[:], in_=CI[:], scalar=15, op=ALU.bitwise_and),
                 r=["PKI", "PKA"], w=["PKI2"])
            P.cp("dve", CBf[:], CI2[:], r=["PKI2"], w=["PKA"])
            P.cp("dve", SIf[:], SI[:], r=["PKI"], w=["PKA"])
            for h in range(8):
                for c, (src, dstI) in enumerate(((CA, I1), (CBf, I2))):
                    eq = self.T5[5][:, c * 256:(c + 1) * 256]; eqn = "T5_5"
                    eq3 = eq.rearrange("p (k a) -> p k a", k=16)
                    P.tt("dve", eq3, apx(src[:, h, :], [[1, 16], [0, 16]]), apx(iota, [[0, 16], [1, 16]]),
                         ALU.is_equal, r=["PKA", "CF"], w=[eqn])
                    P.tt("dve", eq3, eq3, apx(SIf[:, 2 * h + c, :], [[0, 16], [1, 16]]), ALU.mult, r=[eqn, "PKA"], w=[eqn])
                    P.op("dve", lambda g_, eq3=eq3, dstI=dstI, h=h: g_.tensor_reduce(
                        out=dstI[:, h * 16:(h + 1) * 16], in_=eq3, axis=AX.X, op=ALU.add), r=[eqn], w=["PKB"])
            P.ts("dve", I1, I1, 128.0, float(l * 16384), ALU.mult, ALU.add, r=["PKB"], w=["PKB"])
            P.tt("dve", I1, I1, I2, ALU.add, r=["PKB"], w=["PKB"])
            P.cp("dve", EIDX, I1, r=["PKB"], w=["EIDX"])
            if PEER_STAGE < 3:
                continue
            G3 = GATE.rearrange("p (h k) -> p h k", h=8)
            P.tt("dve", G3, CV, apx(CV[:, 0, 0:1], [[16, 8], [0, 16]]), ALU.subtract, r=["PK"], w=["GATE"])
            P.act(GATE, GATE, AF.Exp, r=["GATE"], w=["GATE"])
            zs = self.SM[:, 16:24]
            P.op("dve", lambda g_, G3=G3: g_.tensor_reduce(out=zs, in_=G3, axis=AX.X, op=ALU.add), r=["GATE"], w=["SM"])
            P.op("dve", lambda g_: g_.reciprocal(out=zs, in_=zs), r=["SM"], w=["SM"])
            P.tt("dve", G3, G3, apx(zs[:, 0:1], [[1, 8], [0, 16]]), ALU.mult, r=["GATE", "SM"], w=["GATE"])
            if PEER_STAGE < 4:
                continue
            P.dma("sp", "x1r", X1, self.x1d[ts_, :], r=["x1d"], w=["X1"])
            for k in range(128):
                sl = k % 3
                P.kb.add("pool", lambda g_, k=k, sl=sl: g_.indirect_dma_start(
                    out=US[sl], out_offset=None, in_=pu_flat,
                    in_offset=bass.IndirectOffsetOnAxis(ap=EIDX[:, k:k + 1], axis=0)),
                    self._b(["EIDX"]), self._b([f"US{sl}"]), dma=f"us{sl}")
                P.stt(US[sl], US[sl], 1.0, X1, ALU.mult, ALU.mult, r=[f"US{sl}", "X1"], w=[f"US{sl}", "H"],
                      accum_out=H[:, k:k + 1])
            if PEER_STAGE < 5:
                continue
            P.tt("dve", W, H, H, ALU.mult, r=["H"], w=["W"])
            P.ts("dve", W, W, 0.044715, 1.0, ALU.mult, ALU.add, r=["W"], w=["W"])
            P.tt("dve", W, W, H, ALU.mult, r=["W", "H"], w=["W"])
            P.act(W, W, AF.Sigmoid, r=["W"], w=["W"], scale=1.5957691216057308)
            P.tt("dve", W, W, H, ALU.mult, r=["W", "H"], w=["W"])
            P.tt("dve", W, W, GATE, ALU.mult, r=["W", "GATE"], w=["W"])
            for k in range(128):
                sl = k % 3
                P.kb.add("pool", lambda g_, k=k, sl=sl: g_.indirect_dma_start(
                    out=VS[sl], out_offset=None, in_=pv_flat,
                    in_offset=bass.IndirectOffsetOnAxis(ap=EIDX[:, k:k + 1], axis=0)),
                    self._b(["EIDX"]), self._b([f"VS{sl}"]), dma=f"vs{sl}")
                if k == 0:
                    P.ts("dve", ACC, VS[sl], W[:, 0:1], None, ALU.mult, r=[f"VS{sl}", "W"], w=["ACC"])
                else:
                    P.stt(ACC, VS[sl], W[:, k:k + 1], ACC, ALU.mult, ALU.add, r=[f"VS{sl}", "W", "ACC"], w=["ACC"])
            if PEER_STAGE < 6:
                continue
            P.stt(ACC, X1, ALPHA, ACC, ALU.mult, ALU.add, r=["X1", "ACC"], w=["ACC"])
            xb = self.XBT[0]
            P.cp("act", xb[:], ACC, r=["ACC"], w=["XBT0"])
            bk = self.psn(allb)
            psb = self.PS[bk][:].bitcast(BF16)
            for c in range(8):
                P.tr(psb[:, c * 128:(c + 1) * 128], xb[:, c * 128:(c + 1) * 128], self.identb, r=["XBT0", "CB"], w=[f"ps{bk}"])
            RTB = self.T5[5][:].bitcast(BF16)
            RT = RTB.rearrange("p (c t) -> p c t", c=8)
            P.cp("act", RTB, psb, r=[f"ps{bk}"], w=["T5_5"])
            pb16 = self.XBT[1]
            P.dma("pool", "pld", pb16[:, 0:256], self.p_in[l, ts_, :], r=[], w=["XBT1"])
            bk2 = self.psn(allb)
            psb2 = self.PS[bk2][:].bitcast(BF16)
            for c in range(2):
                P.tr(psb2[:, c * 128:(c + 1) * 128], pb16[:, c * 128:(c + 1) * 128], self.identb, r=["XBT1", "CB"], w=[f"ps{bk2}"])
            PTT = self.PT[0][:, 0:256].rearrange("p (c t) -> p c t", c=2)
            P.cp("act", self.PT[0][:, 0:256], psb2[:, 0:256], r=[f"ps{bk2}"], w=["PT0"])
            for half in range(2):
                hs = slice(half * 512, (half + 1) * 512)
                pg = self.psn(allb)
                for c in range(8):
                    P.mm(self.PS[pg][:], RT[:, c, :], GW2[:, c, hs], start=(c == 0), stop=(c == 7),
                         r=["T5_5", "F3"], w=[f"ps{pg}"])
                pp = self.psn(allb)
                for c in range(2):
                    P.mm(self.PS[pp][:], PTT[:, c, :], PW[:, c, hs], start=(c == 0), stop=(c == 1),
                         r=["PT0", "F2"], w=[f"ps{pp}"])
                t = self.T5[half]; tn = f"T5_{half}"
                P.tt("dve", t[:], self.PS[pg][:], GB[:, hs], ALU.add, r=[f"ps{pg}", "F2"], w=[tn])
                P.act(t[:], t[:], AF.Sigmoid, r=[tn], w=[tn])
                P.tt("dve", t[:], t[:], self.PS[pp][:], ALU.mult, r=[tn, f"ps{pp}"], w=[tn])
                P.tt("dve", ACC[:, hs], ACC[:, hs], t[:], ALU.add, r=[tn, "ACC"], w=["ACC"])
            self.layernorm_tile(ACC, "ACC", G2, B2, "F2")
            P.dma("sp", "xw", dst[ts_, :], ACC, r=["ACC"], w=[dstn])


def host_tables(inp):
    L = DEPTH
    chanp = np.zeros((L, 128, CP_N), np.float32)
    for j in range(2):
        chanp[:, :, CP_CB + j] = inp["conv_b"][:, j * 128:(j + 1) * 128]
        chanp[:, :, CP_LG + j] = inp["conv_ln_g"][:, j * 128:(j + 1) * 128]
        chanp[:, :, CP_LB + j] = inp["conv_ln_b"][:, j * 128:(j + 1) * 128]
        chanp[:, :, CP_CW + j * 31:CP_CW + (j + 1) * 31] = np.transpose(
            inp["conv_w"][:, :, j * 128:(j + 1) * 128], (0, 2, 1))
    for h in range(6):
        chanp[:, 0:32, CP_GB + h] = inp["gla_gate_b"][:, h * 32:(h + 1) * 32]
        chanp[:, 0:64, CP_NG + h] = inp["gla_norm_g"][:, h * 64:(h + 1) * 64]
        chanp[:, :, CP_FB + h] = inp["fox_forget_b"][:, h][:, None]
    keysT = np.ascontiguousarray(
        np.transpose(np.asarray(inp["peer_keys"]).reshape(L, 16, 128, 128), (0, 1, 3, 2)))
    return chanp, keysT


SHARED = ["w_in", "gla_gate_w", "w_out", "ln1_g", "ln1_b", "peer_wq", "peer_u", "peer_v",
          "ple_w", "ple_gw", "ple_gb", "ln2_g", "ln2_b"]


def make_in_maps(inp, cores):
    inp = {k: np.asarray(v) for k, v in inp.items()}
    chanp, keysT = host_tables(inp)
    cf, cb = host_consts()
    shared = {k: np.ascontiguousarray(inp[k], dtype=np.float32) for k in SHARED}
    shared.update(chanp=chanp, keysT=keysT, cf32=cf, cb16=cb)
    maps = []
    for c in cores:
        m = dict(shared)
        m["x"] = np.ascontiguousarray(inp["x"][c])
        m["p"] = np.ascontiguousarray(inp["p"][:, c])
        maps.append(m)
    return maps


_PROG = None


def kernel(**inputs):
    global _PROG
    if _PROG is None:
        _PROG = Program()
        _PROG.build()
    maps = make_in_maps(inputs, list(range(8)))
    res = run_bass_kernel_spmd(_PROG.nc, maps, core_ids=list(range(8)))
    return np.stack([np.asarray(r["y"]) for r in res.results], axis=0).astype(np.float32)
```

```python
import numpy as np
import ml_dtypes
from contextlib import ExitStack
import concourse.bass as bass
import concourse.mybir as mybir
from concourse.bass_utils import run_bass_kernel_spmd

F32 = mybir.dt.float32
BF16 = mybir.dt.bfloat16
U32 = mybir.dt.uint32
I32 = mybir.dt.int32
AF = mybir.ActivationFunctionType
ALU = mybir.AluOpType
AX = mybir.AxisListType

D = 1024
S = 4096
NT = 32
DEPTH = 4
IN_COLS = 2838
ALPHA = (2.0 * DEPTH) ** 0.25
EPS = 1e-5
C_AVAL, C_AGATE, C_BQ, C_BK, C_BV, C_BG, C_BLR, C_CQ, C_CK, C_CV, C_CF = (
    0, 256, 512, 704, 896, 1280, 1664, 1680, 2064, 2448, 2832)


class Op:
    __slots__ = ("eng", "fn", "deps", "stream", "needed", "val", "isdma")


class Buf:
    __slots__ = ("name", "w", "r")

    def __init__(self, name):
        self.name = name
        self.w = {}
        self.r = {}


class KB:
    def __init__(self, nc, es):
        self.nc = nc
        self.es = es
        self.ops = []
        self.eng = {"pe": nc.tensor, "act": nc.scalar, "dve": nc.vector,
                    "pool": nc.gpsimd, "sp": nc.sync}
        self.sems = {}
        self.nsem = 0

    def sem(self, name):
        if name not in self.sems:
            self.sems[name] = self.es.enter_context(self.nc.semaphore("s_" + name))
            self.nsem += 1
        return self.sems[name]

    def add(self, eng, fn, reads=(), writes=(), dma=None):
        op = Op()
        op.eng = eng
        op.fn = fn
        op.isdma = dma is not None
        op.stream = ("d_" + dma) if dma is not None else eng
        op.needed = False
        op.val = 0
        deps = set()
        for b in reads:
            deps.update(b.w.values())
        for b in writes:
            deps.update(b.w.values())
            deps.update(b.r.values())
        op.deps = [d for d in deps if not (eng == "pe" and d.stream == "pe")]
        for d in op.deps:
            d.needed = True
        for b in reads:
            b.r[op.stream] = op
        for b in writes:
            b.w = {op.stream: op}
            b.r = {}
        self.ops.append(op)
        return op

    def emit(self):
        counters = {}
        for op in self.ops:
            if op.needed or op.isdma:
                inc = 16 if op.isdma else 1
                counters[op.stream] = counters.get(op.stream, 0) + inc
                op.val = counters[op.stream]
        for st in counters:
            self.sem(st)
        seen = {e: {} for e in self.eng}
        n = 0
        for op in self.ops:
            e = self.eng[op.eng]
            need = {}
            for d in op.deps:
                if d.val > need.get(d.stream, 0):
                    need[d.stream] = d.val
            sn = seen[op.eng]
            for st, v in need.items():
                if sn.get(st, 0) < v:
                    e.wait_ge(self.sems[st], v)
                    sn[st] = v
                    n += 1
            if op.fn is not None:
                ins = op.fn(e)
                n += 1
                if op.needed or op.isdma:
                    ins.then_inc(self.sems[op.stream], 16 if op.isdma else 1)
        return n


def apx(base, dims):
    return bass.AP(base.tensor, base.offset, [list(base.ap[0])] + [list(d) for d in dims])


class Prog:
    def __init__(self, n_layers=DEPTH, dbg=None):
        self.n_layers = n_layers
        self.dbg = dbg or {}
        self.nc = bass.Bass("TRN2", target_bir_lowering=False)
        self.es = ExitStack()
        self.kb = KB(self.nc, self.es)
        self.bufs = {}

    def dram_in(self, name, shape, dt=F32):
        return self.nc.dram_tensor(name, list(shape), dt, kind="ExternalInput").ap()

    def dram_out(self, name, shape, dt=F32):
        return self.nc.dram_tensor(name, list(shape), dt, kind="ExternalOutput").ap()

    def dram_tmp(self, name, shape, dt=F32):
        return self.nc.dram_tensor(name, list(shape), dt, kind="Internal").ap()

    def sb(self, name, shape, dt=F32):
        return self.es.enter_context(self.nc.sbuf_tensor(name, list(shape), dt))

    def B(self, name):
        if name not in self.bufs:
            self.bufs[name] = Buf(name)
        return self.bufs[name]

    def _b(self, xs):
        return [self.B(x) if isinstance(x, str) else x for x in xs]

    def op(self, eng, fn, r=(), w=()):
        return self.kb.add(eng, fn, self._b(r), self._b(w))

    def dma(self, q, sem, out, in_, r=(), w=(), **kw):
        e = {"sp": "sp", "pool": "pool", "act": "act"}[q]
        return self.kb.add(e, lambda g: g.dma_start(out=out, in_=in_, **kw),
                           self._b(r), self._b(w), dma=sem)

    def mm(self, out, lhsT, rhs, start, stop, r=(), w=()):
        return self.op("pe", lambda g: g.matmul(out, lhsT, rhs, start=start, stop=stop), r, w)

    def tr(self, out, in_, ident, r=(), w=()):
        return self.op("pe", lambda g: g.transpose(out, in_, ident), r, w)

    def act(self, out, in_, func, r=(), w=(), **kw):
        return self.op("act", lambda g: g.activation(out=out, in_=in_, func=func, **kw), r, w)

    def tt(self, eng, out, in0, in1, op, r=(), w=()):
        return self.op(eng, lambda g: g.tensor_tensor(out=out, in0=in0, in1=in1, op=op), r, w)

    def ts(self, eng, out, in0, s1, s2, op0, op1=None, r=(), w=(), **kw):
        if op1 is None:
            return self.op(eng, lambda g: g.tensor_scalar(out=out, in0=in0, scalar1=s1, scalar2=None,
                                                          op0=op0, **kw), r, w)
        return self.op(eng, lambda g: g.tensor_scalar(out=out, in0=in0, scalar1=s1, scalar2=s2,
                                                      op0=op0, op1=op1, **kw), r, w)

    def stt(self, out, in0, scalar, in1, op0, op1, r=(), w=(), **kw):
        return self.op("dve", lambda g: g.scalar_tensor_tensor(out=out, in0=in0, scalar=scalar, in1=in1,
                                                               op0=op0, op1=op1, **kw), r, w)

    def cp(self, eng, out, in_, r=(), w=()):
        if eng == "act":
            return self.op("act", lambda g: g.copy(out=out, in_=in_), r, w)
        return self.op(eng, lambda g: g.tensor_copy(out=out, in_=in_), r, w)


CP_CB, CP_LG, CP_LB, CP_CW, CP_GB, CP_NG, CP_FB, CP_N = 0, 2, 4, 6, 68, 74, 80, 86
CF_IDENT, CF_ONES, CF_SCM, CF_IOTA, CF_E0, CF_E1, CF_M0, CF_M1, CF_N = 0, 128, 256, 768, 784, 785, 786, 787, 788
CB_IDENT, CB_CMASK, CB_GMASK, CB_ONES, CB_N = 0, 128, 256, 384, 512


def host_consts():
    cf = np.zeros((128, CF_N), np.float32)
    cf[:, CF_IDENT:CF_IDENT + 128] = np.eye(128, dtype=np.float32)
    cf[:, CF_ONES:CF_ONES + 128] = 1.0
    scm = np.ones((512,), np.float32)
    scm[::64] = 0.0
    cf[:, CF_SCM:CF_SCM + 512] = scm[None, :]
    cf[:, CF_IOTA:CF_IOTA + 16] = np.arange(16, dtype=np.float32)[None, :]
    cf[64, CF_E0] = 1.0
    cf[65, CF_E1] = 1.0
    cf[0:64, CF_M0] = 1.0
    cf[64:128, CF_M1] = 1.0
    cb = np.zeros((128, CB_N), np.float32)
    cb[:, CB_IDENT:CB_IDENT + 128] = np.eye(128)
    s = np.arange(128)[:, None]
    t = np.arange(128)[None, :]
    cb[:, CB_CMASK:CB_CMASK + 128] = (t >= s)
    cb[:, CB_GMASK:CB_GMASK + 128] = (t >= s) & ((t // 64) == (s // 64))
    cb[:, CB_ONES:CB_ONES + 128] = 1.0
    return cf, cb.astype(ml_dtypes.bfloat16)


GLA_STAGE = 99
PEER_STAGE = 99
PEER_TILES = NT


class Program(Prog):
    def build(self):
        nc = self.nc
        P = self
        L = DEPTH
        self.x_in = P.dram_in("x", [S, D])
        self.p_in = P.dram_in("p", [L, S, 256])
        self.w_in = P.dram_in("w_in", [L, D, IN_COLS])
        self.chanp_d = P.dram_in("chanp", [L, 128, CP_N])
        self.gatew_d = P.dram_in("gla_gate_w", [L, 16, 192])
        self.w_out = P.dram_in("w_out", [L, D, D])
        self.ln1_g = P.dram_in("ln1_g", [L, D]); self.ln1_b = P.dram_in("ln1_b", [L, D])
        self.wq = P.dram_in("peer_wq", [L, D, 2048])
        self.keysT = P.dram_in("keysT", [L, 16, 128, 128])
        self.pu = P.dram_in("peer_u", [L, 16384, D])
        self.pv = P.dram_in("peer_v", [L, 16384, D])
        self.ple_w = P.dram_in("ple_w", [L, 256, D])
        self.ple_gw = P.dram_in("ple_gw", [L, D, D])
        self.ple_gb = P.dram_in("ple_gb", [L, D])
        self.ln2_g = P.dram_in("ln2_g", [L, D]); self.ln2_b = P.dram_in("ln2_b", [L, D])
        self.cf_d = P.dram_in("cf32", [128, CF_N])
        self.cb_d = P.dram_in("cb16", [128, CB_N], BF16)
        self.y = P.dram_out("y", [S, D])
        self.xcur = P.dram_tmp("xcur", [S, D])
        self.x1d = P.dram_tmp("x1d", [S, D])
        self.mixT = P.dram_tmp("mixT", [D, S], BF16)
        self.dbg_out = {}
        for name, (shape, dt, attr, bufs) in self.dbg.items():
            self.dbg_out[name] = P.dram_out("dbg_" + name, shape, dt)
        self.XT = P.sb("XT", [128, 8, S], BF16)
        self.FA = P.sb("FA", [128, 4, 4128], F32)
        self.BA = P.sb("BA", [128, 4, S], BF16)
        self.WB = [P.sb("WB0", [128, 8, 256], BF16), P.sb("WB1", [128, 8, 256], BF16)]
        self.CF = P.sb("CF", [128, CF_N], F32)
        self.CB = P.sb("CB", [128, CB_N], BF16)
        self.CHP = P.sb("CHP", [128, CP_N], F32)
        self.XBT = [P.sb("XBT0", [128, 1024], BF16), P.sb("XBT1", [128, 1024], BF16)]
        self.T5 = [P.sb(f"T5_{i}", [128, 512], F32) for i in range(6)]
        self.VT = P.sb("VT", [128, NT, 64], BF16)
        self.KET = P.sb("KET", [128, 2, NT, 32], BF16)
        self.CT = P.sb("CT", [128, NT], F32)
        self.PT = [P.sb(f"PT{i}", [128, 512], BF16) for i in range(3)]
        self.GW = P.sb("GW", [16, 192], BF16)
        self.SM = P.sb("SM", [128, 64], F32)
        self.PK = P.sb("PK", [128, 1536], F32)
        self.EIDX = P.sb("EIDX", [128, 2, 128], U32)
        self.gs_rot = 0
        self.PS = [self.es.enter_context(nc.psum_tensor(f"ps{i}", [128, 512], F32)) for i in range(8)]
        self.ps_rot = 0
        self.identb = self.CB[:, CB_IDENT:CB_IDENT + 128]
        self.identf = self.CF[:, CF_IDENT:CF_IDENT + 128]
        self.onesf = self.CF[:, CF_ONES:CF_ONES + 128]
        self.onesb = self.CB[:, CB_ONES:CB_ONES + 128]
        P.dma("sp", "cst", self.CF[:], self.cf_d, w=["CF"])
        P.dma("sp", "cst", self.CB[:], self.cb_d, w=["CB"])
        P.op("dve", lambda g: g.memset(self.FA[:, 0, 0:30], 0.0), w=["F0"])

        for l in range(self.n_layers):
            self.layer(l)

        fin = []
        for name, (shape, dt, attr, bufs) in self.dbg.items():
            P.dma("sp", "dbg", self.dbg_out[name], getattr(self, attr), r=bufs, w=["dbg_" + name])
            fin.append("dbg_" + name)
        fin.append("y")
        P.op("sp", None, r=fin)
        n = self.kb.emit()
        return n

    def psn(self, banks):
        b = banks[self.ps_rot % len(banks)]
        self.ps_rot += 1
        return b

    def F(self, i, lo=0, hi=4096):
        return self.FA[:, i, lo:hi]

    def layer(self, l):
        P = self
        src = self.x_in if l == 0 else self.xcur
        P.dma("sp", "chp", self.CHP[:], self.chanp_d[l], r=[], w=["CHP"])
        P.dma("pool", "gw", self.GW[:], self.gatew_d[l], r=[], w=["GW"])
        self.phase_xT(src, "xsrc")
        self.unit_conv(l)
        self.lrt_done = False
        for h in range(6):
            self.unit_gla(l, h)
        for h in range(6):
            self.unit_fox(l, h)
        self.phase_out(l, src)
        self.phase_peer(l)

    def phase_xT(self, src, srcname):
        P = self
        for i in range(NT):
            s = i % 2
            xb = self.XBT[s]
            P.dma("pool", f"xbt{s}", xb[:], src[i * 128:(i + 1) * 128, :], r=[srcname], w=[f"XBT{s}"])
            self.transpose_to_XT(xb, f"XBT{s}", i)

    def transpose_to_XT(self, xb, xbname, i):
        P = self
        bank = self.psn([0, 1])
        psb = self.PS[bank][:].bitcast(BF16)
        for c in range(8):
            P.tr(psb[:, c * 128:(c + 1) * 128], xb[:, c * 128:(c + 1) * 128], self.identb,
                 r=[xbname, "CB"], w=[f"ps{bank}"])
        P.cp("act" if i % 2 else "dve", self.XT[:, :, i * 128:(i + 1) * 128],
             psb.rearrange("p (c t) -> p c t", c=8), r=[f"ps{bank}"], w=["XT"])

    def load_w(self, slot, l, cols):
        off = 0
        for (c0, n) in cols:
            kw = dict(allow_slow_non_contiguous=True) if n == 1 else {}
            self.dma("pool", f"wb{slot}", self.WB[slot][:, :, off:off + n],
                     self.w_in[l, :, c0:c0 + n].rearrange("(c p) n -> p c n", p=128), r=[], w=[f"WB{slot}"], **kw)
            off += n

    def proj_fm(self, bank, slot, woff, m, g, prow=0):
        for c in range(8):
            self.mm(self.PS[bank][prow:prow + m, :], self.WB[slot][:, c, woff:woff + m],
                    self.XT[:, c, g * 512:(g + 1) * 512], start=(c == 0), stop=(c == 7),
                    r=[f"WB{slot}", "XT"], w=[f"ps{bank}"])

    def proj_v_tm(self, slot, woff, g, banks):
        bank = self.psn(banks)
        for j in range(4):
            i = 4 * g + j
            for c in range(8):
                self.mm(self.PS[bank][:, j * 64:(j + 1) * 64], self.XT[:, c, i * 128:(i + 1) * 128],
                        self.WB[slot][:, c, woff:woff + 64], start=(c == 0), stop=(c == 7),
                        r=[f"WB{slot}", "XT"], w=[f"ps{bank}"])
        self.cp("act", self.VT[:, 4 * g:4 * g + 4, :],
                self.PS[bank][:, 0:256].rearrange("p (j e) -> p j e", j=4), r=[f"ps{bank}"], w=["VT"])

    def unit_conv(self, l):
        P = self
        UP = self.FA[:, 0, :]
        for j in range(2):
            slot = j
            self.load_w(slot, l, [(C_AVAL + j * 128, 128), (C_AGATE + j * 128, 128)])
            for g in range(8):
                a = self.psn([2, 3, 4, 5]); b = self.psn([2, 3, 4, 5])
                self.proj_fm(a, slot, 0, 128, g)
                self.proj_fm(b, slot, 128, 128, g)
                t = self.T5[g % 2]
                P.act(t[:], self.PS[b][:], AF.Sigmoid, r=[f"ps{b}"], w=[f"T5_{g % 2}"])
                P.tt("dve", UP[:, 30 + g * 512:30 + (g + 1) * 512], self.PS[a][:], t[:], ALU.mult,
                     r=[f"ps{a}", f"T5_{g % 2}"], w=["F0"])
            acc = self.F(1 + j)
            cw = self.CHP[:, CP_CW + j * 31:CP_CW + (j + 1) * 31]
            P.ts("dve", acc, UP[:, 0:4096], cw[:, 0:1], self.CHP[:, CP_CB + j:CP_CB + j + 1],
                 ALU.mult, ALU.add, r=["F0", "CHP"], w=[f"F{1 + j}"])
            for k in range(1, 31):
                P.stt(acc, UP[:, k:k + 4096], cw[:, k:k + 1], acc, ALU.mult, ALU.add,
                      r=["F0", "CHP", f"F{1 + j}"], w=[f"F{1 + j}"])
        for g in range(8):
            gs = slice(g * 512, (g + 1) * 512)
            pm = self.psn([2, 3, 4, 5]); pe = self.psn([2, 3, 4, 5])
            for j in range(2):
                P.mm(self.PS[pm][:], self.onesf, self.F(1 + j)[:, gs], start=(j == 0), stop=(j == 1),
                     r=["CF", f"F{1 + j}"], w=[f"ps{pm}"])
            for j in range(2):
                sq = self.T5[j]
                P.act(sq[:], self.F(1 + j)[:, gs], AF.Square, r=[f"F{1 + j}"], w=[f"T5_{j}"])
                P.mm(self.PS[pe][:], self.onesf, sq[:], start=(j == 0), stop=(j == 1),
                     r=["CF", f"T5_{j}"], w=[f"ps{pe}"])
            mean = self.T5[2]; msq = self.T5[3]; var = self.T5[4]
            P.act(mean[:], self.PS[pm][:], AF.Copy, r=[f"ps{pm}"], w=["T5_2"], scale=1.0 / 256)
            P.act(msq[:], mean[:], AF.Square, r=["T5_2"], w=["T5_3"])
            P.stt(var[:], self.PS[pe][:], 1.0 / 256, msq[:], ALU.mult, ALU.subtract, r=[f"ps{pe}", "T5_3"], w=["T5_4"])
            P.ts("dve", var[:], var[:], 0.0, EPS, ALU.max, ALU.add, r=["T5_4"], w=["T5_4"])
            P.act(var[:], var[:], AF.Sqrt, r=["T5_4"], w=["T5_4"])
            P.op("dve", lambda g_, v=var: g_.reciprocal(out=v[:], in_=v[:]), r=["T5_4"], w=["T5_4"])
            for j in range(2):
                t = self.T5[j]
                P.tt("dve", t[:], self.F(1 + j)[:, gs], mean[:], ALU.subtract, r=[f"F{1 + j}", "T5_2"], w=[f"T5_{j}"])
                P.tt("dve", t[:], t[:], var[:], ALU.mult, r=[f"T5_{j}", "T5_4"], w=[f"T5_{j}"])
                P.act(self.BA[:, j, gs], t[:], AF.Silu, r=[f"T5_{j}", "CHP"], w=[f"B{j}"],
                      scale=self.CHP[:, CP_LG + j:CP_LG + j + 1], bias=self.CHP[:, CP_LB + j:CP_LB + j + 1])
        for j in range(2):
            P.dma("sp", f"mixw{j}", self.mixT[j * 128:(j + 1) * 128, :], self.BA[:, j, :], r=[f"B{j}"], w=["mixT"])

    def gla_end(self, h):
        self.dma("sp", f"mixw{h % 2}", self.mixT[256 + h * 64:320 + h * 64, :], self.BA[0:64, 2, :], r=["B2"], w=["mixT"])

    def unit_gla(self, l, h):
        P = self
        slot = h % 2
        wn = f"WB{slot}"
        banks = [0, 1, 2, 3, 4, 5]
        QE = self.BA[:, 0, :]; KE = self.BA[:, 1, :]; AT = self.BA[:, 2, :]; SB = self.BA[:, 3, :]
        GZ = self.FA[:, 0, 30:4126]; EC = self.F(1); EN = self.F(2); ST = self.F(3)
        D1 = GZ; D0 = EN
        rq = slice(0, 32)
        self.load_w(slot, l, [(C_BQ + h * 32, 32), (C_BK + h * 32, 32), (C_BV + h * 64, 64), (C_BG + h * 64, 64),
                              (C_BLR, 16)])
        for g in range(8):
            gs = slice(g * 512, (g + 1) * 512)
            a0 = self.psn(banks)
            self.proj_fm(a0, slot, 192, 16, g)
            lrt = self.PT[g % 2]; lrn = f"PT{g % 2}"
            P.cp("act", lrt[0:16, :], self.PS[a0][0:16, :], r=[f"ps{a0}"], w=[lrn])
            a = self.psn(banks)
            P.mm(self.PS[a][rq, :], self.GW[0:16, h * 32:(h + 1) * 32], lrt[0:16, :], start=True, stop=True,
                 r=["GW", lrn], w=[f"ps{a}"])
            P.ts("dve", GZ[rq, gs], self.PS[a][rq, :], self.CHP[rq, CP_GB + h:CP_GB + h + 1], None, ALU.add,
                 r=[f"ps{a}", "CHP"], w=["F0"])
        P.act(GZ[rq, :], GZ[rq, :], AF.Exp, r=["F0"], w=["F0"], scale=-1.0)
        P.ts("dve", GZ[rq, :], GZ[rq, :], 1.0, None, ALU.add, r=["F0"], w=["F0"])
        P.act(GZ[rq, :], GZ[rq, :], AF.Ln, r=["F0"], w=["F0"])
        scm = self.CF[rq, CF_SCM:CF_SCM + 512]
        for g in range(8):
            gs = slice(g * 512, (g + 1) * 512)
            P.op("dve", lambda g_, gs=gs: g_.tensor_tensor_scan(
                out=ST[rq, gs], data0=scm, data1=GZ[rq, gs], initial=0.0, op0=ALU.mult, op1=ALU.subtract),
                r=["F0", "CF"], w=["F3"])
        P.act(EC[rq, :], ST[rq, :], AF.Exp, r=["F3"], w=["F1"], scale=1.0 / 16)
        P.act(EN[rq, :], ST[rq, :], AF.Exp, r=["F3"], w=["F2"], scale=-1.0 / 16)
        if GLA_STAGE < 1:
            return
        for g in range(8):
            gs = slice(g * 512, (g + 1) * 512)
            a = self.psn(banks)
            self.proj_fm(a, slot, 0, 32, g)
            P.stt(QE[rq, gs], self.PS[a][rq, :], 32.0 ** -0.5, EC[rq, gs], ALU.mult, ALU.mult,
                  r=[f"ps{a}", "F1"], w=["B0"])
            b = self.psn(banks)
            self.proj_fm(b, slot, 32, 32, g)
            P.tt("dve", KE[rq, gs], self.PS[b][rq, :], EN[rq, gs], ALU.mult, r=[f"ps{b}", "F2"], w=["B1"])
            self.proj_v_tm(slot, 64, g, banks)
        if GLA_STAGE < 2:
            return
        pk = self.psn(banks)
        pkb = self.PS[pk][:].bitcast(BF16)
        for i in range(NT):
            P.tr(pkb[:, i * 32:(i + 1) * 32], KE[rq, i * 128:(i + 1) * 128], self.identb[0:32, 0:32],
                 r=["B1", "CB"], w=[f"ps{pk}"])
        for m in range(2):
            P.ts("dve", self.KET[:, m, :, :], pkb.rearrange("p (i d) -> p i d", i=NT),
                 self.CF[:, CF_M0 + m:CF_M0 + m + 1], None, ALU.mult, r=[f"ps{pk}", "CF"], w=["KET"])
        if GLA_STAGE < 3:
            return
        ATv = AT.rearrange("p (i t) -> p i t", i=NT)
        gmask4 = apx(self.CB[:, CB_GMASK:CB_GMASK + 128], [[0, 4], [1, 128]])
        for i4 in range(8):
            a = self.psn(banks)
            for j in range(4):
                i = 4 * i4 + j
                P.mm(self.PS[a][:, j * 128:(j + 1) * 128], KE[rq, i * 128:(i + 1) * 128], QE[rq, i * 128:(i + 1) * 128],
                     start=True, stop=True, r=["B0", "B1"], w=[f"ps{a}"])
            for j in range(4):
                if GLA_STAGE == 3.5:
                    break
                P.tt("dve", ATv[:, 4 * i4 + j, :], self.PS[a][:, j * 128:(j + 1) * 128],
                     self.CB[:, CB_GMASK:CB_GMASK + 128], ALU.mult, r=[f"ps{a}", "CB"], w=["B2"])
        if GLA_STAGE < 4:
            return self.gla_end(h)
        for c8 in range(8):
            a = self.psn(banks)
            for cc in range(8):
                c = c8 * 8 + cc
                i = c // 2; r0 = (c % 2) * 64
                P.mm(self.PS[a][rq, cc * 64:(cc + 1) * 64], self.KET[:, c % 2, i, :], self.VT[:, i, :],
                     start=True, stop=True, r=["KET", "VT"], w=[f"ps{a}"])
            ac = apx(EC[rq, 64 * (c8 * 8) + 63:64 * (c8 * 8) + 64], [[64, 8], [0, 64]])
            out = apx(D1[rq, c8 * 8:c8 * 8 + 1], [[1, 8], [64, 64]])
            P.tt("dve", out, self.PS[a][rq, :].rearrange("p (c e) -> p c e", c=8), ac, ALU.mult,
                 r=[f"ps{a}", "F1"], w=["F0"])
        if GLA_STAGE < 5:
            return
        P.cp("dve", D0[rq, :].rearrange("p (e c) -> p e c", e=64), apx(EC[rq, 63:64], [[0, 64], [64, 64]]),
             r=["F1"], w=["F2"])
        P.op("dve", lambda g_: g_.memset(apx(D0[rq, 0:1], [[64, 64]]), 0.0), w=["F2"])
        P.op("dve", lambda g_: g_.tensor_tensor_scan(out=ST[rq, :], data0=D0[rq, :], data1=D1[rq, :], initial=0.0,
                                                    op0=ALU.mult, op1=ALU.add), r=["F0", "F2"], w=["F3"])
        if GLA_STAGE < 6:
            return
        P.cp("act", SB[rq, :].rearrange("p (c e) -> p c e", c=64), apx(ST[rq, 0:1], [[1, 64], [64, 64]]),
             r=["F3"], w=["B3"])
        if GLA_STAGE < 7:
            return
        r0m = 256 + h * 64
        BO = KE
        for g in range(8):
            gs = slice(g * 512, (g + 1) * 512)
            po = self.psn(banks)
            for cc in range(8):
                c = g * 8 + cc
                i = c // 2; r0 = (c % 2) * 64
                cs = slice(cc * 64, (cc + 1) * 64)
                P.mm(self.PS[po][0:64, cs], self.VT[:, i, :], ATv[:, i, r0:r0 + 64],
                     start=True, stop=(c == 0), r=["VT", "B2"], w=[f"ps{po}"])
                if c > 0:
                    P.mm(self.PS[po][0:64, cs], SB[rq, (c - 1) * 64:c * 64], QE[rq, c * 64:(c + 1) * 64],
                         start=False, stop=True, r=["B3", "B0"], w=[f"ps{po}"])
            ot = self.T5[0]; sq = self.T5[1]; rst = self.T5[2]; sg = self.T5[3]
            P.act(ot[0:64, :], self.PS[po][0:64, :], AF.Copy, r=[f"ps{po}"], w=["T5_0"])
            P.act(sq[0:64, :], self.PS[po][0:64, :], AF.Square, r=[f"ps{po}"], w=["T5_1"])
            pm = self.psn(banks)
            P.mm(self.PS[pm][0:64, :], self.onesf[0:64, 0:64], sq[0:64, :], start=True, stop=True,
                 r=["CF", "T5_1"], w=[f"ps{pm}"])
            P.ts("dve", rst[0:64, :], self.PS[pm][0:64, :], 1.0 / 64, EPS, ALU.mult, ALU.add, r=[f"ps{pm}"], w=["T5_2"])
            P.act(rst[0:64, :], rst[0:64, :], AF.Sqrt, r=["T5_2"], w=["T5_2"])
            P.op("dve", lambda g_, rst=rst: g_.reciprocal(out=rst[0:64, :], in_=rst[0:64, :]), r=["T5_2"], w=["T5_2"])
            pg = self.psn(banks)
            self.proj_fm(pg, slot, 128, 64, g)
            P.act(sg[0:64, :], self.PS[pg][0:64, :], AF.Silu, r=[f"ps{pg}"], w=["T5_3"])
            P.tt("dve", ot[0:64, :], ot[0:64, :], rst[0:64, :], ALU.mult, r=["T5_0", "T5_2"], w=["T5_0"])
            P.stt(BO[0:64, gs], ot[0:64, :], self.CHP[0:64, CP_NG + h:CP_NG + h + 1],
                  sg[0:64, :], ALU.mult, ALU.mult, r=["T5_0", "T5_3", "CHP"], w=["B1"])
        P.dma("sp", f"mixw{h % 2}", self.mixT[r0m:r0m + 64, :], BO[0:64, :], r=["B1"], w=["mixT"])

    def unit_fox(self, l, h):
        P = self
        slot = h % 2
        WBs = self.WB[slot]
        wn = f"WB{slot}"
        self.load_w(slot, l, [(C_CQ + h * 64, 64), (C_CF + h, 1), (C_CF + h, 1),
                              (C_CK + h * 64, 64), (C_CV + h * 64, 64)])
        QA = self.BA[:, 0, :]; KA = self.BA[:, 1, :]; CO = self.BA[:, 2, :]; TB = self.BA[:, 3, :]
        FZ = self.F(1); CC = self.F(2); HF = self.F(3)
        r2 = slice(64, 66)
        fb = self.CHP[64:66, CP_FB + h:CP_FB + h + 1]
        banks = [0, 1, 2, 3, 4, 5]
        for g in range(8):
            gs = slice(g * 512, (g + 1) * 512)
            a = self.psn(banks)
            self.proj_fm(a, slot, 0, 66, g)
            P.act(QA[0:64, gs], self.PS[a][0:64, :], AF.Copy, r=[f"ps{a}"], w=["B0"], scale=0.125)
            P.ts("dve", FZ[r2, gs], self.PS[a][r2, :], fb, None, ALU.add, r=[f"ps{a}", "CHP"], w=["F1"])
            b = self.psn(banks)
            self.proj_fm(b, slot, 66, 64, g)
            P.cp("dve", KA[0:64, gs], self.PS[b][0:64, :], r=[f"ps{b}"], w=["B1"])
            self.proj_v_tm(slot, 130, g, banks)
        P.act(FZ[r2, :], FZ[r2, :], AF.Exp, r=["F1"], w=["F1"], scale=-1.0)
        P.ts("dve", FZ[r2, :], FZ[r2, :], 1.0, None, ALU.add, r=["F1"], w=["F1"])
        P.act(FZ[r2, :], FZ[r2, :], AF.Ln, r=["F1"], w=["F1"])
        ones512 = self.CF[r2, CF_ONES:CF_ONES + 128]
        for pc in range(32):
            sl = slice(pc * 128, (pc + 1) * 128)
            init = 0.0 if pc == 0 else CC[r2, pc * 128 - 1:pc * 128]
            P.op("dve", lambda g_, sl=sl, init=init: g_.tensor_tensor_scan(
                out=CC[r2, sl], data0=ones512, data1=FZ[r2, sl], initial=init,
                op0=ALU.mult, op1=ALU.subtract), r=["F1", "F2", "CF"], w=["F2"])
        P.cp("dve", TB[r2, :], CC[r2, :], r=["F2"], w=["B3"])
        P.cp("dve", HF[r2, :], TB[r2, :], r=["B3"], w=["F3"])
        P.tt("dve", FZ[r2, :], CC[r2, :], HF[r2, :], ALU.subtract, r=["F2", "F3"], w=["F1"])
        P.ts("dve", HF[r2, :], HF[r2, :], self.CF[r2, CF_E0:CF_E0 + 1], None, ALU.mult, r=["F3", "CF"], w=["F3"])
        P.stt(QA[r2, :], FZ[r2, :], self.CF[r2, CF_E1:CF_E1 + 1], HF[r2, :], ALU.mult, ALU.add,
              r=["F1", "F3", "CF"], w=["B0"])
        P.op("dve", lambda g_: g_.memset(KA[r2, :], 1.0), w=["B1"])
        pb = self.psn(banks)
        for i in range(NT):
            P.tr(self.PS[pb][:, i:i + 1], CC[64:65, i * 128:(i + 1) * 128], self.identf[64:65, 64:65],
                 r=["F2", "CF"], w=[f"ps{pb}"])
        P.act(self.CT[:], self.PS[pb][:, 0:NT], AF.Copy, r=[f"ps{pb}"], w=["CT"], scale=-1.0)
        pO, pS = 6, 7
        iters = [(qg, kb) for qg in range(8) for kb in range(4 * (qg + 1))]
        psts = {}

        def stageA(n):
            qg, kb = iters[n]
            gs0 = qg * 512
            col0 = max(0, (kb - 4 * qg) * 128)
            pst = self.psn(banks)
            psts[n] = pst
            P.mm(self.PS[pst][:, col0:512], KA[0:66, kb * 128:(kb + 1) * 128], QA[0:66, gs0 + col0:gs0 + 512],
                 start=True, stop=True, r=["B0", "B1"], w=[f"ps{pst}"])

        LOOK = 2
        for n in range(min(LOOK, len(iters))):
            stageA(n)
        for n, (qg, kb) in enumerate(iters):
            if n + LOOK < len(iters):
                stageA(n + LOOK)
            nkb = 4 * (qg + 1)
            gs0 = qg * 512
            col0 = max(0, (kb - 4 * qg) * 128)
            pst = psts.pop(n)
            pt = self.PT[n % 3]; ptn = f"PT{n % 3}"
            P.act(pt[:, col0:512], self.PS[pst][:, col0:512], AF.Exp, r=[f"ps{pst}", "CT"], w=[ptn],
                  bias=self.CT[:, kb:kb + 1], scale=1.0)
            if kb >= 4 * qg:
                P.tt("pool", pt[:, col0:col0 + 128], pt[:, col0:col0 + 128],
                     self.CB[:, CB_CMASK:CB_CMASK + 128], ALU.mult, r=[ptn, "CB"], w=[ptn])
            P.mm(self.PS[pO][0:64, col0:512], self.VT[:, kb, :], pt[:, col0:512],
                 start=(kb == 0), stop=(kb == nkb - 1), r=["VT", ptn], w=[f"ps{pO}"])
            P.mm(self.PS[pS][0:64, col0:512], self.onesb[:, 0:64], pt[:, col0:512],
                 start=(kb == 0), stop=(kb == nkb - 1), r=["CB", ptn], w=[f"ps{pS}"])
            if kb == nkb - 1:
                rs = self.T5[5]
                P.op("dve", lambda g_, rs=rs: g_.reciprocal(out=rs[0:64, :], in_=self.PS[pS][0:64, :]),
                     r=[f"ps{pS}"], w=["T5_5"])
                P.tt("dve", CO[0:64, gs0:gs0 + 512], self.PS[pO][0:64, :], rs[0:64, :], ALU.mult,
                     r=[f"ps{pO}", "T5_5"], w=["B2"])
        r0 = 640 + h * 64
        P.dma("sp", f"mixw{h % 2}", self.mixT[r0:r0 + 64, :], CO[0:64, :], r=["B2"], w=["mixT"])

    def phase_out(self, l, src):
        P = self
        WO = self.BA[:, 0:2, :].rearrange("p a (c n) -> p (a c) n", n=1024)
        for c in range(8):
            P.dma("pool", "wo", WO[:, c, :], self.w_out[l, c * 128:(c + 1) * 128, :], r=[], w=["B0", "B1"])
        G1 = self.FA[:, 0, 30:1054]; B1 = self.FA[:, 0, 1054:2078]
        P.dma("sp", "lnp", G1, self.ln1_g[l:l + 1, :].partition_broadcast(128), r=[], w=["F0"])
        P.dma("sp", "lnp", B1, self.ln1_b[l:l + 1, :].partition_broadcast(128), r=[], w=["F0"])
        for i in range(NT):
            g = i // 4
            ms = g % 2
            MX = self.BA[:, 2 + ms, :].rearrange("p (c n) -> p c n", c=8)
            if i % 4 == 0:
                P.dma("sp", f"mx{ms}", MX, self.mixT[:, g * 512:(g + 1) * 512].rearrange("(c p) n -> p c n", p=128),
                      r=["mixT"], w=[f"B{2 + ms}"])
            s2 = i % 2
            XR = self.FA[:, 1, s2 * 1024:(s2 + 1) * 1024]
            R = self.FA[:, 1, 2048 + s2 * 1024:2048 + (s2 + 1) * 1024]
            xrn = f"XR{s2}"; rn = f"R{s2}"
            P.dma("sp", f"xr{s2}", XR, src[i * 128:(i + 1) * 128, :], r=["xsrc"], w=[xrn])
            for half in range(2):
                pb = self.psn([2, 3, 4, 5])
                for c in range(8):
                    P.mm(self.PS[pb][:], MX[:, c, (i % 4) * 128:(i % 4 + 1) * 128], WO[:, c, half * 512:(half + 1) * 512],
                         start=(c == 0), stop=(c == 7), r=[f"B{2 + ms}", "B0", "B1"], w=[f"ps{pb}"])
                P.stt(R[:, half * 512:(half + 1) * 512], XR[:, half * 512:(half + 1) * 512], ALPHA, self.PS[pb][:],
                      ALU.mult, ALU.add, r=[xrn, f"ps{pb}"], w=[rn])
            self.layernorm_tile(R, rn, G1, B1, "F0")
            P.dma("sp", f"x1w{s2}", self.x1d[i * 128:(i + 1) * 128, :], R, r=[rn], w=["x1d"])
            xb = self.XBT[s2]
            P.cp("act", xb[:], R, r=[rn], w=[f"XBT{s2}"])
            self.transpose_to_XT(xb, f"XBT{s2}", i)

    def layernorm_tile(self, R, rn, G, Bb, gbn):
        P = self
        st = self.SM[:, 0:12]; mv = self.SM[:, 12:14]; rs = self.SM[:, 14:15]
        for half in range(2):
            P.op("dve", lambda g_, half=half: g_.bn_stats(out=st[:, half * 6:(half + 1) * 6],
                                                        in_=R[:, half * 512:(half + 1) * 512]), r=[rn], w=["SM"])
        P.op("dve", lambda g_: g_.bn_aggr(out=mv, in_=st), r=["SM"], w=["SM"])
        P.ts("dve", rs, mv[:, 1:2], EPS, None, ALU.add, r=["SM"], w=["SM"])
        P.act(rs, rs, AF.Sqrt, r=["SM"], w=["SM"])
        P.op("dve", lambda g_: g_.reciprocal(out=rs, in_=rs), r=["SM"], w=["SM"])
        P.ts("dve", R, R, mv[:, 0:1], rs, ALU.subtract, ALU.mult, r=[rn, "SM"], w=[rn])
        P.tt("pool", R, R, G, ALU.mult, r=[rn, gbn], w=[rn])
        P.tt("dve", R, R, Bb, ALU.add, r=[rn, gbn], w=[rn])

    def phase_peer(self, l):
        P = self
        dst = self.y if (l == DEPTH - 1) else self.xcur
        dstn = "y" if (l == DEPTH - 1) else "xsrc"
        allb = [0, 1, 2, 3, 4, 5, 6, 7]
        WQ = self.BA[:, 0:4, :].rearrange("p a (c n) -> p (a c) n", n=2048)
        for c in range(8):
            P.dma("pool", "wo", WQ[:, c, :], self.wq[l, c * 128:(c + 1) * 128, :], r=[], w=["B0", "B1", "B2", "B3"])
        KT = self.VT[:].rearrange("p a b -> p (a b)").rearrange("p (h n) -> p h n", h=16)
        P.dma("pool", "kt", KT, self.keysT[l].rearrange("h d n -> d h n"), r=[], w=["VT"])
        QPT = self.KET[:].rearrange("p a b c -> p (a b c)").rearrange("p (h t) -> p h t", h=16)
        GW2 = self.FA[:, 3, 0:4096].bitcast(BF16).rearrange("p (c n) -> p c n", c=8)
        for c in range(8):
            P.dma("pool", "gw2", GW2[:, c, :], self.ple_gw[l, c * 128:(c + 1) * 128, :], r=[], w=["F3"])
        PW = self.FA[:, 2, 0:1024].bitcast(BF16).rearrange("p (c n) -> p c n", c=2)
        for c in range(2):
            P.dma("pool", "gw2", PW[:, c, :], self.ple_w[l, c * 128:(c + 1) * 128, :], r=[], w=["F2"])
        G2 = self.FA[:, 2, 1024:2048]; B2 = self.FA[:, 2, 2048:3072]; GB = self.FA[:, 2, 3072:4096]
        P.dma("sp", "lnp", G2, self.ln2_g[l:l + 1, :].partition_broadcast(128), r=[], w=["F2"])
        P.dma("sp", "lnp", B2, self.ln2_b[l:l + 1, :].partition_broadcast(128), r=[], w=["F2"])
        P.dma("sp", "lnp", GB, self.ple_gb[l:l + 1, :].partition_broadcast(128), r=[], w=["F2"])
        GS = [self.FA[:, 0, 30 + k * 1024:30 + (k + 1) * 1024] for k in range(4)] + \
             [self.FA[:, 1, 0:1024], self.FA[:, 1, 1024:2048]]
        NGS = len(GS)
        X1 = self.FA[:, 1, 2048:3072]; ACC = self.FA[:, 1, 3072:4096]
        PK = self.PK
        SV = PK[:, 0:256].rearrange("p (h k) -> p h k", h=16)
        SIf = PK[:, 256:512].rearrange("p (h k) -> p h k", h=16)
        CV = PK[:, 512:640].rearrange("p (h k) -> p h k", h=8)
        CA = PK[:, 640:768].rearrange("p (h k) -> p h k", h=8)
        CBf = PK[:, 768:896].rearrange("p (h k) -> p h k", h=8)
        I1 = PK[:, 896:1024]; I2 = PK[:, 1024:1152]
        GATE = PK[:, 1152:1280]; H = PK[:, 1280:1408]; W = PK[:, 1408:1536]
        SI = self.PT[1][:].bitcast(U32).rearrange("p (h k) -> p h k", h=16)
        CI = self.PT[2][:].bitcast(U32)[:, 0:128].rearrange("p (h k) -> p h k", h=8)
        CI2 = self.PT[2][:].bitcast(U32)[:, 128:256].rearrange("p (h k) -> p h k", h=8)
        iota = self.CF[:, CF_IOTA:CF_IOTA + 16]
        pu_flat = self.pu.rearrange("l n d -> (l n) d")
        pv_flat = self.pv.rearrange("l n d -> (l n) d")
        NEG = -1.0e30

        def top16(vals, wk, sv_out, si_out, rd, wkn):
            P.op("dve", lambda g_: g_.max(out=sv_out[:, 0:8], in_=vals), r=rd, w=["PK"])
            yield
            P.op("dve", lambda g_: g_.max_index(out=si_out[:, 0:8], in_max=sv_out[:, 0:8], in_values=vals),
                 r=rd + ["PK"], w=["PKI"])
            yield
            P.op("dve", lambda g_: g_.match_replace(out=wk, in_to_replace=sv_out[:, 0:8], in_values=vals,
                                                   imm_value=NEG), r=rd + ["PK"], w=[wkn])
            yield
            P.op("dve", lambda g_: g_.max(out=sv_out[:, 8:16], in_=wk), r=[wkn], w=["PK"])
            yield
            P.op("dve", lambda g_: g_.max_index(out=si_out[:, 8:16], in_max=sv_out[:, 8:16], in_values=wk),
                 r=[wkn, "PK"], w=["PKI"])
            yield

        def topk_gen(i, eb):
            ts_ = slice(i * 128, (i + 1) * 128)
            EIDX = self.EIDX[:, eb, :]
            en = f"EIDX{eb}"
            for q4 in range(4):
                bk = self.psn(allb)
                for j in range(4):
                    hc = 4 * q4 + j
                    for c in range(8):
                        P.mm(self.PS[bk][:, j * 128:(j + 1) * 128], WQ[:, c, hc * 128:(hc + 1) * 128], self.XT[:, c, ts_],
                             start=(c == 0), stop=(c == 7), r=["B0", "XT"], w=[f"ps{bk}"])
                P.cp("act", QPT[:, 4 * q4:4 * q4 + 4, :], self.PS[bk][:].rearrange("p (j t) -> p j t", j=4),
                     r=[f"ps{bk}"], w=["KET"])
            for q4 in range(4):
                bk = self.psn(allb)
                for j in range(4):
                    hc = 4 * q4 + j
                    P.mm(self.PS[bk][:, j * 128:(j + 1) * 128], QPT[:, hc, :], KT[:, hc, :], start=True, stop=True,
                         r=["KET", "VT"], w=[f"ps{bk}"])
                sc = self.T5[q4 % 2]; scn = f"T5_{q4 % 2}"
                wk = self.T5[2 + q4 % 2]; wkn = f"T5_{2 + q4 % 2}"
                P.cp("act", sc[:], self.PS[bk][:], r=[f"ps{bk}"], w=[scn])
                for j in range(4):
                    hc = 4 * q4 + j
                    yield from top16(sc[:, j * 128:(j + 1) * 128], wk[:, j * 128:(j + 1) * 128], SV[:, hc, :],
                                     SI[:, hc, :], [scn], wkn)
            for h in range(8):
                cd = self.T5[4]; cdn = "T5_4"
                cand = cd[:, 0:256]; cwk = cd[:, 256:512]
                P.tt("dve", cand.rearrange("p (a b) -> p a b", a=16), apx(SV[:, 2 * h, :], [[1, 16], [0, 16]]),
                     apx(SV[:, 2 * h + 1, :], [[0, 16], [1, 16]]), ALU.add, r=["PK"], w=[cdn])
                yield
                yield from top16(cand, cwk, CV[:, h, :], CI[:, h, :], [cdn], cdn)
            P.op("dve", lambda g_: g_.tensor_single_scalar(out=CI2[:], in_=CI[:], scalar=4, op=ALU.logical_shift_right),
                 r=["PKI"], w=["PKI2"])
            yield
            P.cp("dve", CA[:], CI2[:], r=["PKI2"], w=["PKA"])
            yield
            P.op("dve", lambda g_: g_.tensor_single_scalar(out=CI2[:], in_=CI[:], scalar=15, op=ALU.bitwise_and),
                 r=["PKI", "PKA"], w=["PKI2"])
            yield
            P.cp("dve", CBf[:], CI2[:], r=["PKI2"], w=["PKA"])
            yield
            P.cp("dve", SIf[:], SI[:], r=["PKI"], w=["PKA"])
            yield
            for h in range(8):
                for c, (src, dstI) in enumerate(((CA, I1), (CBf, I2))):
                    eq = self.T5[5][:, c * 256:(c + 1) * 256]; eqn = "T5_5"
                    eq3 = eq.rearrange("p (k a) -> p k a", k=16)
                    P.tt("dve", eq3, apx(src[:, h, :], [[1, 16], [0, 16]]), apx(iota, [[0, 16], [1, 16]]),
                         ALU.is_equal, r=["PKA", "CF"], w=[eqn])
                    yield
                    P.tt("dve", eq3, eq3, apx(SIf[:, 2 * h + c, :], [[0, 16], [1, 16]]), ALU.mult, r=[eqn, "PKA"], w=[eqn])
                    yield
                    P.op("dve", lambda g_, eq3=eq3, dstI=dstI, h=h: g_.tensor_reduce(
                        out=dstI[:, h * 16:(h + 1) * 16], in_=eq3, axis=AX.X, op=ALU.add), r=[eqn], w=["PKB"])
                    yield
            P.ts("dve", I1, I1, 128.0, float(l * 16384), ALU.mult, ALU.add, r=["PKB"], w=["PKB"])
            yield
            P.tt("dve", I1, I1, I2, ALU.add, r=["PKB"], w=["PKB"])
            yield
            P.cp("dve", EIDX, I1, r=["PKB"], w=[en])
            yield
            G3 = GATE.rearrange("p (h k) -> p h k", h=8)
            P.tt("dve", G3, CV, apx(CV[:, 0, 0:1], [[16, 8], [0, 16]]), ALU.subtract, r=["PK"], w=["GATE"])
            yield
            P.act(GATE, GATE, AF.Exp, r=["GATE"], w=["GATE"])
            zs = self.SM[:, 16:24]
            P.op("dve", lambda g_, G3=G3: g_.tensor_reduce(out=zs, in_=G3, axis=AX.X, op=ALU.add), r=["GATE"], w=["SM2"])
            yield
            P.op("dve", lambda g_: g_.reciprocal(out=zs, in_=zs), r=["SM2"], w=["SM2"])
            yield
            P.tt("dve", G3, G3, apx(zs[:, 0:1], [[1, 8], [0, 16]]), ALU.mult, r=["GATE", "SM2"], w=["GATE"])
            yield

        def gather(table, eb, k):
            sl = self.gs_rot % NGS
            self.gs_rot += 1
            P.kb.add("pool", lambda g_: g_.indirect_dma_start(
                out=GS[sl], out_offset=None, in_=table,
                in_offset=bass.IndirectOffsetOnAxis(ap=self.EIDX[:, eb, k:k + 1], axis=0)),
                self._b([f"EIDX{eb}"]), self._b([f"GS{sl}"]), dma=f"gs{sl}")
            return GS[sl], f"GS{sl}"

        nt = PEER_TILES
        for _ in topk_gen(0, 0):
            pass
        for i in range(nt):
            ts_ = slice(i * 128, (i + 1) * 128)
            eb = i % 2
            nxt = topk_gen(i + 1, 1 - eb) if i + 1 < nt else None
            P.dma("sp", "x1r", X1, self.x1d[ts_, :], r=["x1d"], w=["X1"])
            for k in range(128):
                g, gn = gather(pu_flat, eb, k)
                P.stt(g, g, 1.0, X1, ALU.mult, ALU.mult, r=[gn, "X1"], w=[gn, "H"], accum_out=H[:, k:k + 1])
            P.tt("dve", W, H, H, ALU.mult, r=["H"], w=["W"])
            P.ts("dve", W, W, 0.044715, 1.0, ALU.mult, ALU.add, r=["W"], w=["W"])
            P.tt("dve", W, W, H, ALU.mult, r=["W", "H"], w=["W"])
            P.act(W, W, AF.Sigmoid, r=["W"], w=["W"], scale=1.5957691216057308)
            P.tt("dve", W, W, H, ALU.mult, r=["W", "H"], w=["W"])
            P.tt("dve", W, W, GATE, ALU.mult, r=["W", "GATE"], w=["W"])
            for k in range(128):
                g, gn = gather(pv_flat, eb, k)
                if k == 0:
                    P.ts("dve", ACC, g, W[:, 0:1], None, ALU.mult, r=[gn, "W"], w=["ACC"])
                else:
                    P.stt(ACC, g, W[:, k:k + 1], ACC, ALU.mult, ALU.add, r=[gn, "W", "ACC"], w=["ACC"])
                if nxt is not None:
                    for _ in range(2):
                        if next(nxt, "end") == "end":
                            nxt = None
                            break
            if nxt is not None:
                for _ in nxt:
                    pass
            P.stt(ACC, X1, ALPHA, ACC, ALU.mult, ALU.add, r=["X1", "ACC"], w=["ACC"])
            xb = self.XBT[0]
            P.cp("act", xb[:], ACC, r=["ACC"], w=["XBT0"])
            bk = self.psn(allb)
            psb = self.PS[bk][:].bitcast(BF16)
            for c in range(8):
                P.tr(psb[:, c * 128:(c + 1) * 128], xb[:, c * 128:(c + 1) * 128], self.identb, r=["XBT0", "CB"], w=[f"ps{bk}"])
            RTB = self.T5[5][:].bitcast(BF16)
            RT = RTB.rearrange("p (c t) -> p c t", c=8)
            P.cp("act", RTB, psb, r=[f"ps{bk}"], w=["T5_5"])
            pb16 = self.XBT[1]
            P.dma("pool", "pld", pb16[:, 0:256], self.p_in[l, ts_, :], r=[], w=["XBT1"])
            bk2 = self.psn(allb)
            psb2 = self.PS[bk2][:].bitcast(BF16)
            for c in range(2):
                P.tr(psb2[:, c * 128:(c + 1) * 128], pb16[:, c * 128:(c + 1) * 128], self.identb, r=["XBT1", "CB"], w=[f"ps{bk2}"])
            PTT = self.PT[0][:, 0:256].rearrange("p (c t) -> p c t", c=2)
            P.cp("act", self.PT[0][:, 0:256], psb2[:, 0:256], r=[f"ps{bk2}"], w=["PT0"])
            for half in range(2):
                hs = slice(half * 512, (half + 1) * 512)
                pg = self.psn(allb)
                for c in range(8):
                    P.mm(self.PS[pg][:], RT[:, c, :], GW2[:, c, hs], start=(c == 0), stop=(c == 7),
                         r=["T5_5", "F3"], w=[f"ps{pg}"])
                pp = self.psn(allb)
                for c in range(2):
                    P.mm(self.PS[pp][:], PTT[:, c, :], PW[:, c, hs], start=(c == 0), stop=(c == 1),
                         r=["PT0", "F2"], w=[f"ps{pp}"])
                t = self.T5[half]; tn = f"T5_{half}"
                P.tt("dve", t[:], self.PS[pg][:], GB[:, hs], ALU.add, r=[f"ps{pg}", "F2"], w=[tn])
                P.act(t[:], t[:], AF.Sigmoid, r=[tn], w=[tn])
                P.tt("dve", t[:], t[:], self.PS[pp][:], ALU.mult, r=[tn, f"ps{pp}"], w=[tn])
                P.tt("dve", ACC[:, hs], ACC[:, hs], t[:], ALU.add, r=[tn, "ACC"], w=["ACC"])
            self.layernorm_tile(ACC, "ACC", G2, B2, "F2")
            P.dma("sp", "xw", dst[ts_, :], ACC, r=["ACC"], w=[dstn])


def host_tables(inp):
    L = DEPTH
    chanp = np.zeros((L, 128, CP_N), np.float32)
    for j in range(2):
        chanp[:, :, CP_CB + j] = inp["conv_b"][:, j * 128:(j + 1) * 128]
        chanp[:, :, CP_LG + j] = inp["conv_ln_g"][:, j * 128:(j + 1) * 128]
        chanp[:, :, CP_LB + j] = inp["conv_ln_b"][:, j * 128:(j + 1) * 128]
        chanp[:, :, CP_CW + j * 31:CP_CW + (j + 1) * 31] = np.transpose(
            inp["conv_w"][:, :, j * 128:(j + 1) * 128], (0, 2, 1))
    for h in range(6):
        chanp[:, 0:32, CP_GB + h] = inp["gla_gate_b"][:, h * 32:(h + 1) * 32]
        chanp[:, 0:64, CP_NG + h] = inp["gla_norm_g"][:, h * 64:(h + 1) * 64]
        chanp[:, :, CP_FB + h] = inp["fox_forget_b"][:, h][:, None]
    keysT = np.ascontiguousarray(
        np.transpose(np.asarray(inp["peer_keys"]).reshape(L, 16, 128, 128), (0, 1, 3, 2)))
    return chanp, keysT


SHARED = ["w_in", "gla_gate_w", "w_out", "ln1_g", "ln1_b", "peer_wq", "peer_u", "peer_v",
          "ple_w", "ple_gw", "ple_gb", "ln2_g", "ln2_b"]


def make_in_maps(inp, cores):
    inp = {k: np.asarray(v) for k, v in inp.items()}
    chanp, keysT = host_tables(inp)
    cf, cb = host_consts()
    shared = {k: np.ascontiguousarray(inp[k], dtype=np.float32) for k in SHARED}
    shared.update(chanp=chanp, keysT=keysT, cf32=cf, cb16=cb)
    maps = []
    for c in cores:
        m = dict(shared)
        m["x"] = np.ascontiguousarray(inp["x"][c])
        m["p"] = np.ascontiguousarray(inp["p"][:, c])
        maps.append(m)
    return maps


_PROG = None


def kernel(**inputs):
    global _PROG
    if _PROG is None:
        _PROG = Program()
        _PROG.build()
    maps = make_in_maps(inputs, list(range(8)))
    res = run_bass_kernel_spmd(_PROG.nc, maps, core_ids=list(range(8)))
    return np.stack([np.asarray(r["y"]) for r in res.results], axis=0).astype(np.float32)
```

```python
import numpy as np
import ml_dtypes
from contextlib import ExitStack
import concourse.bass as bass
import concourse.mybir as mybir
from concourse.bass_utils import run_bass_kernel_spmd

F32 = mybir.dt.float32
BF16 = mybir.dt.bfloat16
U32 = mybir.dt.uint32
I32 = mybir.dt.int32
AF = mybir.ActivationFunctionType
ALU = mybir.AluOpType
AX = mybir.AxisListType

D = 1024
S = 4096
NT = 32
DEPTH = 4
IN_COLS = 2838
ALPHA = (2.0 * DEPTH) ** 0.25
EPS = 1e-5
C_AVAL, C_AGATE, C_BQ, C_BK, C_BV, C_BG, C_BLR, C_CQ, C_CK, C_CV, C_CF = (
    0, 256, 512, 704, 896, 1280, 1664, 1680, 2064, 2448, 2832)


class Op:
    __slots__ = ("eng", "fn", "deps", "stream", "needed", "val", "isdma")


class Buf:
    __slots__ = ("name", "w", "r")

    def __init__(self, name):
        self.name = name
        self.w = {}
        self.r = {}


class KB:
    def __init__(self, nc, es):
        self.nc = nc
        self.es = es
        self.ops = []
        self.eng = {"pe": nc.tensor, "act": nc.scalar, "dve": nc.vector,
                    "pool": nc.gpsimd, "sp": nc.sync}
        self.sems = {}
        self.nsem = 0

    def sem(self, name):
        if name not in self.sems:
            self.sems[name] = self.es.enter_context(self.nc.semaphore("s_" + name))
            self.nsem += 1
        return self.sems[name]

    def add(self, eng, fn, reads=(), writes=(), dma=None):
        op = Op()
        op.eng = eng
        op.fn = fn
        op.isdma = dma is not None
        op.stream = ("d_" + dma) if dma is not None else eng
        op.needed = False
        op.val = 0
        deps = set()
        for b in reads:
            deps.update(b.w.values())
        for b in writes:
            deps.update(b.w.values())
            deps.update(b.r.values())
        op.deps = [d for d in deps if not (eng == "pe" and d.stream == "pe")]
        for d in op.deps:
            d.needed = True
        for b in reads:
            b.r[op.stream] = op
        for b in writes:
            b.w = {op.stream: op}
            b.r = {}
        self.ops.append(op)
        return op

    def emit(self):
        counters = {}
        for op in self.ops:
            if op.needed or op.isdma:
                inc = 16 if op.isdma else 1
                counters[op.stream] = counters.get(op.stream, 0) + inc
                op.val = counters[op.stream]
        for st in counters:
            self.sem(st)
        seen = {e: {} for e in self.eng}
        n = 0
        for op in self.ops:
            e = self.eng[op.eng]
            need = {}
            for d in op.deps:
                if d.val > need.get(d.stream, 0):
                    need[d.stream] = d.val
            sn = seen[op.eng]
            for st, v in need.items():
                if sn.get(st, 0) < v:
                    e.wait_ge(self.sems[st], v)
                    sn[st] = v
                    n += 1
            if op.fn is not None:
                ins = op.fn(e)
                n += 1
                if op.needed or op.isdma:
                    ins.then_inc(self.sems[op.stream], 16 if op.isdma else 1)
        return n


def apx(base, dims):
    return bass.AP(base.tensor, base.offset, [list(base.ap[0])] + [list(d) for d in dims])


class Prog:
    def __init__(self, n_layers=DEPTH, dbg=None):
        self.n_layers = n_layers
        self.dbg = dbg or {}
        self.nc = bass.Bass("TRN2", target_bir_lowering=False)
        self.es = ExitStack()
        self.kb = KB(self.nc, self.es)
        self.bufs = {}

    def dram_in(self, name, shape, dt=F32):
        return self.nc.dram_tensor(name, list(shape), dt, kind="ExternalInput").ap()

    def dram_out(self, name, shape, dt=F32):
        return self.nc.dram_tensor(name, list(shape), dt, kind="ExternalOutput").ap()

    def dram_tmp(self, name, shape, dt=F32):
        return self.nc.dram_tensor(name, list(shape), dt, kind="Internal").ap()

    def sb(self, name, shape, dt=F32):
        return self.es.enter_context(self.nc.sbuf_tensor(name, list(shape), dt))

    def B(self, name):
        if name not in self.bufs:
            self.bufs[name] = Buf(name)
        return self.bufs[name]

    def _b(self, xs):
        return [self.B(x) if isinstance(x, str) else x for x in xs]

    def op(self, eng, fn, r=(), w=()):
        return self.kb.add(eng, fn, self._b(r), self._b(w))

    def dma(self, q, sem, out, in_, r=(), w=(), **kw):
        e = {"sp": "sp", "pool": "pool", "act": "act"}[q]
        return self.kb.add(e, lambda g: g.dma_start(out=out, in_=in_, **kw),
                           self._b(r), self._b(w), dma=sem)

    def mm(self, out, lhsT, rhs, start, stop, r=(), w=()):
        return self.op("pe", lambda g: g.matmul(out, lhsT, rhs, start=start, stop=stop), r, w)

    def tr(self, out, in_, ident, r=(), w=()):
        return self.op("pe", lambda g: g.transpose(out, in_, ident), r, w)

    def act(self, out, in_, func, r=(), w=(), **kw):
        return self.op("act", lambda g: g.activation(out=out, in_=in_, func=func, **kw), r, w)

    def tt(self, eng, out, in0, in1, op, r=(), w=()):
        return self.op(eng, lambda g: g.tensor_tensor(out=out, in0=in0, in1=in1, op=op), r, w)

    def ts(self, eng, out, in0, s1, s2, op0, op1=None, r=(), w=(), **kw):
        if op1 is None:
            return self.op(eng, lambda g: g.tensor_scalar(out=out, in0=in0, scalar1=s1, scalar2=None,
                                                          op0=op0, **kw), r, w)
        return self.op(eng, lambda g: g.tensor_scalar(out=out, in0=in0, scalar1=s1, scalar2=s2,
                                                      op0=op0, op1=op1, **kw), r, w)

    def stt(self, out, in0, scalar, in1, op0, op1, r=(), w=(), **kw):
        return self.op("dve", lambda g: g.scalar_tensor_tensor(out=out, in0=in0, scalar=scalar, in1=in1,
                                                               op0=op0, op1=op1, **kw), r, w)

    def cp(self, eng, out, in_, r=(), w=()):
        if eng == "act":
            return self.op("act", lambda g: g.copy(out=out, in_=in_), r, w)
        return self.op(eng, lambda g: g.tensor_copy(out=out, in_=in_), r, w)


CP_CB, CP_LG, CP_LB, CP_CW, CP_GB, CP_NG, CP_FB, CP_N = 0, 2, 4, 6, 68, 74, 80, 86
CF_IDENT, CF_ONES, CF_SCM, CF_IOTA, CF_E0, CF_E1, CF_M0, CF_M1, CF_N = 0, 128, 256, 768, 784, 785, 786, 787, 788
CB_IDENT, CB_CMASK, CB_GMASK, CB_ONES, CB_N = 0, 128, 256, 384, 512


def host_consts():
    cf = np.zeros((128, CF_N), np.float32)
    cf[:, CF_IDENT:CF_IDENT + 128] = np.eye(128, dtype=np.float32)
    cf[:, CF_ONES:CF_ONES + 128] = 1.0
    scm = np.ones((512,), np.float32)
    scm[::64] = 0.0
    cf[:, CF_SCM:CF_SCM + 512] = scm[None, :]
    cf[:, CF_IOTA:CF_IOTA + 16] = np.arange(16, dtype=np.float32)[None, :]
    cf[64, CF_E0] = 1.0
    cf[65, CF_E1] = 1.0
    cf[0:64, CF_M0] = 1.0
    cf[64:128, CF_M1] = 1.0
    cb = np.zeros((128, CB_N), np.float32)
    cb[:, CB_IDENT:CB_IDENT + 128] = np.eye(128)
    s = np.arange(128)[:, None]
    t = np.arange(128)[None, :]
    cb[:, CB_CMASK:CB_CMASK + 128] = (t >= s)
    cb[:, CB_GMASK:CB_GMASK + 128] = (t >= s) & ((t // 64) == (s // 64))
    cb[:, CB_ONES:CB_ONES + 128] = 1.0
    return cf, cb.astype(ml_dtypes.bfloat16)


GLA_STAGE = 99
PEER_STAGE = 99
PEER_TILES = NT


class Program(Prog):
    def build(self):
        nc = self.nc
        P = self
        L = DEPTH
        self.x_in = P.dram_in("x", [S, D])
        self.p_in = P.dram_in("p", [L, S, 256])
        self.w_in = P.dram_in("w_in", [L, D, IN_COLS])
        self.chanp_d = P.dram_in("chanp", [L, 128, CP_N])
        self.gatew_d = P.dram_in("gla_gate_w", [L, 16, 192])
        self.w_out = P.dram_in("w_out", [L, D, D])
        self.ln1_g = P.dram_in("ln1_g", [L, D]); self.ln1_b = P.dram_in("ln1_b", [L, D])
        self.wq = P.dram_in("peer_wq", [L, D, 2048])
        self.keysT = P.dram_in("keysT", [L, 16, 128, 128])
        self.pu = P.dram_in("peer_u", [L, 16384, D])
        self.pv = P.dram_in("peer_v", [L, 16384, D])
        self.ple_w = P.dram_in("ple_w", [L, 256, D])
        self.ple_gw = P.dram_in("ple_gw", [L, D, D])
        self.ple_gb = P.dram_in("ple_gb", [L, D])
        self.ln2_g = P.dram_in("ln2_g", [L, D]); self.ln2_b = P.dram_in("ln2_b", [L, D])
        self.cf_d = P.dram_in("cf32", [128, CF_N])
        self.cb_d = P.dram_in("cb16", [128, CB_N], BF16)
        self.y = P.dram_out("y", [S, D])
        self.xcur = P.dram_tmp("xcur", [S, D])
        self.x1d = P.dram_tmp("x1d", [S, D])
        self.mixT = P.dram_tmp("mixT", [D, S], BF16)
        self.ub = P.dram_tmp("ub", [16384, D], BF16)
        self.vb = P.dram_tmp("vb", [16384, D], BF16)
        self.dbg_out = {}
        for name, (shape, dt, attr, bufs) in self.dbg.items():
            self.dbg_out[name] = P.dram_out("dbg_" + name, shape, dt)
        self.XT = P.sb("XT", [128, 8, S], BF16)
        self.FA = P.sb("FA", [128, 4, 4128], F32)
        self.BA = P.sb("BA", [128, 4, S], BF16)
        self.WB = [P.sb("WB0", [128, 8, 256], BF16), P.sb("WB1", [128, 8, 256], BF16)]
        self.CF = P.sb("CF", [128, CF_N], F32)
        self.CB = P.sb("CB", [128, CB_N], BF16)
        self.CHP = P.sb("CHP", [128, CP_N], F32)
        self.XBT = [P.sb("XBT0", [128, 1024], BF16), P.sb("XBT1", [128, 1024], BF16)]
        self.T5 = [P.sb(f"T5_{i}", [128, 512], F32) for i in range(6)]
        self.VT = P.sb("VT", [128, NT, 64], BF16)
        self.KET = P.sb("KET", [128, 2, NT, 32], BF16)
        self.CT = P.sb("CT", [128, NT], F32)
        self.PT = [P.sb(f"PT{i}", [128, 512], BF16) for i in range(3)]
        self.GW = P.sb("GW", [16, 192], BF16)
        self.SM = P.sb("SM", [128, 64], F32)
        self.PK = P.sb("PK", [128, 1536], F32)
        self.EIDX = P.sb("EIDX", [128, 2, 128], U32)
        self.gs_rot = 0
        self.PS = [self.es.enter_context(nc.psum_tensor(f"ps{i}", [128, 512], F32)) for i in range(8)]
        self.ps_rot = 0
        self.identb = self.CB[:, CB_IDENT:CB_IDENT + 128]
        self.identf = self.CF[:, CF_IDENT:CF_IDENT + 128]
        self.onesf = self.CF[:, CF_ONES:CF_ONES + 128]
        self.onesb = self.CB[:, CB_ONES:CB_ONES + 128]
        P.dma("sp", "cst", self.CF[:], self.cf_d, w=["CF"])
        P.dma("sp", "cst", self.CB[:], self.cb_d, w=["CB"])
        P.op("dve", lambda g: g.memset(self.FA[:, 0, 0:30], 0.0), w=["F0"])

        for l in range(self.n_layers):
            self.layer(l)

        fin = []
        for name, (shape, dt, attr, bufs) in self.dbg.items():
            P.dma("sp", "dbg", self.dbg_out[name], getattr(self, attr), r=bufs, w=["dbg_" + name])
            fin.append("dbg_" + name)
        fin.append("y")
        P.op("sp", None, r=fin)
        n = self.kb.emit()
        return n

    def convert_tables(self, l):
        for (src, dstt, nm) in ((self.pu, self.ub, "UB"), (self.pv, self.vb, "VB")):
            for j in range(16):
                self.dma("pool", "cv" + nm, dstt[j * 1024:(j + 1) * 1024, :], src[l, j * 1024:(j + 1) * 1024, :],
                         r=[], w=[nm])

    def psn(self, banks):
        b = banks[self.ps_rot % len(banks)]
        self.ps_rot += 1
        return b

    def F(self, i, lo=0, hi=4096):
        return self.FA[:, i, lo:hi]

    def layer(self, l):
        P = self
        src = self.x_in if l == 0 else self.xcur
        P.dma("sp", "chp", self.CHP[:], self.chanp_d[l], r=[], w=["CHP"])
        P.dma("pool", "gw", self.GW[:], self.gatew_d[l], r=[], w=["GW"])
        self.phase_xT(src, "xsrc")
        self.unit_conv(l)
        self.convert_tables(l)
        self.lrt_done = False
        for h in range(6):
            self.unit_gla(l, h)
        for h in range(6):
            self.unit_fox(l, h)
        self.phase_out(l, src)
        self.phase_peer(l)

    def phase_xT(self, src, srcname):
        P = self
        for i in range(NT):
            s = i % 2
            xb = self.XBT[s]
            P.dma("pool", f"xbt{s}", xb[:], src[i * 128:(i + 1) * 128, :], r=[srcname], w=[f"XBT{s}"])
            self.transpose_to_XT(xb, f"XBT{s}", i)

    def transpose_to_XT(self, xb, xbname, i):
        P = self
        bank = self.psn([0, 1])
        psb = self.PS[bank][:].bitcast(BF16)
        for c in range(8):
            P.tr(psb[:, c * 128:(c + 1) * 128], xb[:, c * 128:(c + 1) * 128], self.identb,
                 r=[xbname, "CB"], w=[f"ps{bank}"])
        P.cp("act" if i % 2 else "dve", self.XT[:, :, i * 128:(i + 1) * 128],
             psb.rearrange("p (c t) -> p c t", c=8), r=[f"ps{bank}"], w=["XT"])

    def load_w(self, slot, l, cols):
        off = 0
        for (c0, n) in cols:
            kw = dict(allow_slow_non_contiguous=True) if n == 1 else {}
            self.dma("pool", f"wb{slot}", self.WB[slot][:, :, off:off + n],
                     self.w_in[l, :, c0:c0 + n].rearrange("(c p) n -> p c n", p=128), r=[], w=[f"WB{slot}"], **kw)
            off += n

    def proj_fm(self, bank, slot, woff, m, g, prow=0):
        for c in range(8):
            self.mm(self.PS[bank][prow:prow + m, :], self.WB[slot][:, c, woff:woff + m],
                    self.XT[:, c, g * 512:(g + 1) * 512], start=(c == 0), stop=(c == 7),
                    r=[f"WB{slot}", "XT"], w=[f"ps{bank}"])

    def proj_v_tm(self, slot, woff, g, banks):
        bank = self.psn(banks)
        for j in range(4):
            i = 4 * g + j
            for c in range(8):
                self.mm(self.PS[bank][:, j * 64:(j + 1) * 64], self.XT[:, c, i * 128:(i + 1) * 128],
                        self.WB[slot][:, c, woff:woff + 64], start=(c == 0), stop=(c == 7),
                        r=[f"WB{slot}", "XT"], w=[f"ps{bank}"])
        self.cp("act", self.VT[:, 4 * g:4 * g + 4, :],
                self.PS[bank][:, 0:256].rearrange("p (j e) -> p j e", j=4), r=[f"ps{bank}"], w=["VT"])

    def unit_conv(self, l):
        P = self
        UP = self.FA[:, 0, :]
        for j in range(2):
            slot = j
            self.load_w(slot, l, [(C_AVAL + j * 128, 128), (C_AGATE + j * 128, 128)])
            for g in range(8):
                a = self.psn([2, 3, 4, 5]); b = self.psn([2, 3, 4, 5])
                self.proj_fm(a, slot, 0, 128, g)
                self.proj_fm(b, slot, 128, 128, g)
                t = self.T5[g % 2]
                P.act(t[:], self.PS[b][:], AF.Sigmoid, r=[f"ps{b}"], w=[f"T5_{g % 2}"])
                P.tt("dve", UP[:, 30 + g * 512:30 + (g + 1) * 512], self.PS[a][:], t[:], ALU.mult,
                     r=[f"ps{a}", f"T5_{g % 2}"], w=["F0"])
            acc = self.F(1 + j)
            cw = self.CHP[:, CP_CW + j * 31:CP_CW + (j + 1) * 31]
            P.ts("dve", acc, UP[:, 0:4096], cw[:, 0:1], self.CHP[:, CP_CB + j:CP_CB + j + 1],
                 ALU.mult, ALU.add, r=["F0", "CHP"], w=[f"F{1 + j}"])
            for k in range(1, 31):
                P.stt(acc, UP[:, k:k + 4096], cw[:, k:k + 1], acc, ALU.mult, ALU.add,
                      r=["F0", "CHP", f"F{1 + j}"], w=[f"F{1 + j}"])
        for g in range(8):
            gs = slice(g * 512, (g + 1) * 512)
            pm = self.psn([2, 3, 4, 5]); pe = self.psn([2, 3, 4, 5])
            for j in range(2):
                P.mm(self.PS[pm][:], self.onesf, self.F(1 + j)[:, gs], start=(j == 0), stop=(j == 1),
                     r=["CF", f"F{1 + j}"], w=[f"ps{pm}"])
            for j in range(2):
                sq = self.T5[j]
                P.act(sq[:], self.F(1 + j)[:, gs], AF.Square, r=[f"F{1 + j}"], w=[f"T5_{j}"])
                P.mm(self.PS[pe][:], self.onesf, sq[:], start=(j == 0), stop=(j == 1),
                     r=["CF", f"T5_{j}"], w=[f"ps{pe}"])
            mean = self.T5[2]; msq = self.T5[3]; var = self.T5[4]
            P.act(mean[:], self.PS[pm][:], AF.Copy, r=[f"ps{pm}"], w=["T5_2"], scale=1.0 / 256)
            P.act(msq[:], mean[:], AF.Square, r=["T5_2"], w=["T5_3"])
            P.stt(var[:], self.PS[pe][:], 1.0 / 256, msq[:], ALU.mult, ALU.subtract, r=[f"ps{pe}", "T5_3"], w=["T5_4"])
            P.ts("dve", var[:], var[:], 0.0, EPS, ALU.max, ALU.add, r=["T5_4"], w=["T5_4"])
            P.act(var[:], var[:], AF.Sqrt, r=["T5_4"], w=["T5_4"])
            P.op("dve", lambda g_, v=var: g_.reciprocal(out=v[:], in_=v[:]), r=["T5_4"], w=["T5_4"])
            for j in range(2):
                t = self.T5[j]
                P.tt("dve", t[:], self.F(1 + j)[:, gs], mean[:], ALU.subtract, r=[f"F{1 + j}", "T5_2"], w=[f"T5_{j}"])
                P.tt("dve", t[:], t[:], var[:], ALU.mult, r=[f"T5_{j}", "T5_4"], w=[f"T5_{j}"])
                P.act(self.BA[:, j, gs], t[:], AF.Silu, r=[f"T5_{j}", "CHP"], w=[f"B{j}"],
                      scale=self.CHP[:, CP_LG + j:CP_LG + j + 1], bias=self.CHP[:, CP_LB + j:CP_LB + j + 1])
        for j in range(2):
            P.dma("sp", f"mixw{j}", self.mixT[j * 128:(j + 1) * 128, :], self.BA[:, j, :], r=[f"B{j}"], w=["mixT"])

    def gla_end(self, h):
        self.dma("sp", f"mixw{h % 2}", self.mixT[256 + h * 64:320 + h * 64, :], self.BA[0:64, 2, :], r=["B2"], w=["mixT"])

    def unit_gla(self, l, h):
        P = self
        slot = h % 2
        wn = f"WB{slot}"
        banks = [0, 1, 2, 3, 4, 5]
        QE = self.BA[:, 0, :]; KE = self.BA[:, 1, :]; AT = self.BA[:, 2, :]; SB = self.BA[:, 3, :]
        GZ = self.FA[:, 0, 30:4126]; EC = self.F(1); EN = self.F(2); ST = self.F(3)
        D1 = GZ; D0 = EN
        rq = slice(0, 32)
        self.load_w(slot, l, [(C_BQ + h * 32, 32), (C_BK + h * 32, 32), (C_BV + h * 64, 64), (C_BG + h * 64, 64),
                              (C_BLR, 16)])
        for g in range(8):
            gs = slice(g * 512, (g + 1) * 512)
            a0 = self.psn(banks)
            self.proj_fm(a0, slot, 192, 16, g)
            lrt = self.PT[g % 2]; lrn = f"PT{g % 2}"
            P.cp("act", lrt[0:16, :], self.PS[a0][0:16, :], r=[f"ps{a0}"], w=[lrn])
            a = self.psn(banks)
            P.mm(self.PS[a][rq, :], self.GW[0:16, h * 32:(h + 1) * 32], lrt[0:16, :], start=True, stop=True,
                 r=["GW", lrn], w=[f"ps{a}"])
            P.ts("dve", GZ[rq, gs], self.PS[a][rq, :], self.CHP[rq, CP_GB + h:CP_GB + h + 1], None, ALU.add,
                 r=[f"ps{a}", "CHP"], w=["F0"])
        P.act(GZ[rq, :], GZ[rq, :], AF.Exp, r=["F0"], w=["F0"], scale=-1.0)
        P.ts("dve", GZ[rq, :], GZ[rq, :], 1.0, None, ALU.add, r=["F0"], w=["F0"])
        P.act(GZ[rq, :], GZ[rq, :], AF.Ln, r=["F0"], w=["F0"])
        scm = self.CF[rq, CF_SCM:CF_SCM + 512]
        for g in range(8):
            gs = slice(g * 512, (g + 1) * 512)
            P.op("dve", lambda g_, gs=gs: g_.tensor_tensor_scan(
                out=ST[rq, gs], data0=scm, data1=GZ[rq, gs], initial=0.0, op0=ALU.mult, op1=ALU.subtract),
                r=["F0", "CF"], w=["F3"])
        P.act(EC[rq, :], ST[rq, :], AF.Exp, r=["F3"], w=["F1"], scale=1.0 / 16)
        P.act(EN[rq, :], ST[rq, :], AF.Exp, r=["F3"], w=["F2"], scale=-1.0 / 16)
        if GLA_STAGE < 1:
            return
        for g in range(8):
            gs = slice(g * 512, (g + 1) * 512)
            a = self.psn(banks)
            self.proj_fm(a, slot, 0, 32, g)
            P.stt(QE[rq, gs], self.PS[a][rq, :], 32.0 ** -0.5, EC[rq, gs], ALU.mult, ALU.mult,
                  r=[f"ps{a}", "F1"], w=["B0"])
            b = self.psn(banks)
            self.proj_fm(b, slot, 32, 32, g)
            P.tt("dve", KE[rq, gs], self.PS[b][rq, :], EN[rq, gs], ALU.mult, r=[f"ps{b}", "F2"], w=["B1"])
            self.proj_v_tm(slot, 64, g, banks)
        if GLA_STAGE < 2:
            return
        pk = self.psn(banks)
        pkb = self.PS[pk][:].bitcast(BF16)
        for i in range(NT):
            P.tr(pkb[:, i * 32:(i + 1) * 32], KE[rq, i * 128:(i + 1) * 128], self.identb[0:32, 0:32],
                 r=["B1", "CB"], w=[f"ps{pk}"])
        for m in range(2):
            P.ts("dve", self.KET[:, m, :, :], pkb.rearrange("p (i d) -> p i d", i=NT),
                 self.CF[:, CF_M0 + m:CF_M0 + m + 1], None, ALU.mult, r=[f"ps{pk}", "CF"], w=["KET"])
        if GLA_STAGE < 3:
            return
        ATv = AT.rearrange("p (i t) -> p i t", i=NT)
        gmask4 = apx(self.CB[:, CB_GMASK:CB_GMASK + 128], [[0, 4], [1, 128]])
        for i4 in range(8):
            a = self.psn(banks)
            for j in range(4):
                i = 4 * i4 + j
                P.mm(self.PS[a][:, j * 128:(j + 1) * 128], KE[rq, i * 128:(i + 1) * 128], QE[rq, i * 128:(i + 1) * 128],
                     start=True, stop=True, r=["B0", "B1"], w=[f"ps{a}"])
            for j in range(4):
                if GLA_STAGE == 3.5:
                    break
                P.tt("dve", ATv[:, 4 * i4 + j, :], self.PS[a][:, j * 128:(j + 1) * 128],
                     self.CB[:, CB_GMASK:CB_GMASK + 128], ALU.mult, r=[f"ps{a}", "CB"], w=["B2"])
        if GLA_STAGE < 4:
            return self.gla_end(h)
        for c8 in range(8):
            a = self.psn(banks)
            for cc in range(8):
                c = c8 * 8 + cc
                i = c // 2; r0 = (c % 2) * 64
                P.mm(self.PS[a][rq, cc * 64:(cc + 1) * 64], self.KET[:, c % 2, i, :], self.VT[:, i, :],
                     start=True, stop=True, r=["KET", "VT"], w=[f"ps{a}"])
            ac = apx(EC[rq, 64 * (c8 * 8) + 63:64 * (c8 * 8) + 64], [[64, 8], [0, 64]])
            out = apx(D1[rq, c8 * 8:c8 * 8 + 1], [[1, 8], [64, 64]])
            P.tt("dve", out, self.PS[a][rq, :].rearrange("p (c e) -> p c e", c=8), ac, ALU.mult,
                 r=[f"ps{a}", "F1"], w=["F0"])
        if GLA_STAGE < 5:
            return
        P.cp("dve", D0[rq, :].rearrange("p (e c) -> p e c", e=64), apx(EC[rq, 63:64], [[0, 64], [64, 64]]),
             r=["F1"], w=["F2"])
        P.op("dve", lambda g_: g_.memset(apx(D0[rq, 0:1], [[64, 64]]), 0.0), w=["F2"])
        P.op("dve", lambda g_: g_.tensor_tensor_scan(out=ST[rq, :], data0=D0[rq, :], data1=D1[rq, :], initial=0.0,
                                                    op0=ALU.mult, op1=ALU.add), r=["F0", "F2"], w=["F3"])
        if GLA_STAGE < 6:
            return
        P.cp("act", SB[rq, :].rearrange("p (c e) -> p c e", c=64), apx(ST[rq, 0:1], [[1, 64], [64, 64]]),
             r=["F3"], w=["B3"])
        if GLA_STAGE < 7:
            return
        r0m = 256 + h * 64
        BO = KE
        for g in range(8):
            gs = slice(g * 512, (g + 1) * 512)
            po = self.psn(banks)
            for cc in range(8):
                c = g * 8 + cc
                i = c // 2; r0 = (c % 2) * 64
                cs = slice(cc * 64, (cc + 1) * 64)
                P.mm(self.PS[po][0:64, cs], self.VT[:, i, :], ATv[:, i, r0:r0 + 64],
                     start=True, stop=(c == 0), r=["VT", "B2"], w=[f"ps{po}"])
                if c > 0:
                    P.mm(self.PS[po][0:64, cs], SB[rq, (c - 1) * 64:c * 64], QE[rq, c * 64:(c + 1) * 64],
                         start=False, stop=True, r=["B3", "B0"], w=[f"ps{po}"])
            ot = self.T5[0]; sq = self.T5[1]; rst = self.T5[2]; sg = self.T5[3]
            P.act(ot[0:64, :], self.PS[po][0:64, :], AF.Copy, r=[f"ps{po}"], w=["T5_0"])
            P.act(sq[0:64, :], self.PS[po][0:64, :], AF.Square, r=[f"ps{po}"], w=["T5_1"])
            pm = self.psn(banks)
            P.mm(self.PS[pm][0:64, :], self.onesf[0:64, 0:64], sq[0:64, :], start=True, stop=True,
                 r=["CF", "T5_1"], w=[f"ps{pm}"])
            P.ts("dve", rst[0:64, :], self.PS[pm][0:64, :], 1.0 / 64, EPS, ALU.mult, ALU.add, r=[f"ps{pm}"], w=["T5_2"])
            P.act(rst[0:64, :], rst[0:64, :], AF.Sqrt, r=["T5_2"], w=["T5_2"])
            P.op("dve", lambda g_, rst=rst: g_.reciprocal(out=rst[0:64, :], in_=rst[0:64, :]), r=["T5_2"], w=["T5_2"])
            pg = self.psn(banks)
            self.proj_fm(pg, slot, 128, 64, g)
            P.act(sg[0:64, :], self.PS[pg][0:64, :], AF.Silu, r=[f"ps{pg}"], w=["T5_3"])
            P.tt("dve", ot[0:64, :], ot[0:64, :], rst[0:64, :], ALU.mult, r=["T5_0", "T5_2"], w=["T5_0"])
            P.stt(BO[0:64, gs], ot[0:64, :], self.CHP[0:64, CP_NG + h:CP_NG + h + 1],
                  sg[0:64, :], ALU.mult, ALU.mult, r=["T5_0", "T5_3", "CHP"], w=["B1"])
        P.dma("sp", f"mixw{h % 2}", self.mixT[r0m:r0m + 64, :], BO[0:64, :], r=["B1"], w=["mixT"])

    def unit_fox(self, l, h):
        P = self
        slot = h % 2
        WBs = self.WB[slot]
        wn = f"WB{slot}"
        self.load_w(slot, l, [(C_CQ + h * 64, 64), (C_CF + h, 1), (C_CF + h, 1),
                              (C_CK + h * 64, 64), (C_CV + h * 64, 64)])
        QA = self.BA[:, 0, :]; KA = self.BA[:, 1, :]; CO = self.BA[:, 2, :]; TB = self.BA[:, 3, :]
        FZ = self.F(1); CC = self.F(2); HF = self.F(3)
        r2 = slice(64, 66)
        fb = self.CHP[64:66, CP_FB + h:CP_FB + h + 1]
        banks = [0, 1, 2, 3, 4, 5]
        for g in range(8):
            gs = slice(g * 512, (g + 1) * 512)
            a = self.psn(banks)
            self.proj_fm(a, slot, 0, 66, g)
            P.act(QA[0:64, gs], self.PS[a][0:64, :], AF.Copy, r=[f"ps{a}"], w=["B0"], scale=0.125)
            P.ts("dve", FZ[r2, gs], self.PS[a][r2, :], fb, None, ALU.add, r=[f"ps{a}", "CHP"], w=["F1"])
            b = self.psn(banks)
            self.proj_fm(b, slot, 66, 64, g)
            P.cp("dve", KA[0:64, gs], self.PS[b][0:64, :], r=[f"ps{b}"], w=["B1"])
            self.proj_v_tm(slot, 130, g, banks)
        P.act(FZ[r2, :], FZ[r2, :], AF.Exp, r=["F1"], w=["F1"], scale=-1.0)
        P.ts("dve", FZ[r2, :], FZ[r2, :], 1.0, None, ALU.add, r=["F1"], w=["F1"])
        P.act(FZ[r2, :], FZ[r2, :], AF.Ln, r=["F1"], w=["F1"])
        ones512 = self.CF[r2, CF_ONES:CF_ONES + 128]
        for pc in range(32):
            sl = slice(pc * 128, (pc + 1) * 128)
            init = 0.0 if pc == 0 else CC[r2, pc * 128 - 1:pc * 128]
            P.op("dve", lambda g_, sl=sl, init=init: g_.tensor_tensor_scan(
                out=CC[r2, sl], data0=ones512, data1=FZ[r2, sl], initial=init,
                op0=ALU.mult, op1=ALU.subtract), r=["F1", "F2", "CF"], w=["F2"])
        P.cp("dve", TB[r2, :], CC[r2, :], r=["F2"], w=["B3"])
        P.cp("dve", HF[r2, :], TB[r2, :], r=["B3"], w=["F3"])
        P.tt("dve", FZ[r2, :], CC[r2, :], HF[r2, :], ALU.subtract, r=["F2", "F3"], w=["F1"])
        P.ts("dve", HF[r2, :], HF[r2, :], self.CF[r2, CF_E0:CF_E0 + 1], None, ALU.mult, r=["F3", "CF"], w=["F3"])
        P.stt(QA[r2, :], FZ[r2, :], self.CF[r2, CF_E1:CF_E1 + 1], HF[r2, :], ALU.mult, ALU.add,
              r=["F1", "F3", "CF"], w=["B0"])
        P.op("dve", lambda g_: g_.memset(KA[r2, :], 1.0), w=["B1"])
        pb = self.psn(banks)
        for i in range(NT):
            P.tr(self.PS[pb][:, i:i + 1], CC[64:65, i * 128:(i + 1) * 128], self.identf[64:65, 64:65],
                 r=["F2", "CF"], w=[f"ps{pb}"])
        P.act(self.CT[:], self.PS[pb][:, 0:NT], AF.Copy, r=[f"ps{pb}"], w=["CT"], scale=-1.0)
        pO, pS = 6, 7
        iters = [(qg, kb) for qg in range(8) for kb in range(4 * (qg + 1))]
        psts = {}

        def stageA(n):
            qg, kb = iters[n]
            gs0 = qg * 512
            col0 = max(0, (kb - 4 * qg) * 128)
            pst = self.psn(banks)
            psts[n] = pst
            P.mm(self.PS[pst][:, col0:512], KA[0:66, kb * 128:(kb + 1) * 128], QA[0:66, gs0 + col0:gs0 + 512],
                 start=True, stop=True, r=["B0", "B1"], w=[f"ps{pst}"])

        LOOK = 2
        for n in range(min(LOOK, len(iters))):
            stageA(n)
        for n, (qg, kb) in enumerate(iters):
            if n + LOOK < len(iters):
                stageA(n + LOOK)
            nkb = 4 * (qg + 1)
            gs0 = qg * 512
            col0 = max(0, (kb - 4 * qg) * 128)
            pst = psts.pop(n)
            pt = self.PT[n % 3]; ptn = f"PT{n % 3}"
            P.act(pt[:, col0:512], self.PS[pst][:, col0:512], AF.Exp, r=[f"ps{pst}", "CT"], w=[ptn],
                  bias=self.CT[:, kb:kb + 1], scale=1.0)
            if kb >= 4 * qg:
                P.tt("pool", pt[:, col0:col0 + 128], pt[:, col0:col0 + 128],
                     self.CB[:, CB_CMASK:CB_CMASK + 128], ALU.mult, r=[ptn, "CB"], w=[ptn])
            P.mm(self.PS[pO][0:64, col0:512], self.VT[:, kb, :], pt[:, col0:512],
                 start=(kb == 0), stop=(kb == nkb - 1), r=["VT", ptn], w=[f"ps{pO}"])
            P.mm(self.PS[pS][0:64, col0:512], self.onesb[:, 0:64], pt[:, col0:512],
                 start=(kb == 0), stop=(kb == nkb - 1), r=["CB", ptn], w=[f"ps{pS}"])
            if kb == nkb - 1:
                rs = self.T5[5]
                P.op("dve", lambda g_, rs=rs: g_.reciprocal(out=rs[0:64, :], in_=self.PS[pS][0:64, :]),
                     r=[f"ps{pS}"], w=["T5_5"])
                P.tt("dve", CO[0:64, gs0:gs0 + 512], self.PS[pO][0:64, :], rs[0:64, :], ALU.mult,
                     r=[f"ps{pO}", "T5_5"], w=["B2"])
        r0 = 640 + h * 64
        P.dma("sp", f"mixw{h % 2}", self.mixT[r0:r0 + 64, :], CO[0:64, :], r=["B2"], w=["mixT"])

    def phase_out(self, l, src):
        P = self
        WO = self.BA[:, 0:2, :].rearrange("p a (c n) -> p (a c) n", n=1024)
        for c in range(8):
            P.dma("pool", "wo", WO[:, c, :], self.w_out[l, c * 128:(c + 1) * 128, :], r=[], w=["B0", "B1"])
        G1 = self.FA[:, 0, 30:1054]; B1 = self.FA[:, 0, 1054:2078]
        P.dma("sp", "lnp", G1, self.ln1_g[l:l + 1, :].partition_broadcast(128), r=[], w=["F0"])
        P.dma("sp", "lnp", B1, self.ln1_b[l:l + 1, :].partition_broadcast(128), r=[], w=["F0"])
        for i in range(NT):
            g = i // 4
            ms = g % 2
            MX = self.BA[:, 2 + ms, :].rearrange("p (c n) -> p c n", c=8)
            if i % 4 == 0:
                P.dma("sp", f"mx{ms}", MX, self.mixT[:, g * 512:(g + 1) * 512].rearrange("(c p) n -> p c n", p=128),
                      r=["mixT"], w=[f"B{2 + ms}"])
            s2 = i % 2
            XR = self.FA[:, 1, s2 * 1024:(s2 + 1) * 1024]
            R = self.FA[:, 1, 2048 + s2 * 1024:2048 + (s2 + 1) * 1024]
            xrn = f"XR{s2}"; rn = f"R{s2}"
            P.dma("sp", f"xr{s2}", XR, src[i * 128:(i + 1) * 128, :], r=["xsrc"], w=[xrn])
            for half in range(2):
                pb = self.psn([2, 3, 4, 5])
                for c in range(8):
                    P.mm(self.PS[pb][:], MX[:, c, (i % 4) * 128:(i % 4 + 1) * 128], WO[:, c, half * 512:(half + 1) * 512],
                         start=(c == 0), stop=(c == 7), r=[f"B{2 + ms}", "B0", "B1"], w=[f"ps{pb}"])
                P.stt(R[:, half * 512:(half + 1) * 512], XR[:, half * 512:(half + 1) * 512], ALPHA, self.PS[pb][:],
                      ALU.mult, ALU.add, r=[xrn, f"ps{pb}"], w=[rn])
            self.layernorm_tile(R, rn, G1, B1, "F0")
            P.dma("sp", f"x1w{s2}", self.x1d[i * 128:(i + 1) * 128, :], R, r=[rn], w=["x1d"])
            xb = self.XBT[s2]
            P.cp("act", xb[:], R, r=[rn], w=[f"XBT{s2}"])
            self.transpose_to_XT(xb, f"XBT{s2}", i)

    def layernorm_tile(self, R, rn, G, Bb, gbn):
        P = self
        st = self.SM[:, 0:12]; mv = self.SM[:, 12:14]; rs = self.SM[:, 14:15]
        for half in range(2):
            P.op("dve", lambda g_, half=half: g_.bn_stats(out=st[:, half * 6:(half + 1) * 6],
                                                        in_=R[:, half * 512:(half + 1) * 512]), r=[rn], w=["SM"])
        P.op("dve", lambda g_: g_.bn_aggr(out=mv, in_=st), r=["SM"], w=["SM"])
        P.ts("dve", rs, mv[:, 1:2], EPS, None, ALU.add, r=["SM"], w=["SM"])
        P.act(rs, rs, AF.Sqrt, r=["SM"], w=["SM"])
        P.op("dve", lambda g_: g_.reciprocal(out=rs, in_=rs), r=["SM"], w=["SM"])
        P.ts("dve", R, R, mv[:, 0:1], rs, ALU.subtract, ALU.mult, r=[rn, "SM"], w=[rn])
        P.tt("pool", R, R, G, ALU.mult, r=[rn, gbn], w=[rn])
        P.tt("dve", R, R, Bb, ALU.add, r=[rn, gbn], w=[rn])

    def phase_peer(self, l):
        P = self
        dst = self.y if (l == DEPTH - 1) else self.xcur
        dstn = "y" if (l == DEPTH - 1) else "xsrc"
        allb = [0, 1, 2, 3, 4, 5]
        WQ = self.BA[:, 0:4, :].rearrange("p a (c n) -> p (a c) n", n=2048)
        for c in range(8):
            P.dma("pool", "wo", WQ[:, c, :], self.wq[l, c * 128:(c + 1) * 128, :], r=[], w=["B0", "B1", "B2", "B3"])
        KT = self.VT[:].rearrange("p a b -> p (a b)").rearrange("p (h n) -> p h n", h=16)
        P.dma("pool", "kt", KT, self.keysT[l].rearrange("h d n -> d h n"), r=[], w=["VT"])
        QPT = self.KET[:].rearrange("p a b c -> p (a b c)").rearrange("p (h t) -> p h t", h=16)
        GW2 = self.FA[:, 3, 0:4096].bitcast(BF16).rearrange("p (c n) -> p c n", c=8)
        for c in range(8):
            P.dma("pool", "gw2", GW2[:, c, :], self.ple_gw[l, c * 128:(c + 1) * 128, :], r=[], w=["F3"])
        PW = self.FA[:, 2, 0:1024].bitcast(BF16).rearrange("p (c n) -> p c n", c=2)
        for c in range(2):
            P.dma("pool", "gw2", PW[:, c, :], self.ple_w[l, c * 128:(c + 1) * 128, :], r=[], w=["F2"])
        G2 = self.FA[:, 2, 1024:2048]; B2 = self.FA[:, 2, 2048:3072]; GB = self.FA[:, 2, 3072:4096]
        P.dma("sp", "lnp", G2, self.ln2_g[l:l + 1, :].partition_broadcast(128), r=[], w=["F2"])
        P.dma("sp", "lnp", B2, self.ln2_b[l:l + 1, :].partition_broadcast(128), r=[], w=["F2"])
        P.dma("sp", "lnp", GB, self.ple_gb[l:l + 1, :].partition_broadcast(128), r=[], w=["F2"])
        gsa = self.FA[:, 0, 30:4126].bitcast(BF16)
        gsb = self.FA[:, 1, 1024:2048].bitcast(BF16)
        GS = [gsa[:, k * 1024:(k + 1) * 1024] for k in range(8)] + [gsb[:, k * 1024:(k + 1) * 1024] for k in range(2)]
        NGS = len(GS)
        JUNK = self.FA[:, 1, 0:1024]
        X1 = self.FA[:, 1, 2048:3072]; ACC = self.FA[:, 1, 3072:4096]
        PK = self.PK
        SV = PK[:, 0:256].rearrange("p (h k) -> p h k", h=16)
        SIf = PK[:, 256:512].rearrange("p (h k) -> p h k", h=16)
        CV = PK[:, 512:640].rearrange("p (h k) -> p h k", h=8)
        CA = PK[:, 640:768].rearrange("p (h k) -> p h k", h=8)
        CBf = PK[:, 768:896].rearrange("p (h k) -> p h k", h=8)
        I1 = PK[:, 896:1024]; I2 = PK[:, 1024:1152]
        GATE = PK[:, 1152:1280]; H = PK[:, 1280:1408]; W = PK[:, 1408:1536]
        SI = self.PT[1][:].bitcast(U32).rearrange("p (h k) -> p h k", h=16)
        CI = self.PT[2][:].bitcast(U32)[:, 0:128].rearrange("p (h k) -> p h k", h=8)
        CI2 = self.PT[2][:].bitcast(U32)[:, 128:256].rearrange("p (h k) -> p h k", h=8)
        iota = self.CF[:, CF_IOTA:CF_IOTA + 16]
        pu_flat = self.ub
        pv_flat = self.vb
        NEG = -1.0e30

        def top16(vals, wk, sv_out, si_out, rd, wkn):
            P.op("dve", lambda g_: g_.max(out=sv_out[:, 0:8], in_=vals), r=rd, w=["PK"])
            yield
            P.op("dve", lambda g_: g_.max_index(out=si_out[:, 0:8], in_max=sv_out[:, 0:8], in_values=vals),
                 r=rd + ["PK"], w=["PKI"])
            yield
            P.op("dve", lambda g_: g_.match_replace(out=wk, in_to_replace=sv_out[:, 0:8], in_values=vals,
                                                   imm_value=NEG), r=rd + ["PK"], w=[wkn])
            yield
            P.op("dve", lambda g_: g_.max(out=sv_out[:, 8:16], in_=wk), r=[wkn], w=["PK"])
            yield
            P.op("dve", lambda g_: g_.max_index(out=si_out[:, 8:16], in_max=sv_out[:, 8:16], in_values=wk),
                 r=[wkn, "PK"], w=["PKI"])
            yield

        def topk_gen(i, eb):
            ts_ = slice(i * 128, (i + 1) * 128)
            EIDX = self.EIDX[:, eb, :]
            en = f"EIDX{eb}"
            for q4 in range(4):
                bk = self.psn(allb)
                for j in range(4):
                    hc = 4 * q4 + j
                    for c in range(8):
                        P.mm(self.PS[bk][:, j * 128:(j + 1) * 128], WQ[:, c, hc * 128:(hc + 1) * 128], self.XT[:, c, ts_],
                             start=(c == 0), stop=(c == 7), r=["B0", "XT"], w=[f"ps{bk}"])
                P.cp("act", QPT[:, 4 * q4:4 * q4 + 4, :], self.PS[bk][:].rearrange("p (j t) -> p j t", j=4),
                     r=[f"ps{bk}"], w=["KET"])
            for q4 in range(4):
                bk = self.psn(allb)
                for j in range(4):
                    hc = 4 * q4 + j
                    P.mm(self.PS[bk][:, j * 128:(j + 1) * 128], QPT[:, hc, :], KT[:, hc, :], start=True, stop=True,
                         r=["KET", "VT"], w=[f"ps{bk}"])
                sc = self.T5[q4 % 2]; scn = f"T5_{q4 % 2}"
                wk = self.T5[2 + q4 % 2]; wkn = f"T5_{2 + q4 % 2}"
                P.cp("act", sc[:], self.PS[bk][:], r=[f"ps{bk}"], w=[scn])
                for j in range(4):
                    hc = 4 * q4 + j
                    yield from top16(sc[:, j * 128:(j + 1) * 128], wk[:, j * 128:(j + 1) * 128], SV[:, hc, :],
                                     SI[:, hc, :], [scn], wkn)
            for h in range(8):
                cd = self.T5[4]; cdn = "T5_4"
                cand = cd[:, 0:256]; cwk = cd[:, 256:512]
                P.tt("dve", cand.rearrange("p (a b) -> p a b", a=16), apx(SV[:, 2 * h, :], [[1, 16], [0, 16]]),
                     apx(SV[:, 2 * h + 1, :], [[0, 16], [1, 16]]), ALU.add, r=["PK"], w=[cdn])
                yield
                yield from top16(cand, cwk, CV[:, h, :], CI[:, h, :], [cdn], cdn)
            P.op("dve", lambda g_: g_.tensor_single_scalar(out=CI2[:], in_=CI[:], scalar=4, op=ALU.logical_shift_right),
                 r=["PKI"], w=["PKI2"])
            yield
            P.cp("dve", CA[:], CI2[:], r=["PKI2"], w=["PKA"])
            yield
            P.op("dve", lambda g_: g_.tensor_single_scalar(out=CI2[:], in_=CI[:], scalar=15, op=ALU.bitwise_and),
                 r=["PKI", "PKA"], w=["PKI2"])
            yield
            P.cp("dve", CBf[:], CI2[:], r=["PKI2"], w=["PKA"])
            yield
            P.cp("dve", SIf[:], SI[:], r=["PKI"], w=["PKA"])
            yield
            for h in range(8):
                for c, (src, dstI) in enumerate(((CA, I1), (CBf, I2))):
                    eq = self.T5[5][:, c * 256:(c + 1) * 256]; eqn = "T5_5"
                    eq3 = eq.rearrange("p (k a) -> p k a", k=16)
                    P.tt("dve", eq3, apx(src[:, h, :], [[1, 16], [0, 16]]), apx(iota, [[0, 16], [1, 16]]),
                         ALU.is_equal, r=["PKA", "CF"], w=[eqn])
                    yield
                    P.tt("dve", eq3, eq3, apx(SIf[:, 2 * h + c, :], [[0, 16], [1, 16]]), ALU.mult, r=[eqn, "PKA"], w=[eqn])
                    yield
                    P.op("dve", lambda g_, eq3=eq3, dstI=dstI, h=h: g_.tensor_reduce(
                        out=dstI[:, h * 16:(h + 1) * 16], in_=eq3, axis=AX.X, op=ALU.add), r=[eqn], w=["PKB"])
                    yield
            P.ts("dve", I1, I1, 128.0, 0.0, ALU.mult, ALU.add, r=["PKB"], w=["PKB"])
            yield
            P.tt("dve", I1, I1, I2, ALU.add, r=["PKB"], w=["PKB"])
            yield
            P.cp("dve", EIDX, I1, r=["PKB"], w=[en])
            yield
            G3 = GATE.rearrange("p (h k) -> p h k", h=8)
            P.tt("dve", G3, CV, apx(CV[:, 0, 0:1], [[16, 8], [0, 16]]), ALU.subtract, r=["PK"], w=["GATE"])
            yield
            P.act(GATE, GATE, AF.Exp, r=["GATE"], w=["GATE"])
            zs = self.SM[:, 16:24]
            P.op("dve", lambda g_, G3=G3: g_.tensor_reduce(out=zs, in_=G3, axis=AX.X, op=ALU.add), r=["GATE"], w=["SM2"])
            yield
            P.op("dve", lambda g_: g_.reciprocal(out=zs, in_=zs), r=["SM2"], w=["SM2"])
            yield
            P.tt("dve", G3, G3, apx(zs[:, 0:1], [[1, 8], [0, 16]]), ALU.mult, r=["GATE", "SM2"], w=["GATE"])
            yield

        def gather(table, tn, eb, k):
            sl = self.gs_rot % NGS
            self.gs_rot += 1
            P.kb.add("pool", lambda g_: g_.indirect_dma_start(
                out=GS[sl], out_offset=None, in_=table,
                in_offset=bass.IndirectOffsetOnAxis(ap=self.EIDX[:, eb, k:k + 1], axis=0)),
                self._b([f"EIDX{eb}", tn]), self._b([f"GS{sl}"]), dma=f"gs{sl}")
            return GS[sl], f"GS{sl}"

        nt = PEER_TILES
        for _ in topk_gen(0, 0):
            pass
        for i in range(nt):
            ts_ = slice(i * 128, (i + 1) * 128)
            eb = i % 2
            nxt = topk_gen(i + 1, 1 - eb) if i + 1 < nt else None
            P.dma("sp", "x1r", X1, self.x1d[ts_, :], r=["x1d"], w=["X1"])
            for k in range(128):
                g, gn = gather(pu_flat, "UB", eb, k)
                P.stt(JUNK, g, 1.0, X1, ALU.mult, ALU.mult, r=[gn, "X1"], w=(["H"] if k in (0, 127) else []),
                      accum_out=H[:, k:k + 1])
            P.tt("dve", W, H, H, ALU.mult, r=["H"], w=["W"])
            P.ts("dve", W, W, 0.044715, 1.0, ALU.mult, ALU.add, r=["W"], w=["W"])
            P.tt("dve", W, W, H, ALU.mult, r=["W", "H"], w=["W"])
            P.act(W, W, AF.Sigmoid, r=["W"], w=["W"], scale=1.5957691216057308)
            P.tt("dve", W, W, H, ALU.mult, r=["W", "H"], w=["W"])
            P.tt("dve", W, W, GATE, ALU.mult, r=["W", "GATE"], w=["W"])
            for k in range(128):
                g, gn = gather(pv_flat, "VB", eb, k)
                dgs = k % 4
                dg = self.XBT[1][:, 256 + dgs * 128:256 + (dgs + 1) * 128]
                P.act(dg, self.identb, AF.Copy, r=["CB", "W"], w=[f"DG{dgs}"], scale=W[:, k:k + 1])
                for half in range(2):
                    P.mm(self.PS[6 + half][:], dg, g[:, half * 512:(half + 1) * 512], start=(k == 0), stop=(k == 127),
                         r=[f"DG{dgs}", gn], w=[f"ps{6 + half}"])
                if nxt is not None:
                    for _ in range(2):
                        if next(nxt, "end") == "end":
                            nxt = None
                            break
            if nxt is not None:
                for _ in nxt:
                    pass
            for half in range(2):
                hs = slice(half * 512, (half + 1) * 512)
                P.stt(ACC[:, hs], X1[:, hs], ALPHA, self.PS[6 + half][:], ALU.mult, ALU.add,
                      r=["X1", f"ps{6 + half}"], w=["ACC"])
            xb = self.XBT[0]
            P.cp("act", xb[:], ACC, r=["ACC"], w=["XBT0"])
            bk = self.psn(allb)
            psb = self.PS[bk][:].bitcast(BF16)
            for c in range(8):
                P.tr(psb[:, c * 128:(c + 1) * 128], xb[:, c * 128:(c + 1) * 128], self.identb, r=["XBT0", "CB"], w=[f"ps{bk}"])
            RTB = self.T5[5][:].bitcast(BF16)
            RT = RTB.rearrange("p (c t) -> p c t", c=8)
            P.cp("act", RTB, psb, r=[f"ps{bk}"], w=["T5_5"])
            pb16 = self.XBT[1]
            P.dma("pool", "pld", pb16[:, 0:256], self.p_in[l, ts_, :], r=[], w=["XBT1"])
            bk2 = self.psn(allb)
            psb2 = self.PS[bk2][:].bitcast(BF16)
            for c in range(2):
                P.tr(psb2[:, c * 128:(c + 1) * 128], pb16[:, c * 128:(c + 1) * 128], self.identb, r=["XBT1", "CB"], w=[f"ps{bk2}"])
            PTT = self.PT[0][:, 0:256].rearrange("p (c t) -> p c t", c=2)
            P.cp("act", self.PT[0][:, 0:256], psb2[:, 0:256], r=[f"ps{bk2}"], w=["PT0"])
            for half in range(2):
                hs = slice(half * 512, (half + 1) * 512)
                pg = self.psn(allb)
                for c in range(8):
                    P.mm(self.PS[pg][:], RT[:, c, :], GW2[:, c, hs], start=(c == 0), stop=(c == 7),
                         r=["T5_5", "F3"], w=[f"ps{pg}"])
                pp = self.psn(allb)
                for c in range(2):
                    P.mm(self.PS[pp][:], PTT[:, c, :], PW[:, c, hs], start=(c == 0), stop=(c == 1),
                         r=["PT0", "F2"], w=[f"ps{pp}"])
                t = self.T5[half]; tn = f"T5_{half}"
                P.tt("dve", t[:], self.PS[pg][:], GB[:, hs], ALU.add, r=[f"ps{pg}", "F2"], w=[tn])
                P.act(t[:], t[:], AF.Sigmoid, r=[tn], w=[tn])
                P.tt("dve", t[:], t[:], self.PS[pp][:], ALU.mult, r=[tn, f"ps{pp}"], w=[tn])
                P.tt("dve", ACC[:, hs], ACC[:, hs], t[:], ALU.add, r=[tn, "ACC"], w=["ACC"])
            self.layernorm_tile(ACC, "ACC", G2, B2, "F2")
            P.dma("sp", "xw", dst[ts_, :], ACC, r=["ACC"], w=[dstn])


def host_tables(inp):
    L = DEPTH
    chanp = np.zeros((L, 128, CP_N), np.float32)
    for j in range(2):
        chanp[:, :, CP_CB + j] = inp["conv_b"][:, j * 128:(j + 1) * 128]
        chanp[:, :, CP_LG + j] = inp["conv_ln_g"][:, j * 128:(j + 1) * 128]
        chanp[:, :, CP_LB + j] = inp["conv_ln_b"][:, j * 128:(j + 1) * 128]
        chanp[:, :, CP_CW + j * 31:CP_CW + (j + 1) * 31] = np.transpose(
            inp["conv_w"][:, :, j * 128:(j + 1) * 128], (0, 2, 1))
    for h in range(6):
        chanp[:, 0:32, CP_GB + h] = inp["gla_gate_b"][:, h * 32:(h + 1) * 32]
        chanp[:, 0:64, CP_NG + h] = inp["gla_norm_g"][:, h * 64:(h + 1) * 64]
        chanp[:, :, CP_FB + h] = inp["fox_forget_b"][:, h][:, None]
    keysT = np.ascontiguousarray(
        np.transpose(np.asarray(inp["peer_keys"]).reshape(L, 16, 128, 128), (0, 1, 3, 2)))
    return chanp, keysT


SHARED = ["w_in", "gla_gate_w", "w_out", "ln1_g", "ln1_b", "peer_wq", "peer_u", "peer_v",
          "ple_w", "ple_gw", "ple_gb", "ln2_g", "ln2_b"]


def make_in_maps(inp, cores):
    inp = {k: np.asarray(v) for k, v in inp.items()}
    chanp, keysT = host_tables(inp)
    cf, cb = host_consts()
    shared = {k: np.ascontiguousarray(inp[k], dtype=np.float32) for k in SHARED}
    shared.update(chanp=chanp, keysT=keysT, cf32=cf, cb16=cb)
    maps = []
    for c in cores:
        m = dict(shared)
        m["x"] = np.ascontiguousarray(inp["x"][c])
        m["p"] = np.ascontiguousarray(inp["p"][:, c])
        maps.append(m)
    return maps


_PROG = None


def kernel(**inputs):
    global _PROG
    if _PROG is None:
        _PROG = Program()
        _PROG.build()
    maps = make_in_maps(inputs, list(range(8)))
    res = run_bass_kernel_spmd(_PROG.nc, maps, core_ids=list(range(8)))
    return np.stack([np.asarray(r["y"]) for r in res.results], axis=0).astype(np.float32)
```

```python
import numpy as np
import ml_dtypes
from contextlib import ExitStack
import concourse.bass as bass
import concourse.mybir as mybir
from concourse.bass_utils import run_bass_kernel_spmd

F32 = mybir.dt.float32
BF16 = mybir.dt.bfloat16
U32 = mybir.dt.uint32
I32 = mybir.dt.int32
AF = mybir.ActivationFunctionType
ALU = mybir.AluOpType
AX = mybir.AxisListType

D = 1024
S = 4096
NT = 32
DEPTH = 4
IN_COLS = 2838
ALPHA = (2.0 * DEPTH) ** 0.25
EPS = 1e-5
C_AVAL, C_AGATE, C_BQ, C_BK, C_BV, C_BG, C_BLR, C_CQ, C_CK, C_CV, C_CF = (
    0, 256, 512, 704, 896, 1280, 1664, 1680, 2064, 2448, 2832)


class Op:
    __slots__ = ("eng", "fn", "deps", "stream", "needed", "val", "isdma", "seq")


class Buf:
    __slots__ = ("name", "w", "r")

    def __init__(self, name):
        self.name = name
        self.w = {}
        self.r = {}


class KB:
    def __init__(self, nc, es):
        self.nc = nc
        self.es = es
        self.ops = []
        self.eng = {"pe": nc.tensor, "act": nc.scalar, "dve": nc.vector,
                    "pool": nc.gpsimd, "sp": nc.sync}
        self.sems = {}
        self.nsem = 0
        self.seqs = {}

    def sem(self, name):
        if name not in self.sems:
            self.sems[name] = self.es.enter_context(self.nc.semaphore("s_" + name))
            self.nsem += 1
        return self.sems[name]

    def add(self, eng, fn, reads=(), writes=(), dma=None):
        op = Op()
        op.eng = eng
        op.fn = fn
        op.isdma = dma is not None
        op.stream = ("d_" + dma) if dma is not None else eng
        op.needed = False
        op.val = 0
        deps = set()
        for b in reads:
            deps.update(b.w.values())
        for b in writes:
            deps.update(b.w.values())
            deps.update(b.r.values())
        op.seq = self.seqs.get(eng, 0)
        if fn is not None:
            self.seqs[eng] = op.seq + 1
        op.deps = [d for d in deps if not (eng == "pe" and d.stream == "pe")
                   and not (eng in ("dve", "act") and d.stream == eng and op.seq - d.seq >= 2)]
        for d in op.deps:
            d.needed = True
        for b in reads:
            b.r[op.stream] = op
        for b in writes:
            b.w = {op.stream: op}
            b.r = {}
        self.ops.append(op)
        return op

    def emit(self):
        counters = {}
        for op in self.ops:
            if op.needed or op.isdma:
                inc = 16 if op.isdma else 1
                counters[op.stream] = counters.get(op.stream, 0) + inc
                op.val = counters[op.stream]
        for st in counters:
            self.sem(st)
        seen = {e: {} for e in self.eng}
        n = 0
        for op in self.ops:
            e = self.eng[op.eng]
            need = {}
            for d in op.deps:
                if d.val > need.get(d.stream, 0):
                    need[d.stream] = d.val
            sn = seen[op.eng]
            for st, v in need.items():
                if sn.get(st, 0) < v:
                    e.wait_ge(self.sems[st], v)
                    sn[st] = v
                    n += 1
            if op.fn is not None:
                ins = op.fn(e)
                n += 1
                if op.needed or op.isdma:
                    ins.then_inc(self.sems[op.stream], 16 if op.isdma else 1)
        return n


def apx(base, dims):
    return bass.AP(base.tensor, base.offset, [list(base.ap[0])] + [list(d) for d in dims])


class Prog:
    def __init__(self, n_layers=DEPTH, dbg=None):
        self.n_layers = n_layers
        self.dbg = dbg or {}
        self.nc = bass.Bass("TRN2", target_bir_lowering=False)
        self.es = ExitStack()
        self.kb = KB(self.nc, self.es)
        self.bufs = {}

    def dram_in(self, name, shape, dt=F32):
        return self.nc.dram_tensor(name, list(shape), dt, kind="ExternalInput").ap()

    def dram_out(self, name, shape, dt=F32):
        return self.nc.dram_tensor(name, list(shape), dt, kind="ExternalOutput").ap()

    def dram_tmp(self, name, shape, dt=F32):
        return self.nc.dram_tensor(name, list(shape), dt, kind="Internal").ap()

    def sb(self, name, shape, dt=F32):
        return self.es.enter_context(self.nc.sbuf_tensor(name, list(shape), dt))

    def B(self, name):
        if name not in self.bufs:
            self.bufs[name] = Buf(name)
        return self.bufs[name]

    def _b(self, xs):
        return [self.B(x) if isinstance(x, str) else x for x in xs]

    def op(self, eng, fn, r=(), w=()):
        return self.kb.add(eng, fn, self._b(r), self._b(w))

    def dma(self, q, sem, out, in_, r=(), w=(), **kw):
        e = {"sp": "sp", "pool": "pool", "act": "act"}[q]
        return self.kb.add(e, lambda g: g.dma_start(out=out, in_=in_, **kw),
                           self._b(r), self._b(w), dma=sem)

    def mm(self, out, lhsT, rhs, start, stop, r=(), w=()):
        return self.op("pe", lambda g: g.matmul(out, lhsT, rhs, start=start, stop=stop), r, w)

    def tr(self, out, in_, ident, r=(), w=()):
        return self.op("pe", lambda g: g.transpose(out, in_, ident), r, w)

    def act(self, out, in_, func, r=(), w=(), **kw):
        return self.op("act", lambda g: g.activation(out=out, in_=in_, func=func, **kw), r, w)

    def tt(self, eng, out, in0, in1, op, r=(), w=()):
        return self.op(eng, lambda g: g.tensor_tensor(out=out, in0=in0, in1=in1, op=op), r, w)

    def ts(self, eng, out, in0, s1, s2, op0, op1=None, r=(), w=(), **kw):
        if op1 is None:
            return self.op(eng, lambda g: g.tensor_scalar(out=out, in0=in0, scalar1=s1, scalar2=None,
                                                          op0=op0, **kw), r, w)
        return self.op(eng, lambda g: g.tensor_scalar(out=out, in0=in0, scalar1=s1, scalar2=s2,
                                                      op0=op0, op1=op1, **kw), r, w)

    def stt(self, out, in0, scalar, in1, op0, op1, r=(), w=(), **kw):
        return self.op("dve", lambda g: g.scalar_tensor_tensor(out=out, in0=in0, scalar=scalar, in1=in1,
                                                               op0=op0, op1=op1, **kw), r, w)

    def cp(self, eng, out, in_, r=(), w=()):
        if eng == "act":
            return self.op("act", lambda g: g.copy(out=out, in_=in_), r, w)
        return self.op(eng, lambda g: g.tensor_copy(out=out, in_=in_), r, w)


CP_CB, CP_LG, CP_LB, CP_CW, CP_GB, CP_NG, CP_FB, CP_N = 0, 2, 4, 6, 68, 74, 80, 86
CF_IDENT, CF_ONES, CF_SCM, CF_IOTA, CF_E0, CF_E1, CF_M0, CF_M1, CF_N = 0, 128, 256, 768, 784, 785, 786, 787, 788
CB_IDENT, CB_CMASK, CB_GMASK, CB_ONES, CB_N = 0, 128, 256, 384, 512


def host_consts():
    cf = np.zeros((128, CF_N), np.float32)
    cf[:, CF_IDENT:CF_IDENT + 128] = np.eye(128, dtype=np.float32)
    cf[:, CF_ONES:CF_ONES + 128] = 1.0
    scm = np.ones((512,), np.float32)
    scm[::64] = 0.0
    cf[:, CF_SCM:CF_SCM + 512] = scm[None, :]
    cf[:, CF_IOTA:CF_IOTA + 16] = np.arange(16, dtype=np.float32)[None, :]
    cf[64, CF_E0] = 1.0
    cf[65, CF_E1] = 1.0
    cf[0:64, CF_M0] = 1.0
    cf[64:128, CF_M1] = 1.0
    cb = np.zeros((128, CB_N), np.float32)
    cb[:, CB_IDENT:CB_IDENT + 128] = np.eye(128)
    s = np.arange(128)[:, None]
    t = np.arange(128)[None, :]
    cb[:, CB_CMASK:CB_CMASK + 128] = (t >= s)
    cb[:, CB_GMASK:CB_GMASK + 128] = (t >= s) & ((t // 64) == (s // 64))
    cb[:, CB_ONES:CB_ONES + 128] = 1.0
    return cf, cb.astype(ml_dtypes.bfloat16)


GLA_STAGE = 99
PEER_STAGE = 99
PEER_TILES = NT


class Program(Prog):
    def build(self):
        nc = self.nc
        P = self
        L = DEPTH
        self.x_in = P.dram_in("x", [S, D])
        self.p_in = P.dram_in("p", [L, S, 256])
        self.w_in = P.dram_in("w_in", [L, D, IN_COLS])
        self.chanp_d = P.dram_in("chanp", [L, 128, CP_N])
        self.gatew_d = P.dram_in("gla_gate_w", [L, 16, 192])
        self.w_out = P.dram_in("w_out", [L, D, D])
        self.ln1_g = P.dram_in("ln1_g", [L, D]); self.ln1_b = P.dram_in("ln1_b", [L, D])
        self.wq = P.dram_in("peer_wq", [L, D, 2048])
        self.keysT = P.dram_in("keysT", [L, 16, 128, 128])
        self.pu = P.dram_in("peer_u", [L, 16384, D])
        self.pv = P.dram_in("peer_v", [L, 16384, D])
        self.ple_w = P.dram_in("ple_w", [L, 256, D])
        self.ple_gw = P.dram_in("ple_gw", [L, D, D])
        self.ple_gb = P.dram_in("ple_gb", [L, D])
        self.ln2_g = P.dram_in("ln2_g", [L, D]); self.ln2_b = P.dram_in("ln2_b", [L, D])
        self.cf_d = P.dram_in("cf32", [128, CF_N])
        self.cb_d = P.dram_in("cb16", [128, CB_N], BF16)
        self.y = P.dram_out("y", [S, D])
        self.xcur = P.dram_tmp("xcur", [S, D])
        self.x1d = P.dram_tmp("x1d", [S, D])
        self.mixT = P.dram_tmp("mixT", [D, S], BF16)
        self.ub = P.dram_tmp("ub", [16384, D], BF16)
        self.vb = P.dram_tmp("vb", [16384, D], BF16)
        self.dbg_out = {}
        for name, (shape, dt, attr, bufs) in self.dbg.items():
            self.dbg_out[name] = P.dram_out("dbg_" + name, shape, dt)
        self.XT = P.sb("XT", [128, 8, S], BF16)
        self.FA = P.sb("FA", [128, 4, 4128], F32)
        self.BA = P.sb("BA", [128, 4, S], BF16)
        self.WB = [P.sb("WB0", [128, 8, 256], BF16), P.sb("WB1", [128, 8, 256], BF16)]
        self.CF = P.sb("CF", [128, CF_N], F32)
        self.CB = P.sb("CB", [128, CB_N], BF16)
        self.CHP = P.sb("CHP", [128, CP_N], F32)
        self.XBT = [P.sb("XBT0", [128, 1024], BF16), P.sb("XBT1", [128, 1024], BF16)]
        self.T5 = [P.sb(f"T5_{i}", [128, 512], F32) for i in range(6)]
        self.VT = P.sb("VT", [128, NT, 64], BF16)
        self.KET = P.sb("KET", [128, 2, NT, 32], BF16)
        self.CT = P.sb("CT", [128, NT], F32)
        self.PT = [P.sb(f"PT{i}", [128, 512], BF16) for i in range(3)]
        self.GW = P.sb("GW", [16, 192], BF16)
        self.SM = P.sb("SM", [128, 64], F32)
        self.PK = P.sb("PK", [128, 1536], F32)
        self.EIDX = P.sb("EIDX", [128, 2, 128], U32)
        self.gs_rot = 0
        self.PS = [self.es.enter_context(nc.psum_tensor(f"ps{i}", [128, 512], F32)) for i in range(8)]
        self.ps_rot = 0
        self.identb = self.CB[:, CB_IDENT:CB_IDENT + 128]
        self.identf = self.CF[:, CF_IDENT:CF_IDENT + 128]
        self.onesf = self.CF[:, CF_ONES:CF_ONES + 128]
        self.onesb = self.CB[:, CB_ONES:CB_ONES + 128]
        P.dma("sp", "cst", self.CF[:], self.cf_d, w=["CF"])
        P.dma("sp", "cst", self.CB[:], self.cb_d, w=["CB"])
        P.op("dve", lambda g: g.memset(self.FA[:, 0, 0:30], 0.0), w=["F0"])

        for l in range(self.n_layers):
            self.layer(l)

        fin = []
        for name, (shape, dt, attr, bufs) in self.dbg.items():
            P.dma("sp", "dbg", self.dbg_out[name], getattr(self, attr), r=bufs, w=["dbg_" + name])
            fin.append("dbg_" + name)
        fin.append("y")
        P.op("sp", None, r=fin)
        n = self.kb.emit()
        return n

    def convert_tables(self, l):
        for (src, dstt, nm) in ((self.pu, self.ub, "UB"), (self.pv, self.vb, "VB")):
            for j in range(16):
                self.dma("pool", "cv" + nm, dstt[j * 1024:(j + 1) * 1024, :], src[l, j * 1024:(j + 1) * 1024, :],
                         r=[], w=[nm])

    def psn(self, banks):
        b = banks[self.ps_rot % len(banks)]
        self.ps_rot += 1
        return b

    def F(self, i, lo=0, hi=4096):
        return self.FA[:, i, lo:hi]

    def layer(self, l):
        P = self
        src = self.x_in if l == 0 else self.xcur
        P.dma("sp", "chp", self.CHP[:], self.chanp_d[l], r=[], w=["CHP"])
        P.dma("pool", "gw", self.GW[:], self.gatew_d[l], r=[], w=["GW"])
        self.convert_tables(l)
        self.phase_xT(src, "xsrc")
        self.unit_conv(l)
        self.lrt_done = False
        for h in range(6):
            self.unit_gla(l, h)
        for h in range(6):
            self.unit_fox(l, h)
        self.phase_out(l, src)
        self.phase_peer(l)

    def phase_xT(self, src, srcname):
        P = self
        for i in range(NT):
            s = i % 2
            xb = self.XBT[s]
            P.dma("pool", f"xbt{s}", xb[:], src[i * 128:(i + 1) * 128, :], r=[srcname], w=[f"XBT{s}"])
            self.transpose_to_XT(xb, f"XBT{s}", i)

    def transpose_to_XT(self, xb, xbname, i):
        P = self
        bank = self.psn([0, 1])
        psb = self.PS[bank][:].bitcast(BF16)
        for c in range(8):
            P.tr(psb[:, c * 128:(c + 1) * 128], xb[:, c * 128:(c + 1) * 128], self.identb,
                 r=[xbname, "CB"], w=[f"ps{bank}"])
        P.cp("act" if i % 2 else "dve", self.XT[:, :, i * 128:(i + 1) * 128],
             psb.rearrange("p (c t) -> p c t", c=8), r=[f"ps{bank}"], w=["XT"])

    def load_w(self, slot, l, cols):
        off = 0
        for (c0, n) in cols:
            kw = dict(allow_slow_non_contiguous=True) if n == 1 else {}
            self.dma("pool", f"wb{slot}", self.WB[slot][:, :, off:off + n],
                     self.w_in[l, :, c0:c0 + n].rearrange("(c p) n -> p c n", p=128), r=[], w=[f"WB{slot}"], **kw)
            off += n

    def proj_fm(self, bank, slot, woff, m, g, prow=0):
        for c in range(8):
            self.mm(self.PS[bank][prow:prow + m, :], self.WB[slot][:, c, woff:woff + m],
                    self.XT[:, c, g * 512:(g + 1) * 512], start=(c == 0), stop=(c == 7),
                    r=[f"WB{slot}", "XT"], w=[f"ps{bank}"])

    def proj_v_tm(self, slot, woff, g, banks):
        bank = self.psn(banks)
        for j in range(4):
            i = 4 * g + j
            for c in range(8):
                self.mm(self.PS[bank][:, j * 64:(j + 1) * 64], self.XT[:, c, i * 128:(i + 1) * 128],
                        self.WB[slot][:, c, woff:woff + 64], start=(c == 0), stop=(c == 7),
                        r=[f"WB{slot}", "XT"], w=[f"ps{bank}"])
        self.cp("act", self.VT[:, 4 * g:4 * g + 4, :],
                self.PS[bank][:, 0:256].rearrange("p (j e) -> p j e", j=4), r=[f"ps{bank}"], w=["VT"])

    def unit_conv(self, l):
        P = self
        UP = self.FA[:, 0, :]
        for j in range(2):
            slot = j
            self.load_w(slot, l, [(C_AVAL + j * 128, 128), (C_AGATE + j * 128, 128)])
            for g in range(8):
                a = self.psn([2, 3, 4, 5]); b = self.psn([2, 3, 4, 5])
                self.proj_fm(a, slot, 0, 128, g)
                self.proj_fm(b, slot, 128, 128, g)
                t = self.T5[g % 2]
                P.act(t[:], self.PS[b][:], AF.Sigmoid, r=[f"ps{b}"], w=[f"T5_{g % 2}"])
                P.tt("dve", UP[:, 30 + g * 512:30 + (g + 1) * 512], self.PS[a][:], t[:], ALU.mult,
                     r=[f"ps{a}", f"T5_{g % 2}"], w=["F0"])
            acc = self.F(1 + j)
            cw = self.CHP[:, CP_CW + j * 31:CP_CW + (j + 1) * 31]
            P.ts("dve", acc, UP[:, 0:4096], cw[:, 0:1], self.CHP[:, CP_CB + j:CP_CB + j + 1],
                 ALU.mult, ALU.add, r=["F0", "CHP"], w=[f"F{1 + j}"])
            for k in range(1, 31):
                P.stt(acc, UP[:, k:k + 4096], cw[:, k:k + 1], acc, ALU.mult, ALU.add,
                      r=["F0", "CHP", f"F{1 + j}"], w=[f"F{1 + j}"])
        for g in range(8):
            gs = slice(g * 512, (g + 1) * 512)
            pm = self.psn([2, 3, 4, 5]); pe = self.psn([2, 3, 4, 5])
            for j in range(2):
                P.mm(self.PS[pm][:], self.onesf, self.F(1 + j)[:, gs], start=(j == 0), stop=(j == 1),
                     r=["CF", f"F{1 + j}"], w=[f"ps{pm}"])
            for j in range(2):
                sq = self.T5[j]
                P.act(sq[:], self.F(1 + j)[:, gs], AF.Square, r=[f"F{1 + j}"], w=[f"T5_{j}"])
                P.mm(self.PS[pe][:], self.onesf, sq[:], start=(j == 0), stop=(j == 1),
                     r=["CF", f"T5_{j}"], w=[f"ps{pe}"])
            mean = self.T5[2]; msq = self.T5[3]; var = self.T5[4]
            P.act(mean[:], self.PS[pm][:], AF.Copy, r=[f"ps{pm}"], w=["T5_2"], scale=1.0 / 256)
            P.act(msq[:], mean[:], AF.Square, r=["T5_2"], w=["T5_3"])
            P.stt(var[:], self.PS[pe][:], 1.0 / 256, msq[:], ALU.mult, ALU.subtract, r=[f"ps{pe}", "T5_3"], w=["T5_4"])
            P.ts("dve", var[:], var[:], 0.0, EPS, ALU.max, ALU.add, r=["T5_4"], w=["T5_4"])
            P.act(var[:], var[:], AF.Sqrt, r=["T5_4"], w=["T5_4"])
            P.op("dve", lambda g_, v=var: g_.reciprocal(out=v[:], in_=v[:]), r=["T5_4"], w=["T5_4"])
            for j in range(2):
                t = self.T5[j]
                P.tt("dve", t[:], self.F(1 + j)[:, gs], mean[:], ALU.subtract, r=[f"F{1 + j}", "T5_2"], w=[f"T5_{j}"])
                P.tt("dve", t[:], t[:], var[:], ALU.mult, r=[f"T5_{j}", "T5_4"], w=[f"T5_{j}"])
                P.act(self.BA[:, j, gs], t[:], AF.Silu, r=[f"T5_{j}", "CHP"], w=[f"B{j}"],
                      scale=self.CHP[:, CP_LG + j:CP_LG + j + 1], bias=self.CHP[:, CP_LB + j:CP_LB + j + 1])
        for j in range(2):
            P.dma("sp", f"mixw{j}", self.mixT[j * 128:(j + 1) * 128, :], self.BA[:, j, :], r=[f"B{j}"], w=["mixT"])

    def gla_end(self, h):
        self.dma("sp", f"mixw{h % 2}", self.mixT[256 + h * 64:320 + h * 64, :], self.BA[0:64, 2, :], r=["B2"], w=["mixT"])

    def unit_gla(self, l, h):
        P = self
        slot = h % 2
        wn = f"WB{slot}"
        banks = [0, 1, 2, 3, 4, 5]
        QE = self.BA[:, 0, :]; KE = self.BA[:, 1, :]; AT = self.BA[:, 2, :]; SB = self.BA[:, 3, :]
        GZ = self.FA[:, 0, 30:4126]; EC = self.F(1); EN = self.F(2); ST = self.F(3)
        D1 = GZ; D0 = EN
        rq = slice(0, 32)
        self.load_w(slot, l, [(C_BQ + h * 32, 32), (C_BK + h * 32, 32), (C_BV + h * 64, 64), (C_BG + h * 64, 64),
                              (C_BLR, 16)])
        for g in range(8):
            gs = slice(g * 512, (g + 1) * 512)
            a0 = self.psn(banks)
            self.proj_fm(a0, slot, 192, 16, g)
            lrt = self.PT[g % 2]; lrn = f"PT{g % 2}"
            P.cp("act", lrt[0:16, :], self.PS[a0][0:16, :], r=[f"ps{a0}"], w=[lrn])
            a = self.psn(banks)
            P.mm(self.PS[a][rq, :], self.GW[0:16, h * 32:(h + 1) * 32], lrt[0:16, :], start=True, stop=True,
                 r=["GW", lrn], w=[f"ps{a}"])
            P.ts("dve", GZ[rq, gs], self.PS[a][rq, :], self.CHP[rq, CP_GB + h:CP_GB + h + 1], None, ALU.add,
                 r=[f"ps{a}", "CHP"], w=["F0"])
        P.act(GZ[rq, :], GZ[rq, :], AF.Exp, r=["F0"], w=["F0"], scale=-1.0)
        P.ts("dve", GZ[rq, :], GZ[rq, :], 1.0, None, ALU.add, r=["F0"], w=["F0"])
        P.act(GZ[rq, :], GZ[rq, :], AF.Ln, r=["F0"], w=["F0"])
        scm = self.CF[rq, CF_SCM:CF_SCM + 512]
        for g in range(8):
            gs = slice(g * 512, (g + 1) * 512)
            P.op("dve", lambda g_, gs=gs: g_.tensor_tensor_scan(
                out=ST[rq, gs], data0=scm, data1=GZ[rq, gs], initial=0.0, op0=ALU.mult, op1=ALU.subtract),
                r=["F0", "CF"], w=["F3"])
        P.act(EC[rq, :], ST[rq, :], AF.Exp, r=["F3"], w=["F1"], scale=1.0 / 16)
        P.act(EN[rq, :], ST[rq, :], AF.Exp, r=["F3"], w=["F2"], scale=-1.0 / 16)
        if GLA_STAGE < 1:
            return
        for g in range(8):
            gs = slice(g * 512, (g + 1) * 512)
            a = self.psn(banks)
            self.proj_fm(a, slot, 0, 32, g)
            P.stt(QE[rq, gs], self.PS[a][rq, :], 32.0 ** -0.5, EC[rq, gs], ALU.mult, ALU.mult,
                  r=[f"ps{a}", "F1"], w=["B0"])
            b = self.psn(banks)
            self.proj_fm(b, slot, 32, 32, g)
            P.tt("dve", KE[rq, gs], self.PS[b][rq, :], EN[rq, gs], ALU.mult, r=[f"ps{b}", "F2"], w=["B1"])
            self.proj_v_tm(slot, 64, g, banks)
        if GLA_STAGE < 2:
            return
        pk = self.psn(banks)
        pkb = self.PS[pk][:].bitcast(BF16)
        for i in range(NT):
            P.tr(pkb[:, i * 32:(i + 1) * 32], KE[rq, i * 128:(i + 1) * 128], self.identb[0:32, 0:32],
                 r=["B1", "CB"], w=[f"ps{pk}"])
        for m in range(2):
            P.ts("dve", self.KET[:, m, :, :], pkb.rearrange("p (i d) -> p i d", i=NT),
                 self.CF[:, CF_M0 + m:CF_M0 + m + 1], None, ALU.mult, r=[f"ps{pk}", "CF"], w=["KET"])
        if GLA_STAGE < 3:
            return
        ATv = AT.rearrange("p (i t) -> p i t", i=NT)
        gmask4 = apx(self.CB[:, CB_GMASK:CB_GMASK + 128], [[0, 4], [1, 128]])
        for i4 in range(8):
            a = self.psn(banks)
            for j in range(4):
                i = 4 * i4 + j
                P.mm(self.PS[a][:, j * 128:(j + 1) * 128], KE[rq, i * 128:(i + 1) * 128], QE[rq, i * 128:(i + 1) * 128],
                     start=True, stop=True, r=["B0", "B1"], w=[f"ps{a}"])
            for j in range(4):
                if GLA_STAGE == 3.5:
                    break
                P.tt("dve", ATv[:, 4 * i4 + j, :], self.PS[a][:, j * 128:(j + 1) * 128],
                     self.CB[:, CB_GMASK:CB_GMASK + 128], ALU.mult, r=[f"ps{a}", "CB"], w=["B2"])
        if GLA_STAGE < 4:
            return self.gla_end(h)
        for c8 in range(8):
            a = self.psn(banks)
            for cc in range(8):
                c = c8 * 8 + cc
                i = c // 2; r0 = (c % 2) * 64
                P.mm(self.PS[a][rq, cc * 64:(cc + 1) * 64], self.KET[:, c % 2, i, :], self.VT[:, i, :],
                     start=True, stop=True, r=["KET", "VT"], w=[f"ps{a}"])
            ac = apx(EC[rq, 64 * (c8 * 8) + 63:64 * (c8 * 8) + 64], [[64, 8], [0, 64]])
            out = apx(D1[rq, c8 * 8:c8 * 8 + 1], [[1, 8], [64, 64]])
            P.tt("dve", out, self.PS[a][rq, :].rearrange("p (c e) -> p c e", c=8), ac, ALU.mult,
                 r=[f"ps{a}", "F1"], w=["F0"])
        if GLA_STAGE < 5:
            return
        P.cp("dve", D0[rq, :].rearrange("p (e c) -> p e c", e=64), apx(EC[rq, 63:64], [[0, 64], [64, 64]]),
             r=["F1"], w=["F2"])
        P.op("dve", lambda g_: g_.memset(apx(D0[rq, 0:1], [[64, 64]]), 0.0), w=["F2"])
        P.op("dve", lambda g_: g_.tensor_tensor_scan(out=ST[rq, :], data0=D0[rq, :], data1=D1[rq, :], initial=0.0,
                                                    op0=ALU.mult, op1=ALU.add), r=["F0", "F2"], w=["F3"])
        if GLA_STAGE < 6:
            return
        P.cp("act", SB[rq, :].rearrange("p (c e) -> p c e", c=64), apx(ST[rq, 0:1], [[1, 64], [64, 64]]),
             r=["F3"], w=["B3"])
        if GLA_STAGE < 7:
            return
        r0m = 256 + h * 64
        BO = KE
        for g in range(8):
            gs = slice(g * 512, (g + 1) * 512)
            po = self.psn(banks)
            for cc in range(8):
                c = g * 8 + cc
                i = c // 2; r0 = (c % 2) * 64
                cs = slice(cc * 64, (cc + 1) * 64)
                P.mm(self.PS[po][0:64, cs], self.VT[:, i, :], ATv[:, i, r0:r0 + 64],
                     start=True, stop=(c == 0), r=["VT", "B2"], w=[f"ps{po}"])
                if c > 0:
                    P.mm(self.PS[po][0:64, cs], SB[rq, (c - 1) * 64:c * 64], QE[rq, c * 64:(c + 1) * 64],
                         start=False, stop=True, r=["B3", "B0"], w=[f"ps{po}"])
            ot = self.T5[0]; sq = self.T5[1]; rst = self.T5[2]; sg = self.T5[3]
            P.act(ot[0:64, :], self.PS[po][0:64, :], AF.Copy, r=[f"ps{po}"], w=["T5_0"])
            P.act(sq[0:64, :], self.PS[po][0:64, :], AF.Square, r=[f"ps{po}"], w=["T5_1"])
            pm = self.psn(banks)
            P.mm(self.PS[pm][0:64, :], self.onesf[0:64, 0:64], sq[0:64, :], start=True, stop=True,
                 r=["CF", "T5_1"], w=[f"ps{pm}"])
            P.ts("dve", rst[0:64, :], self.PS[pm][0:64, :], 1.0 / 64, EPS, ALU.mult, ALU.add, r=[f"ps{pm}"], w=["T5_2"])
            P.act(rst[0:64, :], rst[0:64, :], AF.Sqrt, r=["T5_2"], w=["T5_2"])
            P.op("dve", lambda g_, rst=rst: g_.reciprocal(out=rst[0:64, :], in_=rst[0:64, :]), r=["T5_2"], w=["T5_2"])
            pg = self.psn(banks)
            self.proj_fm(pg, slot, 128, 64, g)
            P.act(sg[0:64, :], self.PS[pg][0:64, :], AF.Silu, r=[f"ps{pg}"], w=["T5_3"])
            P.tt("dve", ot[0:64, :], ot[0:64, :], rst[0:64, :], ALU.mult, r=["T5_0", "T5_2"], w=["T5_0"])
            P.stt(BO[0:64, gs], ot[0:64, :], self.CHP[0:64, CP_NG + h:CP_NG + h + 1],
                  sg[0:64, :], ALU.mult, ALU.mult, r=["T5_0", "T5_3", "CHP"], w=["B1"])
        P.dma("sp", f"mixw{h % 2}", self.mixT[r0m:r0m + 64, :], BO[0:64, :], r=["B1"], w=["mixT"])

    def unit_fox(self, l, h):
        P = self
        slot = h % 2
        WBs = self.WB[slot]
        wn = f"WB{slot}"
        self.load_w(slot, l, [(C_CQ + h * 64, 64), (C_CF + h, 1), (C_CF + h, 1),
                              (C_CK + h * 64, 64), (C_CV + h * 64, 64)])
        QA = self.BA[:, 0, :]; KA = self.BA[:, 1, :]; CO = self.BA[:, 2, :]; TB = self.BA[:, 3, :]
        FZ = self.F(1); CC = self.F(2); HF = self.F(3)
        r2 = slice(64, 66)
        fb = self.CHP[64:66, CP_FB + h:CP_FB + h + 1]
        banks = [0, 1, 2, 3, 4, 5]
        for g in range(8):
            gs = slice(g * 512, (g + 1) * 512)
            a = self.psn(banks)
            self.proj_fm(a, slot, 0, 66, g)
            P.act(QA[0:64, gs], self.PS[a][0:64, :], AF.Copy, r=[f"ps{a}"], w=["B0"], scale=0.125)
            P.ts("dve", FZ[r2, gs], self.PS[a][r2, :], fb, None, ALU.add, r=[f"ps{a}", "CHP"], w=["F1"])
            b = self.psn(banks)
            self.proj_fm(b, slot, 66, 64, g)
            P.cp("dve", KA[0:64, gs], self.PS[b][0:64, :], r=[f"ps{b}"], w=["B1"])
            self.proj_v_tm(slot, 130, g, banks)
        P.act(FZ[r2, :], FZ[r2, :], AF.Exp, r=["F1"], w=["F1"], scale=-1.0)
        P.ts("dve", FZ[r2, :], FZ[r2, :], 1.0, None, ALU.add, r=["F1"], w=["F1"])
        P.act(FZ[r2, :], FZ[r2, :], AF.Ln, r=["F1"], w=["F1"])
        ones512 = self.CF[r2, CF_ONES:CF_ONES + 128]
        for pc in range(32):
            sl = slice(pc * 128, (pc + 1) * 128)
            init = 0.0 if pc == 0 else CC[r2, pc * 128 - 1:pc * 128]
            P.op("dve", lambda g_, sl=sl, init=init: g_.tensor_tensor_scan(
                out=CC[r2, sl], data0=ones512, data1=FZ[r2, sl], initial=init,
                op0=ALU.mult, op1=ALU.subtract), r=["F1", "F2", "CF"], w=["F2"])
        P.cp("dve", TB[r2, :], CC[r2, :], r=["F2"], w=["B3"])
        P.cp("dve", HF[r2, :], TB[r2, :], r=["B3"], w=["F3"])
        P.tt("dve", FZ[r2, :], CC[r2, :], HF[r2, :], ALU.subtract, r=["F2", "F3"], w=["F1"])
        P.ts("dve", HF[r2, :], HF[r2, :], self.CF[r2, CF_E0:CF_E0 + 1], None, ALU.mult, r=["F3", "CF"], w=["F3"])
        P.stt(QA[r2, :], FZ[r2, :], self.CF[r2, CF_E1:CF_E1 + 1], HF[r2, :], ALU.mult, ALU.add,
              r=["F1", "F3", "CF"], w=["B0"])
        P.op("dve", lambda g_: g_.memset(KA[r2, :], 1.0), w=["B1"])
        pb = self.psn(banks)
        for i in range(NT):
            P.tr(self.PS[pb][:, i:i + 1], CC[64:65, i * 128:(i + 1) * 128], self.identf[64:65, 64:65],
                 r=["F2", "CF"], w=[f"ps{pb}"])
        P.act(self.CT[:], self.PS[pb][:, 0:NT], AF.Copy, r=[f"ps{pb}"], w=["CT"], scale=-1.0)
        pO, pS = 6, 7
        iters = [(qg, kb) for qg in range(8) for kb in range(4 * (qg + 1))]
        psts = {}

        def stageA(n):
            qg, kb = iters[n]
            gs0 = qg * 512
            col0 = max(0, (kb - 4 * qg) * 128)
            pst = self.psn(banks)
            psts[n] = pst
            P.mm(self.PS[pst][:, col0:512], KA[0:66, kb * 128:(kb + 1) * 128], QA[0:66, gs0 + col0:gs0 + 512],
                 start=True, stop=True, r=["B0", "B1"], w=[f"ps{pst}"])

        LOOK = 2
        for n in range(min(LOOK, len(iters))):
            stageA(n)
        for n, (qg, kb) in enumerate(iters):
            if n + LOOK < len(iters):
                stageA(n + LOOK)
            nkb = 4 * (qg + 1)
            gs0 = qg * 512
            col0 = max(0, (kb - 4 * qg) * 128)
            pst = psts.pop(n)
            pt = self.PT[n % 3]; ptn = f"PT{n % 3}"
            P.act(pt[:, col0:512], self.PS[pst][:, col0:512], AF.Exp, r=[f"ps{pst}", "CT"], w=[ptn],
                  bias=self.CT[:, kb:kb + 1], scale=1.0)
            if kb >= 4 * qg:
                P.tt("pool", pt[:, col0:col0 + 128], pt[:, col0:col0 + 128],
                     self.CB[:, CB_CMASK:CB_CMASK + 128], ALU.mult, r=[ptn, "CB"], w=[ptn])
            P.mm(self.PS[pO][0:64, col0:512], self.VT[:, kb, :], pt[:, col0:512],
                 start=(kb == 0), stop=(kb == nkb - 1), r=["VT", ptn], w=[f"ps{pO}"])
            P.mm(self.PS[pS][0:64, col0:512], self.onesb[:, 0:64], pt[:, col0:512],
                 start=(kb == 0), stop=(kb == nkb - 1), r=["CB", ptn], w=[f"ps{pS}"])
            if kb == nkb - 1:
                rs = self.T5[5]
                P.op("dve", lambda g_, rs=rs: g_.reciprocal(out=rs[0:64, :], in_=self.PS[pS][0:64, :]),
                     r=[f"ps{pS}"], w=["T5_5"])
                P.tt("dve", CO[0:64, gs0:gs0 + 512], self.PS[pO][0:64, :], rs[0:64, :], ALU.mult,
                     r=[f"ps{pO}", "T5_5"], w=["B2"])
        r0 = 640 + h * 64
        P.dma("sp", f"mixw{h % 2}", self.mixT[r0:r0 + 64, :], CO[0:64, :], r=["B2"], w=["mixT"])

    def phase_out(self, l, src):
        P = self
        WO = self.BA[:, 0:2, :].rearrange("p a (c n) -> p (a c) n", n=1024)
        for c in range(8):
            P.dma("pool", "wo", WO[:, c, :], self.w_out[l, c * 128:(c + 1) * 128, :], r=[], w=["B0", "B1"])
        G1 = self.FA[:, 0, 30:1054]; B1 = self.FA[:, 0, 1054:2078]
        P.dma("sp", "lnp", G1, self.ln1_g[l:l + 1, :].partition_broadcast(128), r=[], w=["F0"])
        P.dma("sp", "lnp", B1, self.ln1_b[l:l + 1, :].partition_broadcast(128), r=[], w=["F0"])
        for i in range(NT):
            g = i // 4
            ms = g % 2
            MX = self.BA[:, 2 + ms, :].rearrange("p (c n) -> p c n", c=8)
            if i % 4 == 0:
                P.dma("sp", f"mx{ms}", MX, self.mixT[:, g * 512:(g + 1) * 512].rearrange("(c p) n -> p c n", p=128),
                      r=["mixT"], w=[f"B{2 + ms}"])
            s2 = i % 2
            XR = self.FA[:, 1, s2 * 1024:(s2 + 1) * 1024]
            R = self.FA[:, 1, 2048 + s2 * 1024:2048 + (s2 + 1) * 1024]
            xrn = f"XR{s2}"; rn = f"R{s2}"
            P.dma("sp", f"xr{s2}", XR, src[i * 128:(i + 1) * 128, :], r=["xsrc"], w=[xrn])
            for half in range(2):
                pb = self.psn([2, 3, 4, 5])
                for c in range(8):
                    P.mm(self.PS[pb][:], MX[:, c, (i % 4) * 128:(i % 4 + 1) * 128], WO[:, c, half * 512:(half + 1) * 512],
                         start=(c == 0), stop=(c == 7), r=[f"B{2 + ms}", "B0", "B1"], w=[f"ps{pb}"])
                P.stt(R[:, half * 512:(half + 1) * 512], XR[:, half * 512:(half + 1) * 512], ALPHA, self.PS[pb][:],
                      ALU.mult, ALU.add, r=[xrn, f"ps{pb}"], w=[rn])
            self.layernorm_tile(R, rn, G1, B1, "F0")
            P.dma("sp", f"x1w{s2}", self.x1d[i * 128:(i + 1) * 128, :], R, r=[rn], w=["x1d"])
            xb = self.XBT[s2]
            P.cp("act", xb[:], R, r=[rn], w=[f"XBT{s2}"])
            self.transpose_to_XT(xb, f"XBT{s2}", i)

    def layernorm_tile(self, R, rn, G, Bb, gbn):
        P = self
        st = self.SM[:, 0:12]; mv = self.SM[:, 12:14]; rs = self.SM[:, 14:15]
        for half in range(2):
            P.op("dve", lambda g_, half=half: g_.bn_stats(out=st[:, half * 6:(half + 1) * 6],
                                                        in_=R[:, half * 512:(half + 1) * 512]), r=[rn], w=["SM"])
        P.op("dve", lambda g_: g_.bn_aggr(out=mv, in_=st), r=["SM"], w=["SM"])
        P.ts("dve", rs, mv[:, 1:2], EPS, None, ALU.add, r=["SM"], w=["SM"])
        P.act(rs, rs, AF.Sqrt, r=["SM"], w=["SM"])
        P.op("dve", lambda g_: g_.reciprocal(out=rs, in_=rs), r=["SM"], w=["SM"])
        P.ts("dve", R, R, mv[:, 0:1], rs, ALU.subtract, ALU.mult, r=[rn, "SM"], w=[rn])
        P.tt("pool", R, R, G, ALU.mult, r=[rn, gbn], w=[rn])
        P.tt("dve", R, R, Bb, ALU.add, r=[rn, gbn], w=[rn])

    def phase_peer(self, l):
        P = self
        dst = self.y if (l == DEPTH - 1) else self.xcur
        dstn = "y" if (l == DEPTH - 1) else "xsrc"
        allb = [0, 1, 2, 3, 4, 5]
        WQ = self.BA[:, 0:4, :].rearrange("p a (c n) -> p (a c) n", n=2048)
        for c in range(8):
            P.dma("pool", "wo", WQ[:, c, :], self.wq[l, c * 128:(c + 1) * 128, :], r=[], w=["B0", "B1", "B2", "B3"])
        KT = self.VT[:].rearrange("p a b -> p (a b)").rearrange("p (h n) -> p h n", h=16)
        P.dma("pool", "kt", KT, self.keysT[l].rearrange("h d n -> d h n"), r=[], w=["VT"])
        QPT = self.KET[:].rearrange("p a b c -> p (a b c)").rearrange("p (h t) -> p h t", h=16)
        GW2 = self.FA[:, 3, 0:4096].bitcast(BF16).rearrange("p (c n) -> p c n", c=8)
        for c in range(8):
            P.dma("pool", "gw2", GW2[:, c, :], self.ple_gw[l, c * 128:(c + 1) * 128, :], r=[], w=["F3"])
        PW = self.FA[:, 2, 0:1024].bitcast(BF16).rearrange("p (c n) -> p c n", c=2)
        for c in range(2):
            P.dma("pool", "gw2", PW[:, c, :], self.ple_w[l, c * 128:(c + 1) * 128, :], r=[], w=["F2"])
        G2 = self.FA[:, 2, 1024:2048]; B2 = self.FA[:, 2, 2048:3072]; GB = self.FA[:, 2, 3072:4096]
        P.dma("sp", "lnp", G2, self.ln2_g[l:l + 1, :].partition_broadcast(128), r=[], w=["F2"])
        P.dma("sp", "lnp", B2, self.ln2_b[l:l + 1, :].partition_broadcast(128), r=[], w=["F2"])
        P.dma("sp", "lnp", GB, self.ple_gb[l:l + 1, :].partition_broadcast(128), r=[], w=["F2"])
        gsa = self.FA[:, 0, 30:4126].bitcast(BF16)
        gsb = self.FA[:, 1, 1024:2048].bitcast(BF16)
        GS = [gsa[:, k * 1024:(k + 1) * 1024] for k in range(8)] + [gsb[:, k * 1024:(k + 1) * 1024] for k in range(2)]
        NGS = len(GS)
        JUNK = self.FA[:, 1, 0:1024]
        X1 = self.FA[:, 1, 2048:3072]; ACC = self.FA[:, 1, 3072:4096]
        PK = self.PK
        SV = PK[:, 0:256].rearrange("p (h k) -> p h k", h=16)
        SIf = PK[:, 256:512].rearrange("p (h k) -> p h k", h=16)
        CV = PK[:, 512:640].rearrange("p (h k) -> p h k", h=8)
        CA = PK[:, 640:768].rearrange("p (h k) -> p h k", h=8)
        CBf = PK[:, 768:896].rearrange("p (h k) -> p h k", h=8)
        I1 = PK[:, 896:1024]; I2 = PK[:, 1024:1152]
        GATE = PK[:, 1152:1280]; H = PK[:, 1280:1408]; W = PK[:, 1408:1536]
        SI = self.PT[1][:].bitcast(U32).rearrange("p (h k) -> p h k", h=16)
        CI = self.PT[2][:].bitcast(U32)[:, 0:128].rearrange("p (h k) -> p h k", h=8)
        CI2 = self.PT[2][:].bitcast(U32)[:, 128:256].rearrange("p (h k) -> p h k", h=8)
        iota = self.CF[:, CF_IOTA:CF_IOTA + 16]
        pu_flat = self.ub
        pv_flat = self.vb
        NEG = -1.0e30

        def top16(vals, wk, sv_out, si_out, rd, wkn, svn, sin_):
            P.op("dve", lambda g_: g_.max(out=sv_out[:, 0:8], in_=vals), r=rd, w=[svn])
            yield
            P.op("dve", lambda g_: g_.max_index(out=si_out[:, 0:8], in_max=sv_out[:, 0:8], in_values=vals),
                 r=rd + [svn], w=[sin_])
            yield
            P.op("dve", lambda g_: g_.match_replace(out=wk, in_to_replace=sv_out[:, 0:8], in_values=vals,
                                                   imm_value=NEG), r=rd + [svn], w=[wkn])
            yield
            P.op("dve", lambda g_: g_.max(out=sv_out[:, 8:16], in_=wk), r=[wkn], w=[svn])
            yield
            P.op("dve", lambda g_: g_.max_index(out=si_out[:, 8:16], in_max=sv_out[:, 8:16], in_values=wk),
                 r=[wkn, svn], w=[sin_])
            yield

        def rr(gens):
            gens = list(gens)
            while gens:
                for g in list(gens):
                    if next(g, "end") == "end":
                        gens.remove(g)
                    else:
                        yield

        def topk_gen(i, eb):
            ts_ = slice(i * 128, (i + 1) * 128)
            EIDX = self.EIDX[:, eb, :]
            en = f"EIDX{eb}"
            for q4 in range(4):
                bk = self.psn(allb)
                for j in range(4):
                    hc = 4 * q4 + j
                    for c in range(8):
                        P.mm(self.PS[bk][:, j * 128:(j + 1) * 128], WQ[:, c, hc * 128:(hc + 1) * 128], self.XT[:, c, ts_],
                             start=(c == 0), stop=(c == 7), r=["B0", "XT"], w=[f"ps{bk}"])
                P.cp("act", QPT[:, 4 * q4:4 * q4 + 4, :], self.PS[bk][:].rearrange("p (j t) -> p j t", j=4),
                     r=[f"ps{bk}"], w=["KET"])
            for q4 in range(4):
                bk = self.psn(allb)
                for j in range(4):
                    hc = 4 * q4 + j
                    P.mm(self.PS[bk][:, j * 128:(j + 1) * 128], QPT[:, hc, :], KT[:, hc, :], start=True, stop=True,
                         r=["KET", "VT"], w=[f"ps{bk}"])
                sc = self.T5[q4 % 2]; scn = f"T5_{q4 % 2}"
                wk = self.T5[2 + q4 % 2]; wkn = f"T5_{2 + q4 % 2}"
                P.cp("act", sc[:], self.PS[bk][:], r=[f"ps{bk}"], w=[scn])
                yield from rr([top16(sc[:, j * 128:(j + 1) * 128], wk[:, j * 128:(j + 1) * 128], SV[:, 4 * q4 + j, :],
                                     SI[:, 4 * q4 + j, :], [scn], f"{wkn}_{j}", f"SV{4 * q4 + j}", f"SI{4 * q4 + j}")
                               for j in range(4)])

            def cand_chain(h, slot):
                cd = self.T5[slot]; cdn = f"T5_{slot}"
                cand = cd[:, 0:256]; cwk = cd[:, 256:512]
                P.tt("dve", cand.rearrange("p (a b) -> p a b", a=16), apx(SV[:, 2 * h, :], [[1, 16], [0, 16]]),
                     apx(SV[:, 2 * h + 1, :], [[0, 16], [1, 16]]), ALU.add, r=[f"SV{2 * h}", f"SV{2 * h + 1}"],
                     w=[cdn + "c"])
                yield
                yield from top16(cand, cwk, CV[:, h, :], CI[:, h, :], [cdn + "c"], cdn + "w", f"CV{h}", f"CI{h}")

            for hp in range(4):
                yield from rr([cand_chain(2 * hp + j, 4 + j) for j in range(2)])
            allCI = [f"CI{h}" for h in range(8)]
            allSI = [f"SI{h}" for h in range(16)]
            allCV = [f"CV{h}" for h in range(8)]
            P.op("dve", lambda g_: g_.tensor_single_scalar(out=CI2[:], in_=CI[:], scalar=4, op=ALU.logical_shift_right),
                 r=allCI, w=["PKI2"])
            yield
            P.cp("dve", SIf[:], SI[:], r=allSI, w=["PKS"])
            yield
            P.cp("dve", CA[:], CI2[:], r=["PKI2"], w=["PKA"])
            yield
            P.op("dve", lambda g_: g_.tensor_single_scalar(out=CI2[:], in_=CI[:], scalar=15, op=ALU.bitwise_and),
                 r=allCI + ["PKA"], w=["PKI2"])
            yield
            P.cp("dve", CBf[:], CI2[:], r=["PKI2"], w=["PKB2"])
            yield

            def onehot_chain(h, c, slot):
                src, dstI, srcn = ((CA, I1, "PKA"), (CBf, I2, "PKB2"))[c]
                eq = self.T5[4 + slot // 2][:, (slot % 2) * 256:(slot % 2 + 1) * 256]
                eqn = f"T5_{4 + slot // 2}" + ("c" if slot % 2 == 0 else "w")
                eq3 = eq.rearrange("p (k a) -> p k a", k=16)
                P.tt("dve", eq3, apx(src[:, h, :], [[1, 16], [0, 16]]), apx(iota, [[0, 16], [1, 16]]),
                     ALU.is_equal, r=[srcn, "CF"], w=[eqn])
                yield
                P.tt("dve", eq3, eq3, apx(SIf[:, 2 * h + c, :], [[0, 16], [1, 16]]), ALU.mult, r=[eqn, "PKS"], w=[eqn])
                yield
                P.op("dve", lambda g_: g_.tensor_reduce(
                    out=dstI[:, h * 16:(h + 1) * 16], in_=eq3, axis=AX.X, op=ALU.add), r=[eqn], w=[f"I{c}_{h}"])
                yield

            for h2 in range(4):
                yield from rr([onehot_chain(2 * h2 + (s_ // 2), s_ % 2, s_) for s_ in range(4)])
            allI = [f"I{c}_{h}" for c in range(2) for h in range(8)]
            P.ts("dve", I1, I1, 128.0, 0.0, ALU.mult, ALU.add, r=allI, w=["PKB"])
            yield
            G3 = GATE.rearrange("p (h k) -> p h k", h=8)
            P.tt("dve", G3, CV, apx(CV[:, 0, 0:1], [[16, 8], [0, 16]]), ALU.subtract, r=allCV, w=["GATE"])
            yield
            P.act(GATE, GATE, AF.Exp, r=["GATE"], w=["GATE"])
            P.tt("dve", I1, I1, I2, ALU.add, r=["PKB"] + allI, w=["PKB"])
            yield
            zs = self.SM[:, 16:24]
            P.op("dve", lambda g_, G3=G3: g_.tensor_reduce(out=zs, in_=G3, axis=AX.X, op=ALU.add), r=["GATE"], w=["SM2"])
            yield
            P.cp("dve", EIDX, I1, r=["PKB"], w=[en])
            yield
            P.op("dve", lambda g_: g_.reciprocal(out=zs, in_=zs), r=["SM2"], w=["SM2"])
            yield
            P.tt("dve", G3, G3, apx(zs[:, 0:1], [[1, 8], [0, 16]]), ALU.mult, r=["GATE", "SM2"], w=["GATE"])
            yield

        def gather(table, tn, eb, k):
            sl = self.gs_rot % NGS
            self.gs_rot += 1
            P.kb.add("pool", lambda g_: g_.indirect_dma_start(
                out=GS[sl], out_offset=None, in_=table,
                in_offset=bass.IndirectOffsetOnAxis(ap=self.EIDX[:, eb, k:k + 1], axis=0)),
                self._b([f"EIDX{eb}", tn]), self._b([f"GS{sl}"]), dma=f"gs{sl}")
            return GS[sl], f"GS{sl}"

        nt = PEER_TILES
        for _ in topk_gen(0, 0):
            pass
        for i in range(nt):
            ts_ = slice(i * 128, (i + 1) * 128)
            eb = i % 2
            nxt = topk_gen(i + 1, 1 - eb) if i + 1 < nt else None
            P.dma("sp", "x1r", X1, self.x1d[ts_, :], r=["x1d"], w=["X1"])
            for k in range(128):
                g, gn = gather(pu_flat, "UB", eb, k)
                P.stt(JUNK, g, 1.0, X1, ALU.mult, ALU.mult, r=[gn, "X1"], w=["JUNK", "H"], accum_out=H[:, k:k + 1])
            P.tt("dve", W, H, H, ALU.mult, r=["H"], w=["W"])
            P.ts("dve", W, W, 0.044715, 1.0, ALU.mult, ALU.add, r=["W"], w=["W"])
            P.tt("dve", W, W, H, ALU.mult, r=["W", "H"], w=["W"])
            P.act(W, W, AF.Sigmoid, r=["W"], w=["W"], scale=1.5957691216057308)
            P.tt("dve", W, W, H, ALU.mult, r=["W", "H"], w=["W"])
            P.tt("dve", W, W, GATE, ALU.mult, r=["W", "GATE"], w=["W"])
            for k in range(128):
                g, gn = gather(pv_flat, "VB", eb, k)
                dgs = k % 4
                dg = self.XBT[1][:, 256 + dgs * 128:256 + (dgs + 1) * 128]
                P.act(dg, self.identb, AF.Copy, r=["CB", "W"], w=[f"DG{dgs}"], scale=W[:, k:k + 1])
                for half in range(2):
                    P.mm(self.PS[6 + half][:], dg, g[:, half * 512:(half + 1) * 512], start=(k == 0), stop=(k == 127),
                         r=[f"DG{dgs}", gn], w=[f"ps{6 + half}"])
                if nxt is not None:
                    for _ in range(2):
                        if next(nxt, "end") == "end":
                            nxt = None
                            break
            if nxt is not None:
                for _ in nxt:
                    pass
            for half in range(2):
                hs = slice(half * 512, (half + 1) * 512)
                P.stt(ACC[:, hs], X1[:, hs], ALPHA, self.PS[6 + half][:], ALU.mult, ALU.add,
                      r=["X1", f"ps{6 + half}"], w=["ACC"])
            xb = self.XBT[0]
            P.cp("act", xb[:], ACC, r=["ACC"], w=["XBT0"])
            bk = self.psn(allb)
            psb = self.PS[bk][:].bitcast(BF16)
            for c in range(8):
                P.tr(psb[:, c * 128:(c + 1) * 128], xb[:, c * 128:(c + 1) * 128], self.identb, r=["XBT0", "CB"], w=[f"ps{bk}"])
            RTB = self.T5[5][:].bitcast(BF16)
            RT = RTB.rearrange("p (c t) -> p c t", c=8)
            P.cp("act", RTB, psb, r=[f"ps{bk}"], w=["T5_5c", "T5_5w"])
            pb16 = self.XBT[1]
            P.dma("pool", "pld", pb16[:, 0:256], self.p_in[l, ts_, :], r=[], w=["XBT1"])
            bk2 = self.psn(allb)
            psb2 = self.PS[bk2][:].bitcast(BF16)
            for c in range(2):
                P.tr(psb2[:, c * 128:(c + 1) * 128], pb16[:, c * 128:(c + 1) * 128], self.identb, r=["XBT1", "CB"], w=[f"ps{bk2}"])
            PTT = self.PT[0][:, 0:256].rearrange("p (c t) -> p c t", c=2)
            P.cp("act", self.PT[0][:, 0:256], psb2[:, 0:256], r=[f"ps{bk2}"], w=["PT0"])
            for half in range(2):
                hs = slice(half * 512, (half + 1) * 512)
                pg = self.psn(allb)
                for c in range(8):
                    P.mm(self.PS[pg][:], RT[:, c, :], GW2[:, c, hs], start=(c == 0), stop=(c == 7),
                         r=["T5_5c", "T5_5w", "F3"], w=[f"ps{pg}"])
                pp = self.psn(allb)
                for c in range(2):
                    P.mm(self.PS[pp][:], PTT[:, c, :], PW[:, c, hs], start=(c == 0), stop=(c == 1),
                         r=["PT0", "F2"], w=[f"ps{pp}"])
                t = self.T5[half]; tn = f"T5_{half}"
                P.tt("dve", t[:], self.PS[pg][:], GB[:, hs], ALU.add, r=[f"ps{pg}", "F2"], w=[tn])
                P.act(t[:], t[:], AF.Sigmoid, r=[tn], w=[tn])
                P.tt("dve", t[:], t[:], self.PS[pp][:], ALU.mult, r=[tn, f"ps{pp}"], w=[tn])
                P.tt("dve", ACC[:, hs], ACC[:, hs], t[:], ALU.add, r=[tn, "ACC"], w=["ACC"])
            self.layernorm_tile(ACC, "ACC", G2, B2, "F2")
            P.dma("sp", "xw", dst[ts_, :], ACC, r=["ACC"], w=[dstn])


def host_tables(inp):
    L = DEPTH
    chanp = np.zeros((L, 128, CP_N), np.float32)
    for j in range(2):
        chanp[:, :, CP_CB + j] = inp["conv_b"][:, j * 128:(j + 1) * 128]
        chanp[:, :, CP_LG + j] = inp["conv_ln_g"][:, j * 128:(j + 1) * 128]
        chanp[:, :, CP_LB + j] = inp["conv_ln_b"][:, j * 128:(j + 1) * 128]
        chanp[:, :, CP_CW + j * 31:CP_CW + (j + 1) * 31] = np.transpose(
            inp["conv_w"][:, :, j * 128:(j + 1) * 128], (0, 2, 1))
    for h in range(6):
        chanp[:, 0:32, CP_GB + h] = inp["gla_gate_b"][:, h * 32:(h + 1) * 32]
        chanp[:, 0:64, CP_NG + h] = inp["gla_norm_g"][:, h * 64:(h + 1) * 64]
        chanp[:, :, CP_FB + h] = inp["fox_forget_b"][:, h][:, None]
    keysT = np.ascontiguousarray(
        np.transpose(np.asarray(inp["peer_keys"]).reshape(L, 16, 128, 128), (0, 1, 3, 2)))
    return chanp, keysT


SHARED = ["w_in", "gla_gate_w", "w_out", "ln1_g", "ln1_b", "peer_wq", "peer_u", "peer_v",
          "ple_w", "ple_gw", "ple_gb", "ln2_g", "ln2_b"]


def make_in_maps(inp, cores):
    inp = {k: np.asarray(v) for k, v in inp.items()}
    chanp, keysT = host_tables(inp)
    cf, cb = host_consts()
    shared = {k: np.ascontiguousarray(inp[k], dtype=np.float32) for k in SHARED}
    shared.update(chanp=chanp, keysT=keysT, cf32=cf, cb16=cb)
    maps = []
    for c in cores:
        m = dict(shared)
        m["x"] = np.ascontiguousarray(inp["x"][c])
        m["p"] = np.ascontiguousarray(inp["p"][:, c])
        maps.append(m)
    return maps


_PROG = None


def kernel(**inputs):
    global _PROG
    if _PROG is None:
        _PROG = Program()
        _PROG.build()
    maps = make_in_maps(inputs, list(range(8)))
    res = run_bass_kernel_spmd(_PROG.nc, maps, core_ids=list(range(8)))
    return np.stack([np.asarray(r["y"]) for r in res.results], axis=0).astype(np.float32)
```

```python
import numpy as np
import ml_dtypes
from contextlib import ExitStack
import concourse.bass as bass
import concourse.mybir as mybir
from concourse.bass_utils import run_bass_kernel_spmd

F32 = mybir.dt.float32
BF16 = mybir.dt.bfloat16
U32 = mybir.dt.uint32
I32 = mybir.dt.int32
AF = mybir.ActivationFunctionType
ALU = mybir.AluOpType
AX = mybir.AxisListType

D = 1024
S = 4096
NT = 32
DEPTH = 4
IN_COLS = 2838
ALPHA = (2.0 * DEPTH) ** 0.25
EPS = 1e-5
C_AVAL, C_AGATE, C_BQ, C_BK, C_BV, C_BG, C_BLR, C_CQ, C_CK, C_CV, C_CF = (
    0, 256, 512, 704, 896, 1280, 1664, 1680, 2064, 2448, 2832)


class Op:
    __slots__ = ("eng", "fn", "deps", "stream", "needed", "val", "isdma")


class Buf:
    __slots__ = ("name", "w", "r")

    def __init__(self, name):
        self.name = name
        self.w = {}
        self.r = {}


class KB:
    def __init__(self, nc, es):
        self.nc = nc
        self.es = es
        self.ops = []
        self.eng = {"pe": nc.tensor, "act": nc.scalar, "dve": nc.vector,
                    "pool": nc.gpsimd, "sp": nc.sync}
        self.sems = {}
        self.nsem = 0

    def sem(self, name):
        if name not in self.sems:
            self.sems[name] = self.es.enter_context(self.nc.semaphore("s_" + name))
            self.nsem += 1
        return self.sems[name]

    def add(self, eng, fn, reads=(), writes=(), dma=None):
        op = Op()
        op.eng = eng
        op.fn = fn
        op.isdma = dma is not None
        op.stream = ("d_" + dma) if dma is not None else eng
        op.needed = False
        op.val = 0
        deps = set()
        for b in reads:
            deps.update(b.w.values())
        for b in writes:
            deps.update(b.w.values())
            deps.update(b.r.values())
        op.deps = [d for d in deps if not (eng == "pe" and d.stream == "pe")]
        for d in op.deps:
            d.needed = True
        for b in reads:
            b.r[op.stream] = op
        for b in writes:
            b.w = {op.stream: op}
            b.r = {}
        self.ops.append(op)
        return op

    def emit(self):
        counters = {}
        for op in self.ops:
            if op.needed or op.isdma:
                inc = 16 if op.isdma else 1
                counters[op.stream] = counters.get(op.stream, 0) + inc
                op.val = counters[op.stream]
        for st in counters:
            self.sem(st)
        seen = {e: {} for e in self.eng}
        n = 0
        for op in self.ops:
            e = self.eng[op.eng]
            need = {}
            for d in op.deps:
                if d.val > need.get(d.stream, 0):
                    need[d.stream] = d.val
            sn = seen[op.eng]
            for st, v in need.items():
                if sn.get(st, 0) < v:
                    e.wait_ge(self.sems[st], v)
                    sn[st] = v
                    n += 1
            if op.fn is not None:
                ins = op.fn(e)
                n += 1
                if op.needed or op.isdma:
                    ins.then_inc(self.sems[op.stream], 16 if op.isdma else 1)
        return n


def apx(base, dims):
    return bass.AP(base.tensor, base.offset, [list(base.ap[0])] + [list(d) for d in dims])


class Prog:
    def __init__(self, n_layers=DEPTH, dbg=None):
        self.n_layers = n_layers
        self.dbg = dbg or {}
        self.nc = bass.Bass("TRN2", target_bir_lowering=False)
        self.es = ExitStack()
        self.kb = KB(self.nc, self.es)
        self.bufs = {}

    def dram_in(self, name, shape, dt=F32):
        return self.nc.dram_tensor(name, list(shape), dt, kind="ExternalInput").ap()

    def dram_out(self, name, shape, dt=F32):
        return self.nc.dram_tensor(name, list(shape), dt, kind="ExternalOutput").ap()

    def dram_tmp(self, name, shape, dt=F32):
        return self.nc.dram_tensor(name, list(shape), dt, kind="Internal").ap()

    def sb(self, name, shape, dt=F32):
        return self.es.enter_context(self.nc.sbuf_tensor(name, list(shape), dt))

    def B(self, name):
        if name not in self.bufs:
            self.bufs[name] = Buf(name)
        return self.bufs[name]

    def _b(self, xs):
        return [self.B(x) if isinstance(x, str) else x for x in xs]

    def op(self, eng, fn, r=(), w=()):
        return self.kb.add(eng, fn, self._b(r), self._b(w))

    def dma(self, q, sem, out, in_, r=(), w=(), **kw):
        e = {"sp": "sp", "pool": "pool", "act": "act"}[q]
        return self.kb.add(e, lambda g: g.dma_start(out=out, in_=in_, **kw),
                           self._b(r), self._b(w), dma=sem)

    def mm(self, out, lhsT, rhs, start, stop, r=(), w=()):
        return self.op("pe", lambda g: g.matmul(out, lhsT, rhs, start=start, stop=stop), r, w)

    def tr(self, out, in_, ident, r=(), w=()):
        return self.op("pe", lambda g: g.transpose(out, in_, ident), r, w)

    def act(self, out, in_, func, r=(), w=(), **kw):
        return self.op("act", lambda g: g.activation(out=out, in_=in_, func=func, **kw), r, w)

    def tt(self, eng, out, in0, in1, op, r=(), w=()):
        return self.op(eng, lambda g: g.tensor_tensor(out=out, in0=in0, in1=in1, op=op), r, w)

    def ts(self, eng, out, in0, s1, s2, op0, op1=None, r=(), w=(), **kw):
        if op1 is None:
            return self.op(eng, lambda g: g.tensor_scalar(out=out, in0=in0, scalar1=s1, scalar2=None,
                                                          op0=op0, **kw), r, w)
        return self.op(eng, lambda g: g.tensor_scalar(out=out, in0=in0, scalar1=s1, scalar2=s2,
                                                      op0=op0, op1=op1, **kw), r, w)

    def stt(self, out, in0, scalar, in1, op0, op1, r=(), w=(), **kw):
        return self.op("dve", lambda g: g.scalar_tensor_tensor(out=out, in0=in0, scalar=scalar, in1=in1,
                                                               op0=op0, op1=op1, **kw), r, w)

    def cp(self, eng, out, in_, r=(), w=()):
        if eng == "act":
            return self.op("act", lambda g: g.copy(out=out, in_=in_), r, w)
        return self.op(eng, lambda g: g.tensor_copy(out=out, in_=in_), r, w)


CP_CB, CP_LG, CP_LB, CP_CW, CP_GB, CP_NG, CP_FB, CP_N = 0, 2, 4, 6, 68, 74, 80, 86
CF_IDENT, CF_ONES, CF_SCM, CF_IOTA, CF_E0, CF_E1, CF_M0, CF_M1, CF_N = 0, 128, 256, 768, 784, 785, 786, 787, 788
CB_IDENT, CB_CMASK, CB_GMASK, CB_ONES, CB_N = 0, 128, 256, 384, 512


def host_consts():
    cf = np.zeros((128, CF_N), np.float32)
    cf[:, CF_IDENT:CF_IDENT + 128] = np.eye(128, dtype=np.float32)
    cf[:, CF_ONES:CF_ONES + 128] = 1.0
    scm = np.ones((512,), np.float32)
    scm[::64] = 0.0
    cf[:, CF_SCM:CF_SCM + 512] = scm[None, :]
    cf[:, CF_IOTA:CF_IOTA + 16] = np.arange(16, dtype=np.float32)[None, :]
    cf[64, CF_E0] = 1.0
    cf[65, CF_E1] = 1.0
    cf[0:64, CF_M0] = 1.0
    cf[64:128, CF_M1] = 1.0
    cb = np.zeros((128, CB_N), np.float32)
    cb[:, CB_IDENT:CB_IDENT + 128] = np.eye(128)
    s = np.arange(128)[:, None]
    t = np.arange(128)[None, :]
    cb[:, CB_CMASK:CB_CMASK + 128] = (t >= s)
    cb[:, CB_GMASK:CB_GMASK + 128] = (t >= s) & ((t // 64) == (s // 64))
    cb[:, CB_ONES:CB_ONES + 128] = 1.0
    return cf, cb.astype(ml_dtypes.bfloat16)


GLA_STAGE = 99
PEER_STAGE = 99
PEER_TILES = NT


class Program(Prog):
    def build(self):
        nc = self.nc
        P = self
        L = DEPTH
        self.x_in = P.dram_in("x", [S, D])
        self.p_in = P.dram_in("p", [L, S, 256])
        self.w_in = P.dram_in("w_in", [L, D, IN_COLS])
        self.chanp_d = P.dram_in("chanp", [L, 128, CP_N])
        self.gatew_d = P.dram_in("gla_gate_w", [L, 16, 192])
        self.w_out = P.dram_in("w_out", [L, D, D])
        self.ln1_g = P.dram_in("ln1_g", [L, D]); self.ln1_b = P.dram_in("ln1_b", [L, D])
        self.wq = P.dram_in("peer_wq", [L, D, 2048])
        self.keysT = P.dram_in("keysT", [L, 16, 128, 128])
        self.pu = P.dram_in("peer_u", [L, 16384, D])
        self.pv = P.dram_in("peer_v", [L, 16384, D])
        self.ple_w = P.dram_in("ple_w", [L, 256, D])
        self.ple_gw = P.dram_in("ple_gw", [L, D, D])
        self.ple_gb = P.dram_in("ple_gb", [L, D])
        self.ln2_g = P.dram_in("ln2_g", [L, D]); self.ln2_b = P.dram_in("ln2_b", [L, D])
        self.cf_d = P.dram_in("cf32", [128, CF_N])
        self.cb_d = P.dram_in("cb16", [128, CB_N], BF16)
        self.y = P.dram_out("y", [S, D])
        self.xcur = P.dram_tmp("xcur", [S, D])
        self.x1d = P.dram_tmp("x1d", [S, D])
        self.mixT = P.dram_tmp("mixT", [D, S], BF16)
        self.ub = P.dram_tmp("ub", [16384, D], BF16)
        self.vb = P.dram_tmp("vb", [16384, D], BF16)
        self.dbg_out = {}
        for name, (shape, dt, attr, bufs) in self.dbg.items():
            self.dbg_out[name] = P.dram_out("dbg_" + name, shape, dt)
        self.XT = P.sb("XT", [128, 8, S], BF16)
        self.FA = P.sb("FA", [128, 4, 4128], F32)
        self.BA = P.sb("BA", [128, 4, S], BF16)
        self.WB = [P.sb("WB0", [128, 8, 256], BF16), P.sb("WB1", [128, 8, 256], BF16)]
        self.CF = P.sb("CF", [128, CF_N], F32)
        self.CB = P.sb("CB", [128, CB_N], BF16)
        self.CHP = P.sb("CHP", [128, CP_N], F32)
        self.XBT = [P.sb("XBT0", [128, 1024], BF16), P.sb("XBT1", [128, 1024], BF16)]
        self.T5 = [P.sb(f"T5_{i}", [128, 512], F32) for i in range(6)]
        self.VT = P.sb("VT", [128, NT, 64], BF16)
        self.KET = P.sb("KET", [128, 2, NT, 32], BF16)
        self.CT = P.sb("CT", [128, NT], F32)
        self.PT = [P.sb(f"PT{i}", [128, 512], BF16) for i in range(3)]
        self.GW = P.sb("GW", [16, 192], BF16)
        self.SM = P.sb("SM", [128, 64], F32)
        self.PK = P.sb("PK", [128, 1536], F32)
        self.EIDX = P.sb("EIDX", [128, 2, 128], U32)
        self.gs_rot = 0
        self.PS = [self.es.enter_context(nc.psum_tensor(f"ps{i}", [128, 512], F32)) for i in range(8)]
        self.ps_rot = 0
        self.identb = self.CB[:, CB_IDENT:CB_IDENT + 128]
        self.identf = self.CF[:, CF_IDENT:CF_IDENT + 128]
        self.onesf = self.CF[:, CF_ONES:CF_ONES + 128]
        self.onesb = self.CB[:, CB_ONES:CB_ONES + 128]
        P.dma("sp", "cst0", self.CF[:], self.cf_d, w=["CF"])
        P.dma("sp", "cst1", self.CB[:], self.cb_d, w=["CB"])
        P.op("dve", lambda g: g.memset(self.FA[:, 0, 0:30], 0.0), w=["F0"])

        for l in range(self.n_layers):
            self.layer(l)

        fin = []
        for name, (shape, dt, attr, bufs) in self.dbg.items():
            P.dma("sp", "dbg", self.dbg_out[name], getattr(self, attr), r=bufs, w=["dbg_" + name])
            fin.append("dbg_" + name)
        fin.append("y")
        P.op("sp", None, r=fin)
        n = self.kb.emit()
        return n

    def convert_tables(self, l):
        for (src, dstt, nm) in ((self.pu, self.ub, "UB"), (self.pv, self.vb, "VB")):
            for j in range(16):
                self.dma("pool", "cv" + nm, dstt[j * 1024:(j + 1) * 1024, :], src[l, j * 1024:(j + 1) * 1024, :],
                         r=[], w=[nm])

    def psn(self, banks):
        b = banks[self.ps_rot % len(banks)]
        self.ps_rot += 1
        return b

    def F(self, i, lo=0, hi=4096):
        return self.FA[:, i, lo:hi]

    def layer(self, l):
        P = self
        src = self.x_in if l == 0 else self.xcur
        P.dma("sp", "chp", self.CHP[:], self.chanp_d[l], r=[], w=["CHP"])
        P.dma("pool", "gw", self.GW[:], self.gatew_d[l], r=[], w=["GW"])
        self.convert_tables(l)
        self.phase_xT(src, "xsrc")
        self.unit_conv(l)
        self.lrt_done = False
        for h in range(6):
            self.unit_gla(l, h)
        for h in range(6):
            self.unit_fox(l, h)
        self.phase_out(l, src)
        self.phase_peer(l)

    def phase_xT(self, src, srcname):
        P = self
        for i in range(NT):
            s = i % 2
            xb = self.XBT[s]
            P.dma("pool", f"xbt{s}", xb[:], src[i * 128:(i + 1) * 128, :], r=[srcname], w=[f"XBT{s}"])
            self.transpose_to_XT(xb, f"XBT{s}", i)

    def transpose_to_XT(self, xb, xbname, i):
        P = self
        bank = self.psn([0, 1])
        psb = self.PS[bank][:].bitcast(BF16)
        for c in range(8):
            P.tr(psb[:, c * 128:(c + 1) * 128], xb[:, c * 128:(c + 1) * 128], self.identb,
                 r=[xbname, "CB"], w=[f"ps{bank}"])
        P.cp("act" if i % 2 else "dve", self.XT[:, :, i * 128:(i + 1) * 128],
             psb.rearrange("p (c t) -> p c t", c=8), r=[f"ps{bank}"], w=["XT"])

    def load_w(self, slot, l, cols):
        off = 0
        for (c0, n) in cols:
            kw = dict(allow_slow_non_contiguous=True) if n == 1 else {}
            self.dma("pool", f"wb{slot}", self.WB[slot][:, :, off:off + n],
                     self.w_in[l, :, c0:c0 + n].rearrange("(c p) n -> p c n", p=128), r=[], w=[f"WB{slot}"], **kw)
            off += n

    def proj_fm(self, bank, slot, woff, m, g, prow=0):
        for c in range(8):
            self.mm(self.PS[bank][prow:prow + m, :], self.WB[slot][:, c, woff:woff + m],
                    self.XT[:, c, g * 512:(g + 1) * 512], start=(c == 0), stop=(c == 7),
                    r=[f"WB{slot}", "XT"], w=[f"ps{bank}"])

    def proj_v_tm(self, slot, woff, g, banks):
        bank = self.psn(banks)
        for j in range(4):
            i = 4 * g + j
            for c in range(8):
                self.mm(self.PS[bank][:, j * 64:(j + 1) * 64], self.XT[:, c, i * 128:(i + 1) * 128],
                        self.WB[slot][:, c, woff:woff + 64], start=(c == 0), stop=(c == 7),
                        r=[f"WB{slot}", "XT"], w=[f"ps{bank}"])
        self.cp("act", self.VT[:, 4 * g:4 * g + 4, :],
                self.PS[bank][:, 0:256].rearrange("p (j e) -> p j e", j=4), r=[f"ps{bank}"], w=["VT"])

    def unit_conv(self, l):
        P = self
        UP = self.FA[:, 0, :]
        for j in range(2):
            slot = j
            self.load_w(slot, l, [(C_AVAL + j * 128, 128), (C_AGATE + j * 128, 128)])
            for g in range(8):
                a = self.psn([2, 3, 4, 5]); b = self.psn([2, 3, 4, 5])
                self.proj_fm(a, slot, 0, 128, g)
                self.proj_fm(b, slot, 128, 128, g)
                t = self.T5[g % 2]
                P.act(t[:], self.PS[b][:], AF.Sigmoid, r=[f"ps{b}"], w=[f"T5_{g % 2}"])
                P.tt("dve", UP[:, 30 + g * 512:30 + (g + 1) * 512], self.PS[a][:], t[:], ALU.mult,
                     r=[f"ps{a}", f"T5_{g % 2}"], w=["F0"])
            acc = self.F(1 + j)
            cw = self.CHP[:, CP_CW + j * 31:CP_CW + (j + 1) * 31]
            P.ts("dve", acc, UP[:, 0:4096], cw[:, 0:1], self.CHP[:, CP_CB + j:CP_CB + j + 1],
                 ALU.mult, ALU.add, r=["F0", "CHP"], w=[f"F{1 + j}"])
            for k in range(1, 31):
                P.stt(acc, UP[:, k:k + 4096], cw[:, k:k + 1], acc, ALU.mult, ALU.add,
                      r=["F0", "CHP", f"F{1 + j}"], w=[f"F{1 + j}"])
        for g in range(8):
            gs = slice(g * 512, (g + 1) * 512)
            pm = self.psn([2, 3, 4, 5]); pe = self.psn([2, 3, 4, 5])
            for j in range(2):
                P.mm(self.PS[pm][:], self.onesf, self.F(1 + j)[:, gs], start=(j == 0), stop=(j == 1),
                     r=["CF", f"F{1 + j}"], w=[f"ps{pm}"])
            for j in range(2):
                sq = self.T5[j]
                P.act(sq[:], self.F(1 + j)[:, gs], AF.Square, r=[f"F{1 + j}"], w=[f"T5_{j}"])
                P.mm(self.PS[pe][:], self.onesf, sq[:], start=(j == 0), stop=(j == 1),
                     r=["CF", f"T5_{j}"], w=[f"ps{pe}"])
            mean = self.T5[2]; msq = self.T5[3]; var = self.T5[4]
            P.act(mean[:], self.PS[pm][:], AF.Copy, r=[f"ps{pm}"], w=["T5_2"], scale=1.0 / 256)
            P.act(msq[:], mean[:], AF.Square, r=["T5_2"], w=["T5_3"])
            P.stt(var[:], self.PS[pe][:], 1.0 / 256, msq[:], ALU.mult, ALU.subtract, r=[f"ps{pe}", "T5_3"], w=["T5_4"])
            P.ts("dve", var[:], var[:], 0.0, EPS, ALU.max, ALU.add, r=["T5_4"], w=["T5_4"])
            P.act(var[:], var[:], AF.Sqrt, r=["T5_4"], w=["T5_4"])
            P.op("dve", lambda g_, v=var: g_.reciprocal(out=v[:], in_=v[:]), r=["T5_4"], w=["T5_4"])
            for j in range(2):
                t = self.T5[j]
                P.tt("dve", t[:], self.F(1 + j)[:, gs], mean[:], ALU.subtract, r=[f"F{1 + j}", "T5_2"], w=[f"T5_{j}"])
                P.tt("dve", t[:], t[:], var[:], ALU.mult, r=[f"T5_{j}", "T5_4"], w=[f"T5_{j}"])
                P.act(self.BA[:, j, gs], t[:], AF.Silu, r=[f"T5_{j}", "CHP"], w=[f"B{j}"],
                      scale=self.CHP[:, CP_LG + j:CP_LG + j + 1], bias=self.CHP[:, CP_LB + j:CP_LB + j + 1])
        for j in range(2):
            P.dma("sp", f"mixw{j}", self.mixT[j * 128:(j + 1) * 128, :], self.BA[:, j, :], r=[f"B{j}"], w=["mixT"])

    def gla_end(self, h):
        self.dma("sp", f"mixw{h % 2}", self.mixT[256 + h * 64:320 + h * 64, :], self.BA[0:64, 2, :], r=["B2"], w=["mixT"])

    def unit_gla(self, l, h):
        P = self
        slot = h % 2
        wn = f"WB{slot}"
        banks = [0, 1, 2, 3, 4, 5]
        QE = self.BA[:, 0, :]; KE = self.BA[:, 1, :]; AT = self.BA[:, 2, :]; SB = self.BA[:, 3, :]
        GZ = self.FA[:, 0, 30:4126]; EC = self.F(1); EN = self.F(2); ST = self.F(3)
        D1 = GZ; D0 = EN
        rq = slice(0, 32)
        self.load_w(slot, l, [(C_BQ + h * 32, 32), (C_BK + h * 32, 32), (C_BV + h * 64, 64), (C_BG + h * 64, 64),
                              (C_BLR, 16)])
        for g in range(8):
            gs = slice(g * 512, (g + 1) * 512)
            a0 = self.psn(banks)
            self.proj_fm(a0, slot, 192, 16, g)
            lrt = self.PT[g % 2]; lrn = f"PT{g % 2}"
            P.cp("act", lrt[0:16, :], self.PS[a0][0:16, :], r=[f"ps{a0}"], w=[lrn])
            a = self.psn(banks)
            P.mm(self.PS[a][rq, :], self.GW[0:16, h * 32:(h + 1) * 32], lrt[0:16, :], start=True, stop=True,
                 r=["GW", lrn], w=[f"ps{a}"])
            P.ts("dve", GZ[rq, gs], self.PS[a][rq, :], self.CHP[rq, CP_GB + h:CP_GB + h + 1], None, ALU.add,
                 r=[f"ps{a}", "CHP"], w=["F0"])
        P.act(GZ[rq, :], GZ[rq, :], AF.Exp, r=["F0"], w=["F0"], scale=-1.0)
        P.ts("dve", GZ[rq, :], GZ[rq, :], 1.0, None, ALU.add, r=["F0"], w=["F0"])
        P.act(GZ[rq, :], GZ[rq, :], AF.Ln, r=["F0"], w=["F0"])
        scm = self.CF[rq, CF_SCM:CF_SCM + 512]
        for g in range(8):
            gs = slice(g * 512, (g + 1) * 512)
            P.op("dve", lambda g_, gs=gs: g_.tensor_tensor_scan(
                out=ST[rq, gs], data0=scm, data1=GZ[rq, gs], initial=0.0, op0=ALU.mult, op1=ALU.subtract),
                r=["F0", "CF"], w=["F3"])
        P.act(EC[rq, :], ST[rq, :], AF.Exp, r=["F3"], w=["F1"], scale=1.0 / 16)
        P.act(EN[rq, :], ST[rq, :], AF.Exp, r=["F3"], w=["F2"], scale=-1.0 / 16)
        if GLA_STAGE < 1:
            return
        for g in range(8):
            gs = slice(g * 512, (g + 1) * 512)
            a = self.psn(banks)
            self.proj_fm(a, slot, 0, 32, g)
            P.stt(QE[rq, gs], self.PS[a][rq, :], 32.0 ** -0.5, EC[rq, gs], ALU.mult, ALU.mult,
                  r=[f"ps{a}", "F1"], w=["B0"])
            b = self.psn(banks)
            self.proj_fm(b, slot, 32, 32, g)
            P.tt("dve", KE[rq, gs], self.PS[b][rq, :], EN[rq, gs], ALU.mult, r=[f"ps{b}", "F2"], w=["B1"])
            self.proj_v_tm(slot, 64, g, banks)
        if GLA_STAGE < 2:
            return
        pk = self.psn(banks)
        pkb = self.PS[pk][:].bitcast(BF16)
        for i in range(NT):
            P.tr(pkb[:, i * 32:(i + 1) * 32], KE[rq, i * 128:(i + 1) * 128], self.identb[0:32, 0:32],
                 r=["B1", "CB"], w=[f"ps{pk}"])
        for m in range(2):
            P.ts("dve", self.KET[:, m, :, :], pkb.rearrange("p (i d) -> p i d", i=NT),
                 self.CF[:, CF_M0 + m:CF_M0 + m + 1], None, ALU.mult, r=[f"ps{pk}", "CF"], w=["KET"])
        if GLA_STAGE < 3:
            return
        ATv = AT.rearrange("p (i t) -> p i t", i=NT)
        gmask4 = apx(self.CB[:, CB_GMASK:CB_GMASK + 128], [[0, 4], [1, 128]])
        for i4 in range(8):
            a = self.psn(banks)
            for j in range(4):
                i = 4 * i4 + j
                P.mm(self.PS[a][:, j * 128:(j + 1) * 128], KE[rq, i * 128:(i + 1) * 128], QE[rq, i * 128:(i + 1) * 128],
                     start=True, stop=True, r=["B0", "B1"], w=[f"ps{a}"])
            for j in range(4):
                if GLA_STAGE == 3.5:
                    break
                P.tt("dve", ATv[:, 4 * i4 + j, :], self.PS[a][:, j * 128:(j + 1) * 128],
                     self.CB[:, CB_GMASK:CB_GMASK + 128], ALU.mult, r=[f"ps{a}", "CB"], w=["B2"])
        if GLA_STAGE < 4:
            return self.gla_end(h)
        for c8 in range(8):
            a = self.psn(banks)
            for cc in range(8):
                c = c8 * 8 + cc
                i = c // 2; r0 = (c % 2) * 64
                P.mm(self.PS[a][rq, cc * 64:(cc + 1) * 64], self.KET[:, c % 2, i, :], self.VT[:, i, :],
                     start=True, stop=True, r=["KET", "VT"], w=[f"ps{a}"])
            ac = apx(EC[rq, 64 * (c8 * 8) + 63:64 * (c8 * 8) + 64], [[64, 8], [0, 64]])
            out = apx(D1[rq, c8 * 8:c8 * 8 + 1], [[1, 8], [64, 64]])
            P.tt("dve", out, self.PS[a][rq, :].rearrange("p (c e) -> p c e", c=8), ac, ALU.mult,
                 r=[f"ps{a}", "F1"], w=["F0"])
        if GLA_STAGE < 5:
            return
        P.cp("dve", D0[rq, :].rearrange("p (e c) -> p e c", e=64), apx(EC[rq, 63:64], [[0, 64], [64, 64]]),
             r=["F1"], w=["F2"])
        P.op("dve", lambda g_: g_.memset(apx(D0[rq, 0:1], [[64, 64]]), 0.0), w=["F2"])
        P.op("dve", lambda g_: g_.tensor_tensor_scan(out=ST[rq, :], data0=D0[rq, :], data1=D1[rq, :], initial=0.0,
                                                    op0=ALU.mult, op1=ALU.add), r=["F0", "F2"], w=["F3"])
        if GLA_STAGE < 6:
            return
        P.cp("act", SB[rq, :].rearrange("p (c e) -> p c e", c=64), apx(ST[rq, 0:1], [[1, 64], [64, 64]]),
             r=["F3"], w=["B3"])
        if GLA_STAGE < 7:
            return
        r0m = 256 + h * 64
        BO = KE
        for g in range(8):
            gs = slice(g * 512, (g + 1) * 512)
            po = self.psn(banks)
            for cc in range(8):
                c = g * 8 + cc
                i = c // 2; r0 = (c % 2) * 64
                cs = slice(cc * 64, (cc + 1) * 64)
                P.mm(self.PS[po][0:64, cs], self.VT[:, i, :], ATv[:, i, r0:r0 + 64],
                     start=True, stop=(c == 0), r=["VT", "B2"], w=[f"ps{po}"])
                if c > 0:
                    P.mm(self.PS[po][0:64, cs], SB[rq, (c - 1) * 64:c * 64], QE[rq, c * 64:(c + 1) * 64],
                         start=False, stop=True, r=["B3", "B0"], w=[f"ps{po}"])
            ot = self.T5[0]; sq = self.T5[1]; rst = self.T5[2]; sg = self.T5[3]
            P.act(ot[0:64, :], self.PS[po][0:64, :], AF.Copy, r=[f"ps{po}"], w=["T5_0"])
            P.act(sq[0:64, :], self.PS[po][0:64, :], AF.Square, r=[f"ps{po}"], w=["T5_1"])
            pm = self.psn(banks)
            P.mm(self.PS[pm][0:64, :], self.onesf[0:64, 0:64], sq[0:64, :], start=True, stop=True,
                 r=["CF", "T5_1"], w=[f"ps{pm}"])
            P.ts("dve", rst[0:64, :], self.PS[pm][0:64, :], 1.0 / 64, EPS, ALU.mult, ALU.add, r=[f"ps{pm}"], w=["T5_2"])
            P.act(rst[0:64, :], rst[0:64, :], AF.Sqrt, r=["T5_2"], w=["T5_2"])
            P.op("dve", lambda g_, rst=rst: g_.reciprocal(out=rst[0:64, :], in_=rst[0:64, :]), r=["T5_2"], w=["T5_2"])
            pg = self.psn(banks)
            self.proj_fm(pg, slot, 128, 64, g)
            P.act(sg[0:64, :], self.PS[pg][0:64, :], AF.Silu, r=[f"ps{pg}"], w=["T5_3"])
            P.tt("dve", ot[0:64, :], ot[0:64, :], rst[0:64, :], ALU.mult, r=["T5_0", "T5_2"], w=["T5_0"])
            P.stt(BO[0:64, gs], ot[0:64, :], self.CHP[0:64, CP_NG + h:CP_NG + h + 1],
                  sg[0:64, :], ALU.mult, ALU.mult, r=["T5_0", "T5_3", "CHP"], w=["B1"])
        P.dma("sp", f"mixw{h % 2}", self.mixT[r0m:r0m + 64, :], BO[0:64, :], r=["B1"], w=["mixT"])

    def unit_fox(self, l, h):
        P = self
        slot = h % 2
        WBs = self.WB[slot]
        wn = f"WB{slot}"
        self.load_w(slot, l, [(C_CQ + h * 64, 64), (C_CF + h, 1), (C_CF + h, 1),
                              (C_CK + h * 64, 64), (C_CV + h * 64, 64)])
        QA = self.BA[:, 0, :]; KA = self.BA[:, 1, :]; CO = self.BA[:, 2, :]; TB = self.BA[:, 3, :]
        FZ = self.F(1); CC = self.F(2); HF = self.F(3)
        r2 = slice(64, 66)
        fb = self.CHP[64:66, CP_FB + h:CP_FB + h + 1]
        banks = [0, 1, 2, 3, 4, 5]
        for g in range(8):
            gs = slice(g * 512, (g + 1) * 512)
            a = self.psn(banks)
            self.proj_fm(a, slot, 0, 66, g)
            P.act(QA[0:64, gs], self.PS[a][0:64, :], AF.Copy, r=[f"ps{a}"], w=["B0"], scale=0.125)
            P.ts("dve", FZ[r2, gs], self.PS[a][r2, :], fb, None, ALU.add, r=[f"ps{a}", "CHP"], w=["F1"])
            b = self.psn(banks)
            self.proj_fm(b, slot, 66, 64, g)
            P.cp("dve", KA[0:64, gs], self.PS[b][0:64, :], r=[f"ps{b}"], w=["B1"])
            self.proj_v_tm(slot, 130, g, banks)
        P.act(FZ[r2, :], FZ[r2, :], AF.Exp, r=["F1"], w=["F1"], scale=-1.0)
        P.ts("dve", FZ[r2, :], FZ[r2, :], 1.0, None, ALU.add, r=["F1"], w=["F1"])
        P.act(FZ[r2, :], FZ[r2, :], AF.Ln, r=["F1"], w=["F1"])
        ones512 = self.CF[r2, CF_ONES:CF_ONES + 128]
        for pc in range(32):
            sl = slice(pc * 128, (pc + 1) * 128)
            init = 0.0 if pc == 0 else CC[r2, pc * 128 - 1:pc * 128]
            P.op("dve", lambda g_, sl=sl, init=init: g_.tensor_tensor_scan(
                out=CC[r2, sl], data0=ones512, data1=FZ[r2, sl], initial=init,
                op0=ALU.mult, op1=ALU.subtract), r=["F1", "F2", "CF"], w=["F2"])
        P.cp("dve", TB[r2, :], CC[r2, :], r=["F2"], w=["B3"])
        P.cp("dve", HF[r2, :], TB[r2, :], r=["B3"], w=["F3"])
        P.tt("dve", FZ[r2, :], CC[r2, :], HF[r2, :], ALU.subtract, r=["F2", "F3"], w=["F1"])
        P.ts("dve", HF[r2, :], HF[r2, :], self.CF[r2, CF_E0:CF_E0 + 1], None, ALU.mult, r=["F3", "CF"], w=["F3"])
        P.stt(QA[r2, :], FZ[r2, :], self.CF[r2, CF_E1:CF_E1 + 1], HF[r2, :], ALU.mult, ALU.add,
              r=["F1", "F3", "CF"], w=["B0"])
        P.op("dve", lambda g_: g_.memset(KA[r2, :], 1.0), w=["B1"])
        pb = self.psn(banks)
        for i in range(NT):
            P.tr(self.PS[pb][:, i:i + 1], CC[64:65, i * 128:(i + 1) * 128], self.identf[64:65, 64:65],
                 r=["F2", "CF"], w=[f"ps{pb}"])
        P.act(self.CT[:], self.PS[pb][:, 0:NT], AF.Copy, r=[f"ps{pb}"], w=["CT"], scale=-1.0)
        pO, pS = 6, 7
        iters = [(qg, kb) for qg in range(8) for kb in range(4 * (qg + 1))]
        psts = {}

        def stageA(n):
            qg, kb = iters[n]
            gs0 = qg * 512
            col0 = max(0, (kb - 4 * qg) * 128)
            pst = self.psn(banks)
            psts[n] = pst
            P.mm(self.PS[pst][:, col0:512], KA[0:66, kb * 128:(kb + 1) * 128], QA[0:66, gs0 + col0:gs0 + 512],
                 start=True, stop=True, r=["B0", "B1"], w=[f"ps{pst}"])

        LOOK = 2
        for n in range(min(LOOK, len(iters))):
            stageA(n)
        for n, (qg, kb) in enumerate(iters):
            if n + LOOK < len(iters):
                stageA(n + LOOK)
            nkb = 4 * (qg + 1)
            gs0 = qg * 512
            col0 = max(0, (kb - 4 * qg) * 128)
            pst = psts.pop(n)
            pt = self.PT[n % 3]; ptn = f"PT{n % 3}"
            P.act(pt[:, col0:512], self.PS[pst][:, col0:512], AF.Exp, r=[f"ps{pst}", "CT"], w=[ptn],
                  bias=self.CT[:, kb:kb + 1], scale=1.0)
            if kb >= 4 * qg:
                P.tt("pool", pt[:, col0:col0 + 128], pt[:, col0:col0 + 128],
                     self.CB[:, CB_CMASK:CB_CMASK + 128], ALU.mult, r=[ptn, "CB"], w=[ptn])
            P.mm(self.PS[pO][0:64, col0:512], self.VT[:, kb, :], pt[:, col0:512],
                 start=(kb == 0), stop=(kb == nkb - 1), r=["VT", ptn], w=[f"ps{pO}"])
            P.mm(self.PS[pS][0:64, col0:512], self.onesb[:, 0:64], pt[:, col0:512],
                 start=(kb == 0), stop=(kb == nkb - 1), r=["CB", ptn], w=[f"ps{pS}"])
            if kb == nkb - 1:
                rs = self.T5[5]
                P.op("dve", lambda g_, rs=rs: g_.reciprocal(out=rs[0:64, :], in_=self.PS[pS][0:64, :]),
                     r=[f"ps{pS}"], w=["T5_5"])
                P.tt("dve", CO[0:64, gs0:gs0 + 512], self.PS[pO][0:64, :], rs[0:64, :], ALU.mult,
                     r=[f"ps{pO}", "T5_5"], w=["B2"])
        r0 = 640 + h * 64
        P.dma("sp", f"mixw{h % 2}", self.mixT[r0:r0 + 64, :], CO[0:64, :], r=["B2"], w=["mixT"])

    def phase_out(self, l, src):
        P = self
        WO = self.BA[:, 0:2, :].rearrange("p a (c n) -> p (a c) n", n=1024)
        for c in range(8):
            P.dma("pool", "wo", WO[:, c, :], self.w_out[l, c * 128:(c + 1) * 128, :], r=[], w=["B0", "B1"])
        G1 = self.FA[:, 0, 30:1054]; B1 = self.FA[:, 0, 1054:2078]
        P.dma("sp", "lnp", G1, self.ln1_g[l:l + 1, :].partition_broadcast(128), r=[], w=["F0"])
        P.dma("sp", "lnp", B1, self.ln1_b[l:l + 1, :].partition_broadcast(128), r=[], w=["F0"])
        for i in range(NT):
            g = i // 4
            ms = g % 2
            MX = self.BA[:, 2 + ms, :].rearrange("p (c n) -> p c n", c=8)
            if i % 4 == 0:
                P.dma("sp", f"mx{ms}", MX, self.mixT[:, g * 512:(g + 1) * 512].rearrange("(c p) n -> p c n", p=128),
                      r=["mixT"], w=[f"B{2 + ms}"])
            s2 = i % 2
            XR = self.FA[:, 1, s2 * 1024:(s2 + 1) * 1024]
            R = self.FA[:, 1, 2048 + s2 * 1024:2048 + (s2 + 1) * 1024]
            xrn = f"XR{s2}"; rn = f"R{s2}"
            P.dma("sp", f"xr{s2}", XR, src[i * 128:(i + 1) * 128, :], r=["xsrc"], w=[xrn])
            for half in range(2):
                pb = self.psn([2, 3, 4, 5])
                for c in range(8):
                    P.mm(self.PS[pb][:], MX[:, c, (i % 4) * 128:(i % 4 + 1) * 128], WO[:, c, half * 512:(half + 1) * 512],
                         start=(c == 0), stop=(c == 7), r=[f"B{2 + ms}", "B0", "B1"], w=[f"ps{pb}"])
                P.stt(R[:, half * 512:(half + 1) * 512], XR[:, half * 512:(half + 1) * 512], ALPHA, self.PS[pb][:],
                      ALU.mult, ALU.add, r=[xrn, f"ps{pb}"], w=[rn])
            self.layernorm_tile(R, rn, G1, B1, "F0")
            P.dma("sp", f"x1w{s2}", self.x1d[i * 128:(i + 1) * 128, :], R, r=[rn], w=["x1d"])
            xb = self.XBT[s2]
            P.cp("act", xb[:], R, r=[rn], w=[f"XBT{s2}"])
            self.transpose_to_XT(xb, f"XBT{s2}", i)

    def layernorm_tile(self, R, rn, G, Bb, gbn):
        P = self
        st = self.SM[:, 0:12]; mv = self.SM[:, 12:14]; rs = self.SM[:, 14:15]
        for half in range(2):
            P.op("dve", lambda g_, half=half: g_.bn_stats(out=st[:, half * 6:(half + 1) * 6],
                                                        in_=R[:, half * 512:(half + 1) * 512]), r=[rn], w=["SM"])
        P.op("dve", lambda g_: g_.bn_aggr(out=mv, in_=st), r=["SM"], w=["SM"])
        P.ts("dve", rs, mv[:, 1:2], EPS, None, ALU.add, r=["SM"], w=["SM"])
        P.act(rs, rs, AF.Sqrt, r=["SM"], w=["SM"])
        P.op("dve", lambda g_: g_.reciprocal(out=rs, in_=rs), r=["SM"], w=["SM"])
        P.ts("dve", R, R, mv[:, 0:1], rs, ALU.subtract, ALU.mult, r=[rn, "SM"], w=[rn])
        P.tt("pool", R, R, G, ALU.mult, r=[rn, gbn], w=[rn])
        P.tt("dve", R, R, Bb, ALU.add, r=[rn, gbn], w=[rn])

    def phase_peer(self, l):
        P = self
        dst = self.y if (l == DEPTH - 1) else self.xcur
        dstn = "y" if (l == DEPTH - 1) else "xsrc"
        allb = [0, 1, 2, 3, 4, 5]
        WQ = self.BA[:, 0:4, :].rearrange("p a (c n) -> p (a c) n", n=2048)
        for c in range(8):
            P.dma("pool", "wo", WQ[:, c, :], self.wq[l, c * 128:(c + 1) * 128, :], r=[], w=["B0", "B1", "B2", "B3"])
        KT = self.VT[:].rearrange("p a b -> p (a b)").rearrange("p (h n) -> p h n", h=16)
        P.dma("pool", "kt", KT, self.keysT[l].rearrange("h d n -> d h n"), r=[], w=["VT"])
        QPT = self.KET[:].rearrange("p a b c -> p (a b c)").rearrange("p (h t) -> p h t", h=16)
        GW2 = self.FA[:, 3, 0:4096].bitcast(BF16).rearrange("p (c n) -> p c n", c=8)
        for c in range(8):
            P.dma("pool", "gw2", GW2[:, c, :], self.ple_gw[l, c * 128:(c + 1) * 128, :], r=[], w=["F3"])
        PW = self.FA[:, 2, 0:1024].bitcast(BF16).rearrange("p (c n) -> p c n", c=2)
        for c in range(2):
            P.dma("pool", "pw", PW[:, c, :], self.ple_w[l, c * 128:(c + 1) * 128, :], r=[], w=["F2"])
        G2 = self.FA[:, 2, 1024:2048]; B2 = self.FA[:, 2, 2048:3072]; GB = self.FA[:, 2, 3072:4096]
        P.dma("sp", "lnp", G2, self.ln2_g[l:l + 1, :].partition_broadcast(128), r=[], w=["F2"])
        P.dma("sp", "lnp", B2, self.ln2_b[l:l + 1, :].partition_broadcast(128), r=[], w=["F2"])
        P.dma("sp", "lnp", GB, self.ple_gb[l:l + 1, :].partition_broadcast(128), r=[], w=["F2"])
        gsa = self.FA[:, 0, 30:4126].bitcast(BF16)
        gsb = self.FA[:, 1, 1024:2048].bitcast(BF16)
        GS = [gsa[:, k * 1024:(k + 1) * 1024] for k in range(8)] + [gsb[:, k * 1024:(k + 1) * 1024] for k in range(2)]
        NGS = len(GS)
        JUNK = self.FA[:, 1, 0:1024]
        X1 = self.FA[:, 1, 2048:3072]; ACC = self.FA[:, 1, 3072:4096]
        PK = self.PK
        SV = PK[:, 0:256].rearrange("p (h k) -> p h k", h=16)
        SIf = PK[:, 256:512].rearrange("p (h k) -> p h k", h=16)
        CV = PK[:, 512:640].rearrange("p (h k) -> p h k", h=8)
        CA = PK[:, 640:768].rearrange("p (h k) -> p h k", h=8)
        CBf = PK[:, 768:896].rearrange("p (h k) -> p h k", h=8)
        I1 = PK[:, 896:1024]; I2 = PK[:, 1024:1152]
        GATE = PK[:, 1152:1280]; H = PK[:, 1280:1408]; W = PK[:, 1408:1536]
        SI = self.PT[1][:].bitcast(U32).rearrange("p (h k) -> p h k", h=16)
        CI = self.PT[2][:].bitcast(U32)[:, 0:128].rearrange("p (h k) -> p h k", h=8)
        CI2 = self.PT[2][:].bitcast(U32)[:, 128:256].rearrange("p (h k) -> p h k", h=8)
        iota = self.CF[:, CF_IOTA:CF_IOTA + 16]
        pu_flat = self.ub
        pv_flat = self.vb
        NEG = -1.0e30

        def top16(vals, wk, sv_out, si_out, rd, wkn):
            P.op("dve", lambda g_: g_.max(out=sv_out[:, 0:8], in_=vals), r=rd, w=["PK"])
            yield
            P.op("dve", lambda g_: g_.max_index(out=si_out[:, 0:8], in_max=sv_out[:, 0:8], in_values=vals),
                 r=rd + ["PK"], w=["PKI"])
            yield
            P.op("dve", lambda g_: g_.match_replace(out=wk, in_to_replace=sv_out[:, 0:8], in_values=vals,
                                                   imm_value=NEG), r=rd + ["PK"], w=[wkn])
            yield
            P.op("dve", lambda g_: g_.max(out=sv_out[:, 8:16], in_=wk), r=[wkn], w=["PK"])
            yield
            P.op("dve", lambda g_: g_.max_index(out=si_out[:, 8:16], in_max=sv_out[:, 8:16], in_values=wk),
                 r=[wkn, "PK"], w=["PKI"])
            yield

        def topk_gen(i, eb):
            ts_ = slice(i * 128, (i + 1) * 128)
            EIDX = self.EIDX[:, eb, :]
            en = f"EIDX{eb}"
            for q4 in range(4):
                bk = self.psn(allb)
                for j in range(4):
                    hc = 4 * q4 + j
                    for c in range(8):
                        P.mm(self.PS[bk][:, j * 128:(j + 1) * 128], WQ[:, c, hc * 128:(hc + 1) * 128], self.XT[:, c, ts_],
                             start=(c == 0), stop=(c == 7), r=["B0", "XT"], w=[f"ps{bk}"])
                P.cp("act", QPT[:, 4 * q4:4 * q4 + 4, :], self.PS[bk][:].rearrange("p (j t) -> p j t", j=4),
                     r=[f"ps{bk}"], w=["KET"])
            for q4 in range(4):
                bk = self.psn(allb)
                for j in range(4):
                    hc = 4 * q4 + j
                    P.mm(self.PS[bk][:, j * 128:(j + 1) * 128], QPT[:, hc, :], KT[:, hc, :], start=True, stop=True,
                         r=["KET", "VT"], w=[f"ps{bk}"])
                sc = self.T5[q4 % 2]; scn = f"T5_{q4 % 2}"
                wk = self.T5[2 + q4 % 2]; wkn = f"T5_{2 + q4 % 2}"
                P.cp("act", sc[:], self.PS[bk][:], r=[f"ps{bk}"], w=[scn])
                for j in range(4):
                    hc = 4 * q4 + j
                    yield from top16(sc[:, j * 128:(j + 1) * 128], wk[:, j * 128:(j + 1) * 128], SV[:, hc, :],
                                     SI[:, hc, :], [scn], wkn)
            for h in range(8):
                cd = self.T5[4]; cdn = "T5_4"
                cand = cd[:, 0:256]; cwk = cd[:, 256:512]
                P.tt("dve", cand.rearrange("p (a b) -> p a b", a=16), apx(SV[:, 2 * h, :], [[1, 16], [0, 16]]),
                     apx(SV[:, 2 * h + 1, :], [[0, 16], [1, 16]]), ALU.add, r=["PK"], w=[cdn])
                yield
                yield from top16(cand, cwk, CV[:, h, :], CI[:, h, :], [cdn], cdn)
            P.op("dve", lambda g_: g_.tensor_single_scalar(out=CI2[:], in_=CI[:], scalar=4, op=ALU.logical_shift_right),
                 r=["PKI"], w=["PKI2"])
            yield
            P.cp("dve", CA[:], CI2[:], r=["PKI2"], w=["PKA"])
            yield
            P.op("dve", lambda g_: g_.tensor_single_scalar(out=CI2[:], in_=CI[:], scalar=15, op=ALU.bitwise_and),
                 r=["PKI", "PKA"], w=["PKI2"])
            yield
            P.cp("dve", CBf[:], CI2[:], r=["PKI2"], w=["PKA"])
            yield
            P.cp("dve", SIf[:], SI[:], r=["PKI"], w=["PKA"])
            yield
            for h in range(8):
                for c, (src, dstI) in enumerate(((CA, I1), (CBf, I2))):
                    eq = self.T5[5][:, c * 256:(c + 1) * 256]; eqn = "T5_5"
                    eq3 = eq.rearrange("p (k a) -> p k a", k=16)
                    P.tt("dve", eq3, apx(src[:, h, :], [[1, 16], [0, 16]]), apx(iota, [[0, 16], [1, 16]]),
                         ALU.is_equal, r=["PKA", "CF"], w=[eqn])
                    yield
                    P.tt("dve", eq3, eq3, apx(SIf[:, 2 * h + c, :], [[0, 16], [1, 16]]), ALU.mult, r=[eqn, "PKA"], w=[eqn])
                    yield
                    P.op("dve", lambda g_, eq3=eq3, dstI=dstI, h=h: g_.tensor_reduce(
                        out=dstI[:, h * 16:(h + 1) * 16], in_=eq3, axis=AX.X, op=ALU.add), r=[eqn], w=["PKB"])
                    yield
            P.ts("dve", I1, I1, 128.0, 0.0, ALU.mult, ALU.add, r=["PKB"], w=["PKB"])
            yield
            P.tt("dve", I1, I1, I2, ALU.add, r=["PKB"], w=["PKB"])
            yield
            P.cp("dve", EIDX, I1, r=["PKB"], w=[en])
            yield
            G3 = GATE.rearrange("p (h k) -> p h k", h=8)
            P.tt("dve", G3, CV, apx(CV[:, 0, 0:1], [[16, 8], [0, 16]]), ALU.subtract, r=["PK"], w=["GATE"])
            yield
            P.act(GATE, GATE, AF.Exp, r=["GATE"], w=["GATE"])
            zs = self.SM[:, 16:24]
            P.op("dve", lambda g_, G3=G3: g_.tensor_reduce(out=zs, in_=G3, axis=AX.X, op=ALU.add), r=["GATE"], w=["SM2"])
            yield
            P.op("dve", lambda g_: g_.reciprocal(out=zs, in_=zs), r=["SM2"], w=["SM2"])
            yield
            P.tt("dve", G3, G3, apx(zs[:, 0:1], [[1, 8], [0, 16]]), ALU.mult, r=["GATE", "SM2"], w=["GATE"])
            yield

        def gather(table, tn, eb, k):
            sl = self.gs_rot % NGS
            self.gs_rot += 1
            P.kb.add("pool", lambda g_: g_.indirect_dma_start(
                out=GS[sl], out_offset=None, in_=table,
                in_offset=bass.IndirectOffsetOnAxis(ap=self.EIDX[:, eb, k:k + 1], axis=0)),
                self._b([f"EIDX{eb}", tn]), self._b([f"GS{sl}"]), dma=f"gs{sl}")
            return GS[sl], f"GS{sl}"

        nt = PEER_TILES
        for _ in topk_gen(0, 0):
            pass
        for i in range(nt):
            ts_ = slice(i * 128, (i + 1) * 128)
            eb = i % 2
            nxt = topk_gen(i + 1, 1 - eb) if i + 1 < nt else None
            P.dma("sp", "x1r", X1, self.x1d[ts_, :], r=["x1d"], w=["X1"])
            for k in range(128):
                g, gn = gather(pu_flat, "UB", eb, k)
                P.stt(JUNK, g, 1.0, X1, ALU.mult, ALU.mult, r=[gn, "X1"], w=["JUNK", "H"], accum_out=H[:, k:k + 1])
            P.tt("dve", W, H, H, ALU.mult, r=["H"], w=["W"])
            P.ts("dve", W, W, 0.044715, 1.0, ALU.mult, ALU.add, r=["W"], w=["W"])
            P.tt("dve", W, W, H, ALU.mult, r=["W", "H"], w=["W"])
            P.act(W, W, AF.Sigmoid, r=["W"], w=["W"], scale=1.5957691216057308)
            P.tt("dve", W, W, H, ALU.mult, r=["W", "H"], w=["W"])
            P.tt("dve", W, W, GATE, ALU.mult, r=["W", "GATE"], w=["W"])
            for k in range(128):
                g, gn = gather(pv_flat, "VB", eb, k)
                dgs = k % 4
                dg = self.XBT[1][:, 256 + dgs * 128:256 + (dgs + 1) * 128]
                P.act(dg, self.identb, AF.Copy, r=["CB", "W"], w=[f"DG{dgs}"], scale=W[:, k:k + 1])
                for half in range(2):
                    P.mm(self.PS[6 + half][:], dg, g[:, half * 512:(half + 1) * 512], start=(k == 0), stop=(k == 127),
                         r=[f"DG{dgs}", gn], w=[f"ps{6 + half}"])
                if nxt is not None:
                    for _ in range(2):
                        if next(nxt, "end") == "end":
                            nxt = None
                            break
            if nxt is not None:
                for _ in nxt:
                    pass
            for half in range(2):
                hs = slice(half * 512, (half + 1) * 512)
                P.stt(ACC[:, hs], X1[:, hs], ALPHA, self.PS[6 + half][:], ALU.mult, ALU.add,
                      r=["X1", f"ps{6 + half}"], w=["ACC"])
            xb = self.XBT[0]
            P.cp("act", xb[:], ACC, r=["ACC"], w=["XBT0"])
            bk = self.psn(allb)
            psb = self.PS[bk][:].bitcast(BF16)
            for c in range(8):
                P.tr(psb[:, c * 128:(c + 1) * 128], xb[:, c * 128:(c + 1) * 128], self.identb, r=["XBT0", "CB"], w=[f"ps{bk}"])
            RTB = self.T5[5][:].bitcast(BF16)
            RT = RTB.rearrange("p (c t) -> p c t", c=8)
            P.cp("act", RTB, psb, r=[f"ps{bk}"], w=["T5_5"])
            pb16 = self.XBT[1]
            P.dma("pool", "pld", pb16[:, 0:256], self.p_in[l, ts_, :], r=[], w=["XBT1"])
            bk2 = self.psn(allb)
            psb2 = self.PS[bk2][:].bitcast(BF16)
            for c in range(2):
                P.tr(psb2[:, c * 128:(c + 1) * 128], pb16[:, c * 128:(c + 1) * 128], self.identb, r=["XBT1", "CB"], w=[f"ps{bk2}"])
            PTT = self.PT[0][:, 0:256].rearrange("p (c t) -> p c t", c=2)
            P.cp("act", self.PT[0][:, 0:256], psb2[:, 0:256], r=[f"ps{bk2}"], w=["PT0"])
            for half in range(2):
                hs = slice(half * 512, (half + 1) * 512)
                pg = self.psn(allb)
                for c in range(8):
                    P.mm(self.PS[pg][:], RT[:, c, :], GW2[:, c, hs], start=(c == 0), stop=(c == 7),
                         r=["T5_5", "F3"], w=[f"ps{pg}"])
                pp = self.psn(allb)
                for c in range(2):
                    P.mm(self.PS[pp][:], PTT[:, c, :], PW[:, c, hs], start=(c == 0), stop=(c == 1),
                         r=["PT0", "F2"], w=[f"ps{pp}"])
                t = self.T5[half]; tn = f"T5_{half}"
                P.tt("dve", t[:], self.PS[pg][:], GB[:, hs], ALU.add, r=[f"ps{pg}", "F2"], w=[tn])
                P.act(t[:], t[:], AF.Sigmoid, r=[tn], w=[tn])
                P.tt("dve", t[:], t[:], self.PS[pp][:], ALU.mult, r=[tn, f"ps{pp}"], w=[tn])
                P.tt("dve", ACC[:, hs], ACC[:, hs], t[:], ALU.add, r=[tn, "ACC"], w=["ACC"])
            self.layernorm_tile(ACC, "ACC", G2, B2, "F2")
            P.dma("sp", "xw", dst[ts_, :], ACC, r=["ACC"], w=[dstn])


def host_tables(inp):
    L = DEPTH
    chanp = np.zeros((L, 128, CP_N), np.float32)
    for j in range(2):
        chanp[:, :, CP_CB + j] = inp["conv_b"][:, j * 128:(j + 1) * 128]
        chanp[:, :, CP_LG + j] = inp["conv_ln_g"][:, j * 128:(j + 1) * 128]
        chanp[:, :, CP_LB + j] = inp["conv_ln_b"][:, j * 128:(j + 1) * 128]
        chanp[:, :, CP_CW + j * 31:CP_CW + (j + 1) * 31] = np.transpose(
            inp["conv_w"][:, :, j * 128:(j + 1) * 128], (0, 2, 1))
    for h in range(6):
        chanp[:, 0:32, CP_GB + h] = inp["gla_gate_b"][:, h * 32:(h + 1) * 32]
        chanp[:, 0:64, CP_NG + h] = inp["gla_norm_g"][:, h * 64:(h + 1) * 64]
        chanp[:, :, CP_FB + h] = inp["fox_forget_b"][:, h][:, None]
    keysT = np.ascontiguousarray(
        np.transpose(np.asarray(inp["peer_keys"]).reshape(L, 16, 128, 128), (0, 1, 3, 2)))
    return chanp, keysT


SHARED = ["w_in", "gla_gate_w", "w_out", "ln1_g", "ln1_b", "peer_wq", "peer_u", "peer_v",
          "ple_w", "ple_gw", "ple_gb", "ln2_g", "ln2_b"]


def make_in_maps(inp, cores):
    inp = {k: np.asarray(v) for k, v in inp.items()}
    chanp, keysT = host_tables(inp)
    cf, cb = host_consts()
    shared = {k: np.ascontiguousarray(inp[k], dtype=np.float32) for k in SHARED}
    shared.update(chanp=chanp, keysT=keysT, cf32=cf, cb16=cb)
    maps = []
    for c in cores:
        m = dict(shared)
        m["x"] = np.ascontiguousarray(inp["x"][c])
        m["p"] = np.ascontiguousarray(inp["p"][:, c])
        maps.append(m)
    return maps


_PROG = None


def kernel(**inputs):
    global _PROG
    if _PROG is None:
        _PROG = Program()
        _PROG.build()
    maps = make_in_maps(inputs, list(range(8)))
    res = run_bass_kernel_spmd(_PROG.nc, maps, core_ids=list(range(8)))
    return np.stack([np.asarray(r["y"]) for r in res.results], axis=0).astype(np.float32)
```
